# Optimizing a Trainium2 kernel written in Bass

```python
import math
import jax, jax.numpy as jnp
from jax import lax
import numpy as np

D_MODEL = 1024
BATCH = 8
SEQ = 4096
DEPTH = 1

CHUNK = 64
D_MIX = D_MODEL
D_LRU = D_MIX // 2
D_SGU = D_MIX - D_LRU
LRU_HEADS = 8
LRU_HEAD_DIM = D_LRU // LRU_HEADS
CONV_WIDTH = 4
LRU_C = 8.0
SGU_HEADS = 8
SGU_HEAD_DIM = D_SGU // SGU_HEADS
SGU_BLOCK = 128
N_GROUPS = 4
EXPERTS_PER_GROUP = 8
N_EXPERTS = N_GROUPS * EXPERTS_PER_GROUP
TOP_K = 2
D_EXPERT = D_MODEL // 2
MOE_BLOCK = 512
EPS = 1e-6

kernel_name = "hybrid_rglru_gmlp_hmoe_block"


def rmsnorm(x, g):
    xf = x.astype(jnp.float32)
    y = xf * lax.rsqrt(jnp.mean(xf * xf, axis=-1, keepdims=True) + EPS)
    return (y * g.astype(jnp.float32)).astype(x.dtype)


def layernorm(x, g, b):
    xf = x.astype(jnp.float32)
    mu = jnp.mean(xf, axis=-1, keepdims=True)
    var = jnp.mean(jnp.square(xf - mu), axis=-1, keepdims=True)
    y = (xf - mu) * lax.rsqrt(var + EPS) * g.astype(jnp.float32) + b.astype(jnp.float32)
    return y.astype(x.dtype)


def causal_conv(x, w, b):
    y = lax.conv_general_dilated(
        x, w[:, None, :].astype(x.dtype), window_strides=(1,),
        padding=[(CONV_WIDTH - 1, 0)], dimension_numbers=('NWC', 'WIO', 'NWC'),
        feature_group_count=x.shape[-1])
    return y + b.astype(x.dtype)


def rg_lru(x, w_a, b_a, w_i, b_i, lam):
    B, S, _ = x.shape
    xf = x.astype(jnp.float32)
    xh = xf.reshape(B, S, LRU_HEADS, LRU_HEAD_DIM)
    r = jax.nn.sigmoid(jnp.einsum('bshd,hde->bshe', xh, w_a.astype(jnp.float32)).reshape(B, S, D_LRU)
                       + b_a.astype(jnp.float32))
    i = jax.nn.sigmoid(jnp.einsum('bshd,hde->bshe', xh, w_i.astype(jnp.float32)).reshape(B, S, D_LRU)
                       + b_i.astype(jnp.float32))
    log_a = -LRU_C * r * jax.nn.softplus(-lam.astype(jnp.float32))
    a = jnp.exp(log_a)
    u = jnp.sqrt(-jnp.expm1(2.0 * log_a)) * (i * xf)

    def combine(lhs, rhs):
        a1, b1 = lhs
        a2, b2 = rhs
        return a1 * a2, a2 * b1 + b2

    _, h = lax.associative_scan(combine, (a, u), axis=1)
    return h.astype(x.dtype)


def spatial_gating(u, v, ln_g, ln_b, w_s, b_s):
    B, S, _ = v.shape
    pos_chunk = jnp.arange(SGU_BLOCK) // CHUNK
    mask = (pos_chunk[None, :] <= pos_chunk[:, None]).astype(w_s.dtype)
    vn = layernorm(v, ln_g, ln_b)
    vb = vn.reshape(B, S // SGU_BLOCK, SGU_BLOCK, SGU_HEADS, SGU_HEAD_DIM)
    s = jnp.einsum('gij,bnjgc->bnigc', (w_s * mask[None]).astype(v.dtype), vb)
    s = s + b_s.T.astype(v.dtype)[None, None, :, :, None]
    return u * s.reshape(B, S, D_SGU)


def hierarchical_moe(h, w_grp, b_grp, w_exp, b_exp, w1, w3, w2):
    B, S, D = h.shape
    T = B * S
    xt = h.reshape(T, D)
    g_logits = (xt @ w_grp).astype(jnp.float32) + b_grp.astype(jnp.float32)
    g_prob = jax.nn.softmax(g_logits, axis=-1)
    g_sel = jnp.argmax(g_logits, axis=-1)
    p_g = jnp.take_along_axis(g_prob, g_sel[:, None], axis=1)[:, 0]
    e_logits = ((xt @ w_exp).astype(jnp.float32) + b_exp.astype(jnp.float32)).reshape(T, N_GROUPS, EXPERTS_PER_GROUP)
    e_logits = jnp.take_along_axis(e_logits, g_sel[:, None, None], axis=1)[:, 0]
    e_prob = jax.nn.softmax(e_logits, axis=-1)
    top_p, top_i = lax.top_k(e_prob, TOP_K)
    top_p = top_p / jnp.sum(top_p, axis=-1, keepdims=True)
    weights = (p_g[:, None] * top_p).reshape(-1)
    expert = (g_sel[:, None] * EXPERTS_PER_GROUP + top_i).reshape(-1).astype(jnp.int32)
    token = jnp.repeat(jnp.arange(T, dtype=jnp.int32), TOP_K)

    n_slots = T * TOP_K
    cap = (n_slots + MOE_BLOCK - 1) // MOE_BLOCK * MOE_BLOCK + N_EXPERTS * MOE_BLOCK
    n_blocks = cap // MOE_BLOCK
    order = jnp.argsort(expert)
    e_sorted = expert[order]
    counts = jnp.bincount(expert, length=N_EXPERTS)
    padded = (counts + MOE_BLOCK - 1) // MOE_BLOCK * MOE_BLOCK
    start = jnp.cumsum(counts) - counts
    pend = jnp.cumsum(padded)
    pstart = pend - padded
    dest = pstart[e_sorted] + (jnp.arange(n_slots, dtype=jnp.int32) - start[e_sorted])
    tok_buf = jnp.full((cap,), T, jnp.int32).at[dest].set(token[order])
    w_buf = jnp.zeros((cap,), jnp.float32).at[dest].set(weights[order])
    block_expert = jnp.minimum(
        jnp.searchsorted(pend, jnp.arange(n_blocks, dtype=jnp.int32) * MOE_BLOCK, side='right'),
        N_EXPERTS - 1).astype(jnp.int32)

    x_pad = jnp.concatenate([xt, jnp.zeros((1, D), xt.dtype)], axis=0)
    xs = x_pad[tok_buf].reshape(n_blocks, MOE_BLOCK, D)

    def expert_block(args):
        xb, e = args
        return (jax.nn.silu(xb @ w1[e]) * (xb @ w3[e])) @ w2[e]

    ys = lax.map(expert_block, (xs, block_expert)).reshape(cap, D)
    ys = ys * w_buf[:, None].astype(ys.dtype)
    out = jnp.zeros((T + 1, D), ys.dtype).at[tok_buf].add(ys)[:T]
    return out.reshape(B, S, D)


def setup_inputs(seed: int = 0) -> dict:
    key = jax.random.key(seed)
    ks = jax.random.split(key, 32)
    f32 = jnp.float32
    L = DEPTH
    nrm = lambda k, shape, scale: jax.random.normal(k, shape, f32) * scale
    gain = lambda k, shape: 1.0 + 0.01 * jax.random.normal(k, shape, f32)
    base = jax.random.uniform(ks[10], (L, D_LRU), f32, 0.9, 0.999) ** (1.0 / LRU_C)
    lru_lambda = jnp.log(base) - jnp.log1p(-base)
    return {
        "x": nrm(ks[0], (BATCH, SEQ, D_MODEL), 1.0),
        "c": nrm(ks[1], (BATCH, D_MODEL), 1.0),
        "ada_w": nrm(ks[2], (L, D_MODEL, 6 * D_MODEL), D_MODEL ** -0.5),
        "ada_b": nrm(ks[3], (L, 6 * D_MODEL), 0.01),
        "norm1_g": gain(ks[4], (L, D_MODEL)),
        "w_in": nrm(ks[5], (L, D_MODEL, 2 * D_MIX), D_MODEL ** -0.5),
        "conv_w": nrm(ks[6], (L, CONV_WIDTH, D_LRU), CONV_WIDTH ** -0.5),
        "conv_b": nrm(ks[7], (L, D_LRU), 0.01),
        "gate_a_w": nrm(ks[8], (L, LRU_HEADS, LRU_HEAD_DIM, LRU_HEAD_DIM), LRU_HEAD_DIM ** -0.5),
        "gate_a_b": nrm(ks[9], (L, D_LRU), 0.01),
        "gate_i_w": nrm(ks[11], (L, LRU_HEADS, LRU_HEAD_DIM, LRU_HEAD_DIM), LRU_HEAD_DIM ** -0.5),
        "gate_i_b": nrm(ks[12], (L, D_LRU), 0.01),
        "lru_lambda": lru_lambda,
        "sgu_ln_g": gain(ks[13], (L, D_SGU)),
        "sgu_ln_b": nrm(ks[14], (L, D_SGU), 0.01),
        "sgu_w": nrm(ks[15], (L, SGU_HEADS, SGU_BLOCK, SGU_BLOCK), SGU_BLOCK ** -0.5),
        "sgu_b": gain(ks[16], (L, SGU_HEADS, SGU_BLOCK)),
        "w_out": nrm(ks[17], (L, D_MIX, D_MODEL), D_MIX ** -0.5),
        "norm2_g": gain(ks[18], (L, D_MODEL)),
        "router_group_w": nrm(ks[19], (L, D_MODEL, N_GROUPS), D_MODEL ** -0.5),
        "router_group_b": nrm(ks[20], (L, N_GROUPS), 0.01),
        "router_expert_w": nrm(ks[21], (L, D_MODEL, N_EXPERTS), D_MODEL ** -0.5),
        "router_expert_b": nrm(ks[22], (L, N_EXPERTS), 0.01),
        "expert_w1": nrm(ks[23], (L, N_EXPERTS, D_MODEL, D_EXPERT), D_MODEL ** -0.5),
        "expert_w3": nrm(ks[24], (L, N_EXPERTS, D_MODEL, D_EXPERT), D_MODEL ** -0.5),
        "expert_w2": nrm(ks[25], (L, N_EXPERTS, D_EXPERT, D_MODEL), D_EXPERT ** -0.5),
        "final_g": gain(ks[26], (D_MODEL,)),
    }


def reference(x, c, ada_w, ada_b, norm1_g, w_in, conv_w, conv_b, gate_a_w, gate_a_b, gate_i_w,
              gate_i_b, lru_lambda, sgu_ln_g, sgu_ln_b, sgu_w, sgu_b, w_out, norm2_g,
              router_group_w, router_group_b, router_expert_w, router_expert_b,
              expert_w1, expert_w3, expert_w2, final_g):
    cond = jax.nn.silu(c)
    for l in range(DEPTH):
        mod = cond @ ada_w[l] + ada_b[l]
        sh1, sc1, g1, sh2, sc2, g2 = jnp.split(mod, 6, axis=-1)
        h = rmsnorm(x, norm1_g[l]) * (1.0 + sc1[:, None]) + sh1[:, None]
        z = h @ w_in[l]
        xr, gr, u, v = jnp.split(z, [D_LRU, 2 * D_LRU, 2 * D_LRU + D_SGU], axis=-1)
        xr = causal_conv(xr, conv_w[l], conv_b[l])
        y_lru = rg_lru(xr, gate_a_w[l], gate_a_b[l], gate_i_w[l], gate_i_b[l], lru_lambda[l]) * jax.nn.gelu(gr)
        y_sgu = spatial_gating(jax.nn.gelu(u), jax.nn.gelu(v), sgu_ln_g[l], sgu_ln_b[l], sgu_w[l], sgu_b[l])
        mix = jnp.concatenate([y_lru, y_sgu], axis=-1) @ w_out[l]
        x = x + g1[:, None] * mix
        h = rmsnorm(x, norm2_g[l]) * (1.0 + sc2[:, None]) + sh2[:, None]
        y = hierarchical_moe(h, router_group_w[l], router_group_b[l], router_expert_w[l], router_expert_b[l],
                             expert_w1[l], expert_w3[l], expert_w2[l])
        x = x + g2[:, None] * y
    return rmsnorm(x, final_g)
```

```python
import contextlib
import numpy as np
import ml_dtypes
import concourse.bass as bass
import concourse.mybir as mybir
from concourse.bass_utils import run_bass_kernel_spmd

F32 = mybir.dt.float32
BF16 = mybir.dt.bfloat16
I32 = mybir.dt.int32
ALU = mybir.AluOpType
AF = mybir.ActivationFunctionType
AX = mybir.AxisListType

D = 1024
S = 4096
NT = S // 128
NE = 32
DH = 512
EPS = 1e-6
DEBUG_STOP = None
import os
KCUT = os.environ.get('KCUT', '')


class Prog:
    ENG = ("pe", "act", "dve", "pool", "sp")

    def __init__(self, nc):
        self.nc = nc
        self.es = contextlib.ExitStack()
        self.ops = {e: [] for e in self.ENG}
        self.cnt = {e: 0 for e in self.ENG}
        self.esem = {e: self.es.enter_context(nc.semaphore("s_" + e)) for e in self.ENG}
        self.seen = {e: {} for e in self.ENG}
        self.dsem = {}
        self.state = {}
        self.nsem = 0

    def sb(self, name, shape, dt):
        return self.es.enter_context(self.nc.sbuf_tensor(name, list(shape), dt))

    def ps(self, name, shape, dt):
        return self.es.enter_context(self.nc.psum_tensor(name, list(shape), dt))

    def _st(self, k):
        s = self.state.get(k)
        if s is None:
            s = self.state[k] = {"w": [], "r": []}
        return s

    def _deps(self, eng, reads, writes):
        need = []
        for k in reads:
            if k.startswith("bank"):
                s = self._st(k)
                need += [(ev, True) for ev in s["w"] + s["r"]]
            else:
                need += [(ev, False) for ev in self._st(k)["w"]]
        for k in writes:
            s = self._st(k)
            isp = k.startswith("bank")
            need += [(ev, isp) for ev in s["w"] + s["r"]]
        waits = {}
        for ((sem_id, sem, val, src_eng), isp) in need:
            if src_eng == eng and (isp or eng in ("pe", "sp")):
                continue
            if self.seen[eng].get(sem_id, 0) >= val:
                continue
            if waits.get(sem_id, (None, 0))[1] < val:
                waits[sem_id] = (sem, val)
        for sem_id, (sem, val) in waits.items():
            self.seen[eng][sem_id] = val
        return list(waits.values())

    def _commit(self, ev, reads, writes):
        for k in reads:
            if k.startswith("bank"):
                s = self._st(k)
                s["w"] = [ev]
                s["r"] = []
            else:
                self._st(k)["r"].append(ev)
        for k in writes:
            s = self._st(k)
            s["w"] = [ev]
            s["r"] = []

    def op(self, eng, fn, reads=(), writes=()):
        waits = self._deps(eng, reads, writes)
        self.cnt[eng] += 1
        val = self.cnt[eng]
        sem = self.esem[eng]
        self.ops[eng].append((waits, fn, sem, 1))
        ev = ("e_" + eng, sem, val, eng)
        self._commit(ev, reads, writes)
        return ev

    def dma(self, q, fn, semkey, reads=(), writes=()):
        waits = self._deps(q, reads, writes)
        ent = self.dsem.get(semkey)
        if ent is None:
            sem = self.es.enter_context(self.nc.semaphore("d%d" % self.nsem))
            self.nsem += 1
            ent = self.dsem[semkey] = [sem, 0]
        ent[1] += 16
        self.ops[q].append((waits, fn, ent[0], 16))
        ev = ("d_" + str(semkey), ent[0], ent[1], "dma")
        self._commit(ev, reads, writes)
        return ev

    def barrier(self, scratch):
        keys = list(self.state.keys())
        self.op("pool", lambda e: e.memset(scratch, 0.0), reads=keys, writes=keys + ["__bar"])
        for eng in ("pe", "act", "dve", "sp"):
            waits = self._deps(eng, ["__bar"], ())
            self.ops[eng].append((waits, None, None, 0))

    def wait_all(self, eng, keys):
        waits = self._deps(eng, keys, ())
        self.ops[eng].append((waits, None, None, 0))

    def emit(self):
        nc = self.nc
        with nc.Block() as block:
            def run(e):
                def body(h):
                    for (waits, fn, sem, inc) in self.ops[e]:
                        for (s, v) in waits:
                            h.wait_ge(s, v)
                        if fn is not None:
                            fn(h).then_inc(sem, inc)
                return body
            block.tensor(run("pe"))
            block.scalar(run("act"))
            block.vector(run("dve"))
            block.gpsimd(run("pool"))
            block.sync(run("sp"))
        self.es.close()


class Arena:
    def __init__(self, ar, nwords):
        self.ar = ar
        self.n = nwords
        self.top = 0
        self.gen = 0

    def _view(self, v, shape):
        if len(shape) == 2:
            v = v.rearrange("p (a b) -> p a b", a=shape[0])
        elif len(shape) == 3:
            v = v.rearrange("p (a b c) -> p a b c", a=shape[0], b=shape[1])
        return v

    def f32(self, *shape):
        n = int(np.prod(shape))
        a = self.top
        self.top += n
        assert self.top <= self.n, ("arena overflow", self.top, self.n)
        return self._view(self.ar[:, a:a + n], shape)

    def bf16(self, *shape):
        n = int(np.prod(shape))
        w = (n + 1) // 2
        a = self.top
        self.top += w
        assert self.top <= self.n, ("arena overflow", self.top, self.n)
        return self._view(self.ar[:, a:a + w].bitcast(BF16), shape)


def build_nc():
    nc = bass.Bass("TRN2", target_bir_lowering=False)

    def din(name, shape, dt=F32):
        return nc.dram_tensor(name, list(shape), dt, kind="ExternalInput").ap()

    x = din("x", [S, D])
    c_col = din("c_col", [128, 8])
    ada_w = din("ada_w", [D, 6 * D])
    ada_b = din("ada_b", [128, 48])
    adab_bc_d = din("adab_bc", [128, 6 * D])
    n2g_bc_d = din("n2g_bc", [128, D])
    n1g = din("n1g", [128, 8])
    n2g = din("n2g", [128, 8])
    fg_bc_d = din("fg_bc", [128, D])
    w_in = din("w_in", [D, 2048])
    w_out = din("w_out", [D, D])
    conv_w = din("conv_w", [128, 4, 4])
    conv_b = din("conv_b", [128, 4])
    gaw = din("gaw", [8, 64, 64])
    giw = din("giw", [8, 64, 64])
    gab = din("gab", [128, 4])
    gib = din("gib", [128, 4])
    lam = din("lam", [128, 4])
    lng_bc_d = din("lng_bc", [128, 512])
    lnb_bc_d = din("lnb_bc", [128, 512])
    sgu_wT = din("sgu_wT", [128, 8, 128])
    bsrep_d = din("bsrep", [128, 4, 512])
    wr_d = din("wr", [128, 8, 36])
    br_bc_d = din("br_bc", [128, 36])
    zeros_d = din("zeros_blk", [512, D], BF16)
    ew1 = din("ew1", [NE, D, DH])
    ew3 = din("ew3", [NE, D, DH])
    ew2 = din("ew2", [NE, DH, D])
    out = nc.dram_tensor("out", [S, D], F32, kind="ExternalOutput").ap()
    x1_d = nc.dram_tensor("x1_scr", [S, D], F32, kind="ExternalOutput" if DEBUG_STOP else "Internal").ap()
    h2tm_d = nc.dram_tensor("h2tm_scr", [S, D], BF16, kind="Internal").ap()
    NBLK = 47
    BLK = 512
    xs_d = nc.dram_tensor("xs_scr", [NBLK * BLK, D], BF16, kind="Internal").ap()
    ybuf_d = nc.dram_tensor("ybuf_scr", [NBLK * BLK, D], F32, kind="Internal").ap()

    p = Prog(nc)
    NW = 53200
    ar = p.sb("arena", [128, NW], F32)
    A = Arena(ar, NW)
    banks = [p.ps("bank%d" % i, [128, 512], F32) for i in range(8)]
    bank_i = [0]

    def nb():
        i = bank_i[0] % 8
        bank_i[0] += 1
        return banks[i][:], "bank%d" % i

    dcount = [0]

    def ld(dst, src, key, reads=(), q="sp"):
        dcount[0] += 1
        p.dma(q, lambda e: e.dma_start(out=dst, in_=src), "ld_" + key, reads=list(reads), writes=[key])

    ident = A.f32(128)
    ones = A.f32(128)
    identb = A.bf16(128)
    modT = A.f32(48)
    gam1 = A.f32(8)
    gam2 = A.f32(8)
    n1g_t = A.f32(8)
    n2g_t = A.f32(8)
    adab_t = A.f32(48)
    ccol = A.f32(8)
    cond = A.f32(8)
    g2_bc = A.f32(1024)
    fg_bc = A.f32(1024)
    W12 = A.f32(NT, 2)
    br_bc = A.f32(36)
    wr_t = A.f32(8, 36)
    barw = A.f32(1)
    NBLK_ = 47
    idxwi = A.ar[:, A.top:A.top + NBLK_].bitcast(I32); A.top += NBLK_
    desti = A.ar[:, A.top:A.top + 2 * NT].bitcast(I32); A.top += 2 * NT
    idx2i = A.ar[:, A.top:A.top + 4 * NBLK_].bitcast(I32); A.top += 4 * NBLK_
    epsA = A.f32(1)
    const_top = A.top
    OH1 = A.bf16(NT, 32)
    OH2 = A.bf16(NT, 32)
    oh_top = A.top
    gam2_bc = A.f32(1024)
    sh2_bc = A.f32(1024)
    w_inb = A.bf16(8, 2048)
    w_outb = A.bf16(8, 1024)
    bda = A.bf16(4, 128)
    bdi = A.bf16(4, 128)
    wmT = A.bf16(8, 128)
    bsrep = A.f32(4, 512)
    lng_bc = A.f32(512)
    lnb_bc = A.f32(512)
    cw = A.f32(4, 4)
    cb = A.f32(4)
    gab_t = A.f32(4)
    gib_t = A.f32(4)
    lam_t = A.f32(4)
    cneg = A.f32(4)
    cneg2 = A.f32(4)
    lt0 = A.f32(4)
    lt1 = A.f32(4)
    hstate = A.f32(4)
    ngab_t = A.f32(4)
    ngib_t = A.f32(4)
    work_base = A.top

    p.op("pool", lambda e: e.memset(ident, 1.0), writes=["ident"])
    p.op("pool", lambda e: e.affine_select(out=ident, in_=ident, pattern=[[-1, 128]], compare_op=ALU.is_equal,
                                           fill=0.0, base=0, channel_multiplier=1), reads=["ident"], writes=["ident"])
    p.op("pool", lambda e: e.tensor_copy(out=identb, in_=ident), reads=["ident"], writes=["identb"])
    p.op("pool", lambda e: e.memset(ones, 1.0), writes=["ones"])

    ld(ccol, c_col, "ccol")
    ld(adab_t, ada_b, "adab")
    ld(n1g_t, n1g, "n1g")
    ld(n2g_t, n2g, "n2g")
    ld(fg_bc, fg_bc_d, "fg_bc")
    ld(br_bc, br_bc_d, "br_bc")
    ld(wr_t, wr_d, "wr")


    def cut(label):
        if KCUT == label:
            p.barrier(barw)
            p.dma("sp", lambda e: e.dma_start(out=x1_d[0:128, 0:48], in_=modT), "dbgc", reads=["__bar"], writes=["dbgc"])
            p.wait_all("sp", ["dbgc"])
            p.emit()
            return True
        return False

    if cut('c0'):
        return nc
    p.op("act", lambda e: e.activation(out=cond, in_=ccol, func=AF.Silu), reads=["ccol"], writes=["cond"])
    ada_stg = [A.f32(8, 1024) for _ in range(2)]
    diag = A.f32(128)
    g1_bc = A.f32(1024)
    accm = A.f32(1024)
    abb = A.f32(1024)
    rowt = A.f32(1024)
    n2gb = A.f32(1024)
    ada_v = ada_w.rearrange("(kc p) n -> p kc n", p=128)
    ld(n2gb, n2g_bc_d, "n2gb")
    row_dst = {2: (g1_bc, "g1_bc"), 3: (sh2_bc, "sh2_bc"), 5: (g2_bc, "g2_bc")}
    condM = A.f32(4, 128)
    for q in range(4):
        p.op("dve", (lambda q=q: lambda e: e.tensor_scalar(out=condM[:, q, :], in0=ones, scalar1=cond[:, 4 + q:5 + q], scalar2=None, op0=ALU.mult))(),
             reads=["ones", "cond"], writes=["condM%d" % q])
    CMK = ["condM%d" % q for q in range(4)]
    wst = [A.f32(4, 512) for _ in range(2)]
    w_in_v = w_in.rearrange("(kc p) n -> p kc n", p=128)

    def w_in_piece(i):
        pc, hf = i // 2, i % 2
        buf = wst[i % 2]
        bk_ = "wst%d" % (i % 2)
        p.dma("sp", (lambda buf=buf, pc=pc, hf=hf: lambda e: e.dma_start(out=buf, in_=w_in_v[:, hf * 4:(hf + 1) * 4, pc * 512:(pc + 1) * 512]))(),
              "ld_" + bk_, writes=[bk_])
        for q in range(4):
            kc = hf * 4 + q
            p.op("pool", (lambda buf=buf, q=q, kc=kc, pc=pc: lambda e: e.tensor_copy(out=w_inb[:, kc, pc * 512:(pc + 1) * 512], in_=buf[:, q, :]))(),
                 reads=[bk_], writes=["w_inb_%d_%d" % (pc, kc)])

    w_out_v = w_out.rearrange("(kc p) n -> p kc n", p=128)

    def w_out_piece(i):
        hf, nh = i // 2, i % 2
        buf = wst[i % 2]
        bk_ = "wst%d" % (i % 2)
        p.dma("sp", (lambda buf=buf, hf=hf, nh=nh: lambda e: e.dma_start(out=buf, in_=w_out_v[:, hf * 4:(hf + 1) * 4, nh * 512:(nh + 1) * 512]))(),
              "ld_" + bk_, writes=[bk_])
        for q in range(4):
            kc = hf * 4 + q
            p.op("pool", (lambda buf=buf, q=q, kc=kc, nh=nh: lambda e: e.tensor_tensor(
                out=w_outb[:, kc, nh * 512:(nh + 1) * 512], in0=buf[:, q, :], in1=g1_bc[:, nh * 512:(nh + 1) * 512], op=ALU.mult))(),
                reads=[bk_, "g1_bc%d" % nh], writes=["w_outb_%d_%d" % (kc, nh)])

    for s in range(6):
        st = ada_stg[s % 2]
        sk = "adastg%d" % (s % 2)
        for hf in range(2):
            ld(st[:, hf * 4:(hf + 1) * 4, :], ada_v[:, hf * 4:(hf + 1) * 4, s * 1024:(s + 1) * 1024], sk + "_%d" % hf)
        ld(abb, adab_bc_d[:, s * 1024:(s + 1) * 1024], "abb")
        if s < 4:
            w_in_piece(2 * s)
            w_in_piece(2 * s + 1)
        else:
            w_out_piece(2 * (s - 4))
            w_out_piece(2 * (s - 4) + 1)
        p.op("dve", (lambda st=st: lambda e: e.tensor_scalar(out=accm, in0=st[:, 0, :], scalar1=cond[:, 0:1], scalar2=None, op0=ALU.mult))(),
             reads=[sk + "_0", "cond"], writes=["accm"])
        for kc in range(1, 4):
            p.op("dve", (lambda st=st, kc=kc: lambda e: e.scalar_tensor_tensor(out=accm, in0=st[:, kc, :], scalar=cond[:, kc:kc + 1], in1=accm,
                                                                              op0=ALU.mult, op1=ALU.add))(),
                 reads=[sk + "_0", "cond", "accm"], writes=["accm"])
        dst, dkey = row_dst.get(s, (rowt, "rowt"))
        for half in range(2):
            bk, bkk = nb()
            for q in range(4):
                p.op("pe", (lambda bk=bk, half=half, q=q, st=st: lambda e: e.matmul(
                    bk, lhsT=condM[:, q, :], rhs=st[:, 4 + q, half * 512:(half + 1) * 512], start=(q == 0), stop=False))(),
                    reads=CMK + [sk + "_1"], writes=[bkk])
            p.op("pe", (lambda bk=bk, half=half: lambda e: e.matmul(bk, lhsT=ones, rhs=accm[:, half * 512:(half + 1) * 512], start=False, stop=True))(),
                 reads=["ones", "accm"], writes=[bkk])
            p.op("dve", (lambda bk=bk, half=half, dst=dst: lambda e: e.tensor_tensor(
                out=dst[:, half * 512:(half + 1) * 512], in0=bk, in1=abb[:, half * 512:(half + 1) * 512], op=ALU.add))(),
                reads=[bkk, "abb"], writes=[dkey + "%d" % half])
        if s in (0, 1, 3, 4):
            for kc in range(8):
                p.op("dve", (lambda dst=dst, kc=kc: lambda e: e.tensor_tensor(out=diag, in0=dst[:, kc * 128:(kc + 1) * 128], in1=ident, op=ALU.mult))(),
                     reads=[dkey + "0", dkey + "1", "ident"], writes=["diag"])
                p.op("dve", (lambda s=s, kc=kc: lambda e: e.tensor_reduce(out=modT[:, s * 8 + kc:s * 8 + kc + 1], in_=diag, axis=AX.X, op=ALU.add))(),
                     reads=["diag"], writes=["modT_%d_%d" % (s, kc)])
        if s == 4:
            p.op("dve", lambda e: e.scalar_tensor_tensor(out=gam2_bc, in0=rowt, scalar=1.0, in1=n2gb, op0=ALU.add, op1=ALU.mult),
                 reads=["rowt0", "rowt1", "n2gb"], writes=["gam2_bc0", "gam2_bc1"])
    MODK = ["modT_%d_%d" % (s, kc) for s in (0, 1, 3, 4) for kc in range(8)]
    p.op("dve", lambda e: e.tensor_copy(out=modT[:, 16:17], in_=modT[:, 0:1]), reads=MODK, writes=["modT"])
    p.op("dve", lambda e: e.scalar_tensor_tensor(out=gam1, in0=modT[:, 8:16], scalar=1.0, in1=n1g_t,
                                                 op0=ALU.add, op1=ALU.mult), reads=["modT", "n1g"], writes=["gam1"])
    p.op("dve", lambda e: e.scalar_tensor_tensor(out=gam2, in0=modT[:, 32:40], scalar=1.0, in1=n2g_t,
                                                 op0=ALU.add, op1=ALU.mult), reads=["modT", "n2g"], writes=["gam2"])
    if cut('c1'):
        return nc
    sh1 = modT[:, 0:8]
    sh2 = modT[:, 24:32]

    if cut('c2'):
        return nc
    stg = [ada_stg[0], ada_stg[1]]

    ld(bsrep, bsrep_d, "bsrep")
    ld(lng_bc, lng_bc_d, "lng_bc")
    ld(lnb_bc, lnb_bc_d, "lnb_bc")
    ld(cw, conv_w, "cw")
    ld(cb, conv_b, "cb")
    ld(gab_t, gab, "gab")
    ld(gib_t, gib, "gib")
    ld(lam_t, lam, "lam")

    W_INB_KEYS = ["w_inb_%d_%d" % (pc, kc) for pc in range(4) for kc in range(8)]
    W_OUTB_KEYS = ["w_outb_%d_%d" % (kc, nh) for kc in range(8) for nh in range(2)]
    if cut('c3'):
        return nc
    st1 = stg[1]
    bdst = st1[:, 0, :].rearrange("p (g c m) -> p g c m", g=2, c=4)
    p.op("pool", lambda e: e.memset(st1[:, 0, :], 0.0), reads=[], writes=["adastg1_0", "adastg1_1"])
    for gi, gw in enumerate((gaw, giw)):
        for cc in range(4):
            for hh in range(2):
                p.dma("sp", (lambda gi=gi, gw=gw, cc=cc, hh=hh: lambda e: e.dma_start(
                    out=bdst[hh * 64:(hh + 1) * 64, gi, cc, hh * 64:(hh + 1) * 64], in_=gw[2 * cc + hh]))(),
                    "ld_bd%d_%d_%d" % (gi, cc, hh), reads=["adastg1_0"], writes=["bdst%d_%d_%d" % (gi, cc, hh)])
    BDK = ["bdst%d_%d_%d" % (gi, cc, hh) for gi in range(2) for cc in range(4) for hh in range(2)]
    p.op("pool", lambda e: e.tensor_copy(out=bda, in_=bdst[:, 0, :, :]), reads=BDK, writes=["bda"])
    p.op("pool", lambda e: e.tensor_copy(out=bdi, in_=bdst[:, 1, :, :]), reads=BDK, writes=["bdi"])
    if cut('c4'):
        return nc
    wst = st1[:, 1, :].rearrange("p (g i) -> p g i", g=8)
    p.dma("sp", lambda e: e.dma_start(out=wst, in_=sgu_wT), "ld_wst", reads=["adastg1_1"], writes=["wst"])
    p.op("pool", lambda e: e.memset(wst[64:128, :, 0:64], 0.0), reads=["wst"], writes=["wst"])
    p.op("pool", lambda e: e.tensor_copy(out=wmT, in_=wst), reads=["wst"], writes=["wmT"])
    if cut('c5'):
        return nc
    p.op("act", lambda e: e.activation(out=lt0, in_=lam_t, func=AF.Exp, scale=-1.0), reads=["lam"], writes=["lt0"])
    p.op("dve", lambda e: e.tensor_scalar(out=lt1, in0=lt0, scalar1=-0.25, scalar2=1.0 / 3.0, op0=ALU.mult, op1=ALU.add),
         reads=["lt0"], writes=["lt1"])
    p.op("dve", lambda e: e.tensor_tensor(out=lt1, in0=lt1, in1=lt0, op=ALU.mult), reads=["lt1", "lt0"], writes=["lt1"])
    p.op("dve", lambda e: e.tensor_scalar(out=lt1, in0=lt1, scalar1=-0.5, scalar2=None, op0=ALU.add), reads=["lt1"], writes=["lt1"])
    p.op("dve", lambda e: e.tensor_tensor(out=lt1, in0=lt1, in1=lt0, op=ALU.mult), reads=["lt1", "lt0"], writes=["lt1"])
    p.op("dve", lambda e: e.tensor_scalar(out=lt1, in0=lt1, scalar1=1.0, scalar2=None, op0=ALU.add), reads=["lt1"], writes=["lt1"])
    p.op("dve", lambda e: e.tensor_tensor(out=lt1, in0=lt1, in1=lt0, op=ALU.mult), reads=["lt1", "lt0"], writes=["lt1"])
    p.op("dve", lambda e: e.tensor_scalar(out=cneg, in0=lt1, scalar1=-8.0, scalar2=None, op0=ALU.mult), reads=["lt1"], writes=["cneg"])
    p.op("dve", lambda e: e.tensor_scalar(out=cneg2, in0=lt1, scalar1=-16.0, scalar2=None, op0=ALU.mult), reads=["lt1"], writes=["cneg2"])
    p.op("dve", lambda e: e.memset(hstate, 0.0), writes=["hstate%d" % cc for cc in range(4)])
    p.op("dve", lambda e: e.tensor_scalar(out=ngab_t, in0=gab_t, scalar1=-1.0, scalar2=None, op0=ALU.mult), reads=["gab"], writes=["ngab"])
    p.op("dve", lambda e: e.tensor_scalar(out=ngib_t, in0=gib_t, scalar1=-1.0, scalar2=None, op0=ALU.mult), reads=["gib"], writes=["ngib"])

    if DEBUG_STOP == "setup":
        p.dma("sp", lambda e: e.dma_start(out=x1_d[0:128, 0:48], in_=modT), "dbg0", reads=["modT"], writes=["dbg0"])
        p.dma("sp", lambda e: e.dma_start(out=x1_d[128:256, :], in_=g2_bc), "dbg1", reads=["g2_bc0", "g2_bc1"], writes=["dbg1"])
        p.dma("sp", lambda e: e.dma_start(out=x1_d[256:384, 0:4], in_=cneg), "dbg2", reads=["cneg"], writes=["dbg2"])
        p.op("pool", lambda e: e.tensor_copy(out=g1_bc, in_=w_outb[:, 0, :]), reads=W_OUTB_KEYS + ["g1_bc0", "g1_bc1"], writes=["g1x"])
        p.dma("sp", lambda e: e.dma_start(out=x1_d[384:512, :], in_=g1_bc), "dbg3", reads=["g1x"], writes=["dbg3"])
        p.barrier(barw)
        p.wait_all("sp", ["dbg0", "dbg1", "dbg2", "dbg3"])
        p.emit()
        return nc
    p.barrier(barw)
    A.top = work_base
    xa = [A.f32(1024) for _ in range(2)]
    xres = [A.f32(1024) for _ in range(2)]
    hT = A.bf16(8, 512)
    xr = A.f32(4, 515)
    grg = A.f32(4, 512)
    ug = A.f32(4, 512)
    vnb = A.bf16(4, 512)
    ysT = A.bf16(8, 512)
    xc = [A.f32(512) for _ in range(2)]
    xcb = [A.bf16(512) for _ in range(2)]
    rg = [A.f32(512) for _ in range(2)]
    ig = [A.f32(512) for _ in range(2)]
    av = [A.f32(512) for _ in range(2)]
    a2 = [A.f32(512) for _ in range(2)]
    hs = A.f32(512)
    sgt = A.f32(512)
    xn = A.bf16(1024)
    junk = xn
    x1t = [A.f32(1024) for _ in range(2)]
    xn2 = [A.f32(1024) for _ in range(2)]
    h2f = [A.f32(8, 128) for _ in range(2)]
    h2tm = [A.bf16(1024) for _ in range(2)]
    sm = A.f32(128)
    sm2v = sm
    lgt = A.f32(36)
    elm = A.f32(32)
    mx8 = A.f32(8)
    ssall = A.f32(96)
    print("phase1 arena top", A.top, "of", NW)

    p.op("pool", lambda e: e.memset(xr[:, :, 0:3], 0.0), writes=["xr%d" % cc for cc in range(4)])

    def rms_rstd(src, skey, dst_col, tag, slot=0, acc=0):
        ss = ssall[:, acc:acc + 1]
        sq = sm[:, 11 + 2 * slot:12 + 2 * slot]
        ssk = "ssacc%d" % acc
        p.op("act", lambda e: e.activation(out=junk, in_=src, func=AF.Square, accum_out=ss),
             reads=list(skey) + ["ssall"], writes=["xn", ssk])
        p.op("act", lambda e: e.activation(out=sq, in_=ss, func=AF.Ln, bias=sm[:, 8:9], scale=1.0 / D),
             reads=[ssk, "epsc"], writes=["sq%d" % slot])
        p.op("act", lambda e: e.activation(out=dst_col, in_=sq, func=AF.Exp, scale=-0.5), reads=["sq%d" % slot], writes=[tag])

    p.op("pool", lambda e: e.memset(ssall, 0.0), writes=["ssall"])
    p.op("pool", lambda e: e.memset(sm[:, 8:9], EPS), writes=["epsc"])
    p.op("pool", lambda e: e.memset(sm[:, 9:10], 1.0), writes=["onec"])

    x_v = x.rearrange("(t p) d -> p t d", p=128)
    out_v = out.rearrange("(t p) d -> p t d", p=128)
    x1_v = x1_d.rearrange("(t p) d -> p t d", p=128)
    h2tm_v = h2tm_d.rearrange("(t p) d -> p t d", p=128)

    def x_load(tq_):
        ld(xa[tq_ % 2], x_v[:, tq_, :], "xa%d" % (tq_ % 2))

    def norm1_a(c, j):
        tq_ = 4 * c + j
        src = xa[tq_ % 2]
        xak = "xa%d" % (tq_ % 2)
        rms_rstd(src, [xak], sm[:, 2:3], "rstd1", slot=0, acc=tq_)
        p.op("dve", (lambda src=src: lambda e: e.tensor_scalar(out=xn, in0=src, scalar1=sm[:, 2:3], scalar2=None,
                                                                op0=ALU.mult))(),
             reads=[xak, "rstd1"], writes=["xn"])
        tb, tbk = nb()
        trp = tb.bitcast(BF16)
        for kc in range(8):
            p.op("pe", (lambda kc=kc, trp=trp: lambda e: e.transpose(out=trp[:, kc * 128:(kc + 1) * 128],
                                                            in_=xn[:, kc * 128:(kc + 1) * 128], identity=identb))(),
                 reads=["xn", "identb"], writes=[tbk])
        return (trp, tbk)

    def norm1_b(c, j, tt):
        trp, tbk = tt
        for kc in range(8):
            dst = hT[:, kc, j * 128:(j + 1) * 128]
            if j % 2 == 0:
                p.op("dve", (lambda kc=kc, dst=dst, trp=trp: lambda e: e.tensor_scalar(
                    out=dst, in0=trp[:, kc * 128:(kc + 1) * 128], scalar1=gam1[:, kc:kc + 1],
                    scalar2=sh1[:, kc:kc + 1], op0=ALU.mult, op1=ALU.add))(),
                    reads=[tbk, "gam1", "modT"], writes=["hT_%d_%d" % (j, kc)])
            else:
                p.op("act", (lambda kc=kc, dst=dst, trp=trp: lambda e: e.activation(
                    out=dst, in_=trp[:, kc * 128:(kc + 1) * 128], func=AF.Identity,
                    bias=sh1[:, kc:kc + 1], scale=gam1[:, kc:kc + 1]))(),
                    reads=[tbk, "gam1", "modT"], writes=["hT_%d_%d" % (j, kc)])

    def zero_fill(b):
        p.dma("act", (lambda b=b: lambda e: e.dma_start(out=xs_d[b * BLK:(b + 1) * BLK, :], in_=zeros_d))(), "xszero",
              writes=["xszero_%d" % b])
    XSZK = ["xszero_%d" % b for b in range(NBLK)]

    x_load(0)
    for j in range(4):
        if j + 1 < 4:
            x_load(j + 1)
        norm1_b(0, j, norm1_a(0, j))


    for c in range(8):
        HTK = ["hT_%d_%d" % (j, kc) for j in range(4) for kc in range(8)]
        if c == 0 and cut('p1'):
            return nc
        for b in range(6 * c, min(6 * c + 6, NBLK)):
            zero_fill(b)
        for fc in range(12):
            bk, bkk = nb()
            for kc in range(8):
                p.op("pe", (lambda bk=bk, fc=fc, kc=kc: lambda e: e.matmul(
                    bk, lhsT=w_inb[:, kc, fc * 128:(fc + 1) * 128], rhs=hT[:, kc, :], start=(kc == 0), stop=(kc == 7)))(),
                    reads=HTK + W_INB_KEYS, writes=[bkk])
            if fc < 4:
                p.op("act", (lambda bk=bk, fc=fc: lambda e: e.copy(out=xr[:, fc, 3:515], in_=bk))(),
                     reads=[bkk], writes=["xr%d" % fc])
            elif fc < 8:
                p.op("act", (lambda bk=bk, fc=fc: lambda e: e.activation(out=grg[:, fc - 4, :], in_=bk, func=AF.Gelu_apprx_tanh))(),
                     reads=[bkk], writes=["grg%d" % (fc - 4)])
            else:
                p.op("act", (lambda bk=bk, fc=fc: lambda e: e.activation(out=ug[:, fc - 8, :], in_=bk, func=AF.Gelu_apprx_tanh))(),
                     reads=[bkk], writes=["ug%d" % (fc - 8)])
        if c == 0 and cut('p2'):
            return nc
        vgs = [(xc[0], "xc0"), (xc[1], "xc1"), (hs, "hs"), (sgt, "sgtV")]
        for j in range(4):
            bk, bkk = nb()
            vgj, vgk = vgs[j]
            for kc in range(8):
                p.op("pe", (lambda bk=bk, j=j, kc=kc: lambda e: e.matmul(
                    bk, lhsT=hT[:, kc, j * 128:(j + 1) * 128], rhs=w_inb[:, kc, 1536:2048], start=(kc == 0), stop=(kc == 7)))(),
                    reads=HTK + W_INB_KEYS, writes=[bkk])
            wk = [vgk] if vgk != "sgtV" else ["sgtA", "sgtB", "sgtV"]
            p.op("act", (lambda bk=bk, vgj=vgj: lambda e: e.activation(out=vgj, in_=bk, func=AF.Gelu_apprx_tanh))(),
                 reads=[bkk], writes=wk)
        vsl = []
        for j in range(4):
            o = 64 + 16 * j
            vsl.append((sm2v[:, o:o + 6], sm2v[:, o + 6:o + 8], sm2v[:, o + 8:o + 9], sm2v[:, o + 9:o + 10]))
        for j in range(4):
            vgj, vgk = vgs[j]
            st6, mv, lnv, rsv = vsl[j]
            p.op("dve", (lambda vgj=vgj, st6=st6: lambda e: e.bn_stats(out=st6, in_=vgj))(), reads=[vgk], writes=["bnst%d" % j])
            p.op("dve", (lambda st6=st6, mv=mv: lambda e: e.bn_aggr(out=mv, in_=st6))(), reads=["bnst%d" % j], writes=["mv%d" % j])
        for j in range(4):
            st6, mv, lnv, rsv = vsl[j]
            p.op("act", (lambda mv=mv, lnv=lnv: lambda e: e.activation(out=lnv, in_=mv[:, 1:2], func=AF.Ln, bias=sm[:, 8:9], scale=1.0))(),
                 reads=["mv%d" % j, "epsc"], writes=["vsq%d" % j])
            p.op("act", (lambda lnv=lnv, rsv=rsv: lambda e: e.activation(out=rsv, in_=lnv, func=AF.Exp, scale=-0.5))(),
                 reads=["vsq%d" % j], writes=["vrstd%d" % j])
        for j in range(4):
            vgj, vgk = vgs[j]
            st6, mv, lnv, rsv = vsl[j]
            p.op("dve", (lambda vgj=vgj, mv=mv, rsv=rsv: lambda e: e.tensor_scalar(out=vgj, in0=vgj, scalar1=mv[:, 0:1], scalar2=rsv,
                                                                                 op0=ALU.subtract, op1=ALU.mult))(),
                 reads=[vgk, "mv%d" % j, "vrstd%d" % j], writes=[vgk])
            p.op("dve", (lambda vgj=vgj: lambda e: e.tensor_tensor(out=vgj, in0=vgj, in1=lng_bc, op=ALU.mult))(), reads=[vgk, "lng_bc"], writes=[vgk])
            p.op("pool", (lambda j=j, vgj=vgj: lambda e: e.tensor_tensor(out=vnb[:, j, :], in0=vgj, in1=lnb_bc, op=ALU.add))(),
                 reads=[vgk, "lnb_bc"], writes=["vnb%d" % j])
        if c == 0 and cut('p3'):
            return nc
        for cc in range(4):
            bkA, bkAk = nb()
            bkB, bkBk = nb()
            for j in range(4):
                p.op("pe", (lambda bkA=bkA, j=j, cc=cc: lambda e: e.matmul(
                    bkA[:, j * 128:(j + 1) * 128], lhsT=vnb[:, j, cc * 128:(cc + 1) * 128], rhs=wmT[:, 2 * cc, :],
                    start=True, stop=True))(), reads=["vnb%d" % j, "wmT"], writes=[bkAk])
                p.op("pe", (lambda bkB=bkB, j=j, cc=cc: lambda e: e.matmul(
                    bkB[:, j * 128:(j + 1) * 128], lhsT=vnb[:, j, cc * 128:(cc + 1) * 128], rhs=wmT[:, 2 * cc + 1, :],
                    start=True, stop=True))(), reads=["vnb%d" % j, "wmT"], writes=[bkBk])
            p.op("dve", (lambda bkA=bkA, cc=cc: lambda e: e.tensor_tensor(
                out=sgt[0:64, :], in0=bkA[0:64, :], in1=bsrep[0:64, cc, :], op=ALU.add))(),
                reads=[bkAk, "bsrep"], writes=["sgtA", "sgtV"])
            p.op("dve", (lambda bkB=bkB, cc=cc: lambda e: e.tensor_tensor(
                out=sgt[64:128, :], in0=bkB[64:128, :], in1=bsrep[64:128, cc, :], op=ALU.add))(),
                reads=[bkBk, "bsrep"], writes=["sgtB", "sgtV"])
            p.op("pool", (lambda cc=cc: lambda e: e.tensor_tensor(out=ysT[:, 4 + cc, :], in0=sgt, in1=ug[:, cc, :], op=ALU.mult))(),
                 reads=["sgtA", "sgtB", "ug%d" % cc], writes=["ysT%d" % (4 + cc)])
        if c == 0 and cut('p4'):
            return nc
        def lru_front(cc):
            q = cc % 2
            xrk = "xr%d" % cc
            xcq, xcbq = xc[q], xcb[q]
            p.op("dve", (lambda cc=cc, xcq=xcq: lambda e: e.tensor_scalar(
                out=xcq, in0=xr[:, cc, 0:512], scalar1=cw[:, cc, 0:1], scalar2=cb[:, cc:cc + 1], op0=ALU.mult, op1=ALU.add))(),
                reads=[xrk, "cw", "cb"], writes=["xc%d" % q])
            for k in range(1, 4):
                p.op("dve", (lambda cc=cc, k=k, xcq=xcq: lambda e: e.scalar_tensor_tensor(
                    out=xcq, in0=xr[:, cc, k:k + 512], scalar=cw[:, cc, k:k + 1], in1=xcq, op0=ALU.mult, op1=ALU.add))(),
                    reads=[xrk, "cw", "xc%d" % q], writes=["xc%d" % q])
            p.op("pool", (lambda cc=cc: lambda e: e.tensor_copy(out=xr[:, cc, 0:3], in_=xr[:, cc, 512:515]))(),
                 reads=[xrk], writes=[xrk])
            p.op("dve", (lambda xcq=xcq, xcbq=xcbq: lambda e: e.tensor_copy(out=xcbq, in_=xcq))(), reads=["xc%d" % q], writes=["xcb%d" % q])
            bka, bkak = nb()
            bki, bkik = nb()
            p.op("pe", (lambda bka=bka, cc=cc, xcbq=xcbq: lambda e: e.matmul(bka, lhsT=bda[:, cc, :], rhs=xcbq, start=True, stop=True))(),
                 reads=["bda", "xcb%d" % q], writes=[bkak])
            p.op("pe", (lambda bki=bki, cc=cc, xcbq=xcbq: lambda e: e.matmul(bki, lhsT=bdi[:, cc, :], rhs=xcbq, start=True, stop=True))(),
                 reads=["bdi", "xcb%d" % q], writes=[bkik])
            return (bka, bkak, bki, bkik)

        def lru_act(cc, bks):
            q = cc % 2
            bka, bkak, bki, bkik = bks
            rgq, igq, avq, a2q = rg[q], ig[q], av[q], a2[q]
            p.op("act", (lambda bka=bka, cc=cc, rgq=rgq: lambda e: e.activation(out=rgq, in_=bka, func=AF.Exp, bias=ngab_t[:, cc:cc + 1], scale=-1.0))(),
                 reads=[bkak, "ngab"], writes=["rg%d" % q])
            p.op("act", (lambda bki=bki, cc=cc, igq=igq: lambda e: e.activation(out=igq, in_=bki, func=AF.Exp, bias=ngib_t[:, cc:cc + 1], scale=-1.0))(),
                 reads=[bkik, "ngib"], writes=["ig%d" % q])
            for (buf, k) in ((rgq, "rg%d" % q), (igq, "ig%d" % q)):
                p.op("act", (lambda buf=buf: lambda e: e.activation(out=buf, in_=buf, func=AF.Ln, bias=sm[:, 9:10], scale=1.0))(),
                     reads=[k, "onec"], writes=[k])
                p.op("act", (lambda buf=buf: lambda e: e.activation(out=buf, in_=buf, func=AF.Exp, scale=-1.0))(), reads=[k], writes=[k])
            p.op("act", (lambda cc=cc, rgq=rgq, avq=avq: lambda e: e.activation(out=avq, in_=rgq, func=AF.Exp, scale=cneg[:, cc:cc + 1]))(),
                 reads=["rg%d" % q, "cneg"], writes=["av%d" % q])
            p.op("act", (lambda cc=cc, rgq=rgq, a2q=a2q: lambda e: e.activation(out=a2q, in_=rgq, func=AF.Exp, scale=cneg2[:, cc:cc + 1]))(),
                 reads=["rg%d" % q, "cneg2"], writes=["a2%d" % q])
            p.op("act", (lambda a2q=a2q: lambda e: e.activation(out=a2q, in_=a2q, func=AF.Ln, bias=sm[:, 9:10], scale=-1.0))(),
                 reads=["a2%d" % q, "onec"], writes=["a2%d" % q])
            p.op("act", (lambda a2q=a2q: lambda e: e.activation(out=a2q, in_=a2q, func=AF.Exp, scale=0.5))(), reads=["a2%d" % q], writes=["a2%d" % q])

        def lru_back(cc):
            q = cc % 2
            igq, xcq, a2q, avq = ig[q], xc[q], a2[q], av[q]
            p.op("pool", (lambda igq=igq, xcq=xcq: lambda e: e.tensor_tensor(out=igq, in0=igq, in1=xcq, op=ALU.mult))(),
                 reads=["ig%d" % q, "xc%d" % q], writes=["ig%d" % q])
            p.op("dve", (lambda igq=igq, a2q=a2q: lambda e: e.tensor_tensor(out=igq, in0=igq, in1=a2q, op=ALU.mult))(),
                 reads=["ig%d" % q, "a2%d" % q], writes=["ig%d" % q])
            p.op("dve", (lambda cc=cc, avq=avq, igq=igq: lambda e: e.tensor_tensor_scan(
                out=hs, data0=avq, data1=igq, initial=hstate[:, cc:cc + 1], op0=ALU.mult, op1=ALU.add))(),
                reads=["av%d" % q, "ig%d" % q, "hstate%d" % cc], writes=["hs"])
            p.op("dve", (lambda cc=cc: lambda e: e.tensor_copy(out=hstate[:, cc:cc + 1], in_=hs[:, 511:512]))(),
                 reads=["hs"], writes=["hstate%d" % cc])
            p.op("pool", (lambda cc=cc: lambda e: e.tensor_tensor(out=ysT[:, cc, :], in0=hs, in1=grg[:, cc, :], op=ALU.mult))(),
                 reads=["hs", "grg%d" % cc], writes=["ysT%d" % cc])

        bks_ = {0: lru_front(0)}
        if c + 1 < 8:
            x_load(4 * (c + 1))
        for cc in range(4):
            if cc + 1 < 4:
                bks_[cc + 1] = lru_front(cc + 1)
            if c + 1 < 8:
                if cc + 1 < 4:
                    x_load(4 * (c + 1) + cc + 1)
                tt_ = norm1_a(c + 1, cc)
            lru_act(cc, bks_[cc])
            if c + 1 < 8:
                norm1_b(c + 1, cc, tt_)
            lru_back(cc)
        YSK = ["ysT%d" % k for k in range(8)]
        if c == 0 and cut('p5'):
            return nc
        gmax = sm[:, 32:33]
        ngmax = sm[:, 33:34]
        pg = sm[:, 35:36]
        goh = sm[:, 36:40]
        pen = sm[:, 40:44]
        gex = sm[:, 44:48]
        dd = sm[:, 48:49]
        w1c = sm[:, 49:50]
        w2c = sm[:, 50:51]
        ELK = ["elm%d" % g for g in range(4)]

        def mix_m1(j):
            t = 4 * c + j
            q = t % 2
            x1b = x1t[q]
            x1k = "x1t%d" % q
            xrs = xres[q]
            xrsk = "xres%d" % q
            xn2q = xn2[q]
            xn2k = "xn2_%d" % q
            ld(xrs, x_v[:, t, :], xrsk)
            for nh in range(2):
                bk, bkk = nb()
                for kc in range(8):
                    p.op("pe", (lambda bk=bk, j=j, kc=kc, nh=nh: lambda e: e.matmul(
                        bk, lhsT=ysT[:, kc, j * 128:(j + 1) * 128], rhs=w_outb[:, kc, nh * 512:(nh + 1) * 512],
                        start=(kc == 0), stop=(kc == 7)))(), reads=YSK + W_OUTB_KEYS, writes=[bkk])
                p.op("dve", (lambda bk=bk, nh=nh, x1b=x1b, xrs=xrs: lambda e: e.tensor_tensor(
                    out=x1b[:, nh * 512:(nh + 1) * 512], in0=bk, in1=xrs[:, nh * 512:(nh + 1) * 512], op=ALU.add))(),
                    reads=[bkk, xrsk], writes=[x1k + "_%d" % nh])
            x1keys = [x1k + "_0", x1k + "_1"]
            p.dma("sp", (lambda x1b=x1b, t=t: lambda e: e.dma_start(out=x1_v[:, t, :], in_=x1b))(),
                  "st_x1_%d" % q, reads=x1keys, writes=["x1d_%d" % t])
            rcol = sm[:, 3 + q:4 + q]
            rtag = "rstd2_%d" % q
            rms_rstd(x1b, x1keys, rcol, rtag, slot=1 + q, acc=32 + t)
            p.op("dve", (lambda x1b=x1b, xn2q=xn2q, rcol=rcol: lambda e: e.tensor_scalar(out=xn2q, in0=x1b, scalar1=rcol, scalar2=None,
                                                                                      op0=ALU.mult))(),
                 reads=x1keys + [rtag], writes=[xn2k])

        def mix_m2(j):
            t = 4 * c + j
            q = t % 2
            xn2q = xn2[q]
            xn2k = "xn2_%d" % q
            h2fq = h2f[q]
            tfs = [nb(), nb()]
            for kc in range(8):
                tf, tfk = tfs[kc // 4]
                qq = kc % 4
                p.op("pe", (lambda kc=kc, tf=tf, qq=qq, xn2q=xn2q: lambda e: e.transpose(out=tf[:, qq * 128:(qq + 1) * 128],
                                                                                      in_=xn2q[:, kc * 128:(kc + 1) * 128], identity=ident))(),
                     reads=[xn2k, "ident"], writes=[tfk])
            for kc in range(8):
                tf, tfk = tfs[kc // 4]
                qq = kc % 4
                if kc // 4 == 0:
                    p.op("dve", (lambda kc=kc, tf=tf, qq=qq, h2fq=h2fq: lambda e: e.tensor_scalar(
                        out=h2fq[:, kc, :], in0=tf[:, qq * 128:(qq + 1) * 128], scalar1=gam2[:, kc:kc + 1],
                        scalar2=sh2[:, kc:kc + 1], op0=ALU.mult, op1=ALU.add))(),
                        reads=[tfk, "gam2", "modT"], writes=["h2f%d_%d" % (q, kc)])
                else:
                    p.op("act", (lambda kc=kc, tf=tf, qq=qq, h2fq=h2fq: lambda e: e.activation(
                        out=h2fq[:, kc, :], in_=tf[:, qq * 128:(qq + 1) * 128], func=AF.Identity,
                        bias=sh2[:, kc:kc + 1], scale=gam2[:, kc:kc + 1]))(),
                        reads=[tfk, "gam2", "modT"], writes=["h2f%d_%d" % (q, kc)])
            H2FK = ["h2f%d_%d" % (q, kc) for kc in range(8)]
            hb = h2tm[q]
            hbk = "h2tm%d" % q
            p.op("dve", (lambda xn2q=xn2q: lambda e: e.tensor_tensor(out=xn2q, in0=xn2q, in1=gam2_bc, op=ALU.mult))(),
                 reads=[xn2k, "gam2_bc0", "gam2_bc1"], writes=[xn2k])
            p.op("pool", (lambda hb=hb, xn2q=xn2q: lambda e: e.tensor_tensor(out=hb, in0=xn2q, in1=sh2_bc, op=ALU.add))(),
                 reads=[xn2k, "sh2_bc0", "sh2_bc1"], writes=[hbk])
            p.dma("sp", (lambda hb=hb, t=t: lambda e: e.dma_start(out=h2tm_v[:, t, :], in_=hb))(),
                  "st_h2tm%d" % q, reads=[hbk], writes=["h2tmd_%d" % t])
            bk, bkk = nb()
            for kc in range(8):
                p.op("pe", (lambda bk=bk, kc=kc, h2fq=h2fq: lambda e: e.matmul(bk[:, 0:36], lhsT=h2fq[:, kc, :], rhs=wr_t[:, kc, :],
                                                                              start=(kc == 0), stop=(kc == 7)))(),
                     reads=H2FK + ["wr"], writes=[bkk])
            return (bk, bkk)

        def router_a(j, rb):
            bk, bkk = rb
            p.op("dve", (lambda bk=bk: lambda e: e.tensor_tensor(out=lgt, in0=bk[:, 0:36], in1=br_bc, op=ALU.add))(),
                 reads=[bkk, "br_bc"], writes=["lgt"])
            p.op("dve", lambda e: e.tensor_reduce(out=gmax, in_=lgt[:, 0:4], axis=AX.X, op=ALU.max), reads=["lgt"], writes=["gmax"])
            p.op("dve", lambda e: e.tensor_scalar(out=ngmax, in0=gmax, scalar1=-1.0, scalar2=None, op0=ALU.mult),
                 reads=["gmax"], writes=["ngmax"])
            gsum = ssall[:, 64 + 4 * c + j:65 + 4 * c + j]
            p.op("act", (lambda gsum=gsum: lambda e: e.activation(out=gex, in_=lgt[:, 0:4], func=AF.Exp, bias=ngmax, scale=1.0, accum_out=gsum))(),
                 reads=["lgt", "ngmax", "ssall"], writes=["gex", "gsum"])
            p.op("dve", lambda e: e.tensor_scalar(out=goh, in0=lgt[:, 0:4], scalar1=gmax, scalar2=-1.0, op0=ALU.is_ge, op1=ALU.add),
                 reads=["lgt", "gmax"], writes=["goh"])
            p.op("dve", lambda e: e.tensor_scalar(out=pen, in0=goh, scalar1=1e30, scalar2=None, op0=ALU.mult),
                 reads=["goh"], writes=["pen"])
            for g in range(4):
                p.op("dve", (lambda g=g: lambda e: e.tensor_scalar(out=elm[:, g * 8:(g + 1) * 8], in0=lgt[:, 4 + g * 8:4 + (g + 1) * 8],
                                                                   scalar1=pen[:, g:g + 1], scalar2=None, op0=ALU.add))(),
                     reads=["lgt", "pen"], writes=["elm%d" % g])
            p.op("dve", lambda e: e.max(out=mx8, in_=elm), reads=ELK, writes=["mx8"])
            p.op("dve", lambda e: e.tensor_tensor(out=dd, in0=mx8[:, 0:1], in1=mx8[:, 1:2], op=ALU.subtract), reads=["mx8"], writes=["dd"])
            p.op("act", lambda e: e.activation(out=w1c, in_=dd, func=AF.Exp, scale=-1.0), reads=["dd"], writes=["w1c"])

        def router_b(j):
            t = 4 * c + j
            gsum = ssall[:, 64 + t:65 + t]
            p.op("dve", (lambda gsum=gsum: lambda e: e.reciprocal(out=pg, in_=gsum))(), reads=["gsum"], writes=["pg"])
            p.op("dve", lambda e: e.tensor_scalar(out=w1c, in0=w1c, scalar1=1.0, scalar2=None, op0=ALU.add), reads=["w1c"], writes=["w1c"])
            p.op("dve", lambda e: e.reciprocal(out=w1c, in_=w1c), reads=["w1c"], writes=["w1c"])
            p.op("dve", lambda e: e.tensor_tensor(out=w1c, in0=w1c, in1=pg, op=ALU.mult), reads=["w1c", "pg"], writes=["w1c"])
            p.op("dve", lambda e: e.tensor_tensor(out=w2c, in0=pg, in1=w1c, op=ALU.subtract), reads=["w1c", "pg"], writes=["w2c"])
            p.op("dve", (lambda t=t: lambda e: e.tensor_scalar(out=OH1[:, t, :], in0=elm, scalar1=mx8[:, 0:1], scalar2=None, op0=ALU.is_equal))(),
                 reads=ELK + ["mx8"], writes=["OH1_%d" % t])
            p.op("dve", (lambda t=t: lambda e: e.tensor_scalar(out=OH2[:, t, :], in0=elm, scalar1=mx8[:, 1:2], scalar2=None, op0=ALU.is_equal))(),
                 reads=ELK + ["mx8"], writes=["OH2_%d" % t])
            p.op("dve", (lambda t=t: lambda e: e.tensor_copy(out=W12[:, t, 0:1], in_=w1c))(), reads=["w1c"], writes=["W1_%d" % t])
            p.op("dve", (lambda t=t: lambda e: e.tensor_copy(out=W12[:, t, 1:2], in_=w2c))(), reads=["w2c"], writes=["W2_%d" % t])

        mix_m1(0)
        for j in range(4):
            if j + 1 < 4:
                mix_m1(j + 1)
            if j >= 1:
                router_b(j - 1)
            rb_ = mix_m2(j)
            router_a(j, rb_)
        router_b(3)
        if c == 0 and cut('p6'):
            return nc

    if DEBUG_STOP == "phase1":
        p.wait_all("sp", ["x1d_%d" % t for t in range(NT)] + ["h2tmd_%d" % t for t in range(NT)])
        p.emit()
        return nc

    p.barrier(barw)
    A.top = oh_top
    Ltri = A.bf16(128)
    onesb = A.bf16(128)
    OS = A.bf16(32)
    base = A.f32(32)
    Rall = A.f32(NT, 32)
    nblk = A.f32(32)
    padded = A.f32(32)
    pend = A.f32(32)
    pstart = A.f32(32)
    ones32 = A.f32(32)
    cmpt = A.f32(32)
    tmpd = A.f32(32)
    prod = A.f32(32)
    bef = A.f32(NBLK)
    idxwf = A.f32(NBLK)
    be512 = A.f32(NBLK)
    idx2f = A.f32(4 * NBLK)
    iop_i = A.ar[:, A.top:A.top + 1].bitcast(I32); A.top += 1
    iop_f = A.f32(1)
    destf = A.f32(2 * NT)
    disp_top = A.top

    p.op("pool", lambda e: e.memset(onesb, 1.0), writes=["onesb"])
    p.op("pool", lambda e: e.memset(Ltri, 1.0), writes=["Ltri"])
    p.op("pool", lambda e: e.affine_select(out=Ltri, in_=Ltri, pattern=[[1, 128]], compare_op=ALU.is_gt,
                                           fill=0.0, base=0, channel_multiplier=-1), reads=["Ltri"], writes=["Ltri"])
    p.op("pool", lambda e: e.iota(out=iop_i, pattern=[[0, 1]], base=0, channel_multiplier=1), writes=["iop_i"])
    p.op("pool", lambda e: e.tensor_copy(out=iop_f, in_=iop_i), reads=["iop_i"], writes=["iop_f"])
    p.op("dve", lambda e: e.memset(base, 0.0), writes=["base"])
    p.op("dve", lambda e: e.memset(ones32, 1.0), writes=["ones32"])
    p.op("dve", lambda e: e.memset(epsA, EPS), writes=["epsA"])
    for t in range(NT):
        p.op("dve", (lambda t=t: lambda e: e.tensor_tensor(out=OS, in0=OH1[:, t, :], in1=OH2[:, t, :], op=ALU.add))(),
             reads=["OH1_%d" % t, "OH2_%d" % t], writes=["OS"])
        bk, bkk = nb()
        p.op("pe", (lambda bk=bk: lambda e: e.matmul(bk[:, 0:32], lhsT=onesb, rhs=OS, start=True, stop=True))(),
             reads=["onesb", "OS"], writes=[bkk])
        p.op("pe", (lambda bk=bk: lambda e: e.matmul(bk[:, 32:64], lhsT=Ltri, rhs=OS, start=True, stop=True))(),
             reads=["Ltri", "OS"], writes=[bkk])
        p.op("dve", (lambda bk=bk, t=t: lambda e: e.tensor_tensor(out=Rall[:, t, :], in0=bk[:, 32:64], in1=base, op=ALU.add))(),
             reads=[bkk, "base"], writes=["R_%d" % t])
        p.op("dve", (lambda bk=bk: lambda e: e.tensor_tensor(out=base, in0=bk[:, 0:32], in1=base, op=ALU.add))(),
             reads=[bkk, "base"], writes=["base"])
    p.op("dve", lambda e: e.memset(nblk, 0.0), writes=["nblk"])
    for m in range(17):
        p.op("dve", (lambda m=m: lambda e: e.scalar_tensor_tensor(out=nblk, in0=base, scalar=float(BLK * m), in1=nblk,
                                                                 op0=ALU.is_gt, op1=ALU.add))(), reads=["base", "nblk"], writes=["nblk"])
    p.op("dve", lambda e: e.tensor_scalar(out=padded, in0=nblk, scalar1=float(BLK), scalar2=None, op0=ALU.mult),
         reads=["nblk"], writes=["padded"])
    p.op("dve", lambda e: e.tensor_tensor_scan(out=pend, data0=ones32, data1=padded, initial=0.0, op0=ALU.mult, op1=ALU.add),
         reads=["ones32", "padded"], writes=["pend"])
    p.op("dve", lambda e: e.tensor_tensor(out=pstart, in0=pend, in1=padded, op=ALU.subtract), reads=["pend", "padded"], writes=["pstart"])
    for b in range(NBLK):
        p.op("dve", (lambda b=b: lambda e: e.tensor_scalar(out=cmpt, in0=pend, scalar1=float(b * BLK), scalar2=0.0,
                                                          op0=ALU.is_le, op1=ALU.add, accum_out=bef[:, b:b + 1]))(),
             reads=["pend"], writes=["cmpt", "bef%d" % b])
    BEK = ["bef%d" % b for b in range(NBLK)]
    p.op("dve", lambda e: e.tensor_scalar(out=idxwf, in0=bef, scalar1=31.0, scalar2=128.0, op0=ALU.min, op1=ALU.mult),
         reads=BEK, writes=["idxwf"])
    p.op("dve", lambda e: e.tensor_scalar(out=idxwf, in0=idxwf, scalar1=iop_f[:, 0:1], scalar2=None, op0=ALU.add),
         reads=["idxwf", "iop_f"], writes=["idxwf"])
    p.op("dve", lambda e: e.tensor_copy(out=idxwi, in_=idxwf), reads=["idxwf"], writes=["idxwi"])
    p.op("dve", lambda e: e.tensor_scalar(out=be512, in0=bef, scalar1=31.0, scalar2=512.0, op0=ALU.min, op1=ALU.mult),
         reads=BEK, writes=["be512"])
    p.op("dve", lambda e: e.tensor_scalar(out=be512, in0=be512, scalar1=iop_f[:, 0:1], scalar2=None, op0=ALU.add),
         reads=["be512", "iop_f"], writes=["be512"])
    idx2f3 = idx2f.rearrange("p (b h) -> p b h", h=4)
    for hc in range(4):
        p.op("dve", (lambda hc=hc: lambda e: e.tensor_scalar(out=idx2f3[:, :, hc], in0=be512, scalar1=float(hc * 128), scalar2=None, op0=ALU.add))(),
             reads=["be512"], writes=["idx2f_%d" % hc])
    p.op("dve", lambda e: e.tensor_copy(out=idx2i, in_=idx2f), reads=["idx2f_%d" % hc for hc in range(4)], writes=["idx2i"])
    NSTG = 6
    stg2 = [A.ar[:, NW - (NSTG - i) * 4096:NW - (NSTG - i - 1) * 4096] for i in range(NSTG)]
    ew1_r = ew1.rearrange("e (p k) n -> (e p) (k n)", p=128)
    ew3_r = ew3.rearrange("e (p k) n -> (e p) (k n)", p=128)
    ew2_r = ew2.rearrange("e h n -> (e h) n")

    def gather_block_weights(b):
        sset = b % 2
        for m, src in enumerate((ew1_r, ew3_r)):
            st = stg2[3 * sset + m]
            sk = "stg2_%d_%d" % (sset, m)
            p.dma("pool", (lambda st=st, src=src, b=b: lambda e: e.indirect_dma_start(
                out=st, out_offset=None, in_=src, in_offset=bass.IndirectOffsetOnAxis(ap=idxwi[:, b:b + 1], axis=0)))(),
                "ld_" + sk, reads=["idxwi"], writes=[sk])
        st = stg2[3 * sset + 2]
        for hc in range(4):
            sk = "stg2_%d_2_%d" % (sset, hc)
            p.dma("pool", (lambda st=st, b=b, hc=hc: lambda e: e.indirect_dma_start(
                out=st[:, hc * 1024:(hc + 1) * 1024], out_offset=None, in_=ew2_r,
                in_offset=bass.IndirectOffsetOnAxis(ap=idx2i[:, 4 * b + hc:4 * b + hc + 1], axis=0)))(),
                "ld_" + sk, reads=["idx2i"], writes=[sk])

    gather_block_weights(0)
    gather_block_weights(1)
    for t in range(NT):
        p.op("dve", (lambda t=t: lambda e: e.tensor_tensor(out=tmpd, in0=Rall[:, t, :], in1=pstart, op=ALU.add))(),
             reads=["R_%d" % t, "pstart"], writes=["tmpd"])
        for k, OH in enumerate((OH1, OH2)):
            p.op("dve", (lambda t=t, OH=OH: lambda e: e.tensor_tensor(out=prod, in0=tmpd, in1=OH[:, t, :], op=ALU.mult))(),
                 reads=["tmpd", "OH%d_%d" % (k + 1, t)], writes=["prod"])
            p.op("dve", (lambda t=t, k=k: lambda e: e.tensor_reduce(out=destf[:, 2 * t + k:2 * t + k + 1], in_=prod, axis=AX.X, op=ALU.add))(),
                 reads=["prod"], writes=["destf_%d_%d" % (t, k)])
    DFK = ["destf_%d_%d" % (t, k) for t in range(NT) for k in range(2)]
    p.op("dve", lambda e: e.tensor_copy(out=desti, in_=destf), reads=DFK, writes=["desti"])

    hsc = [A.bf16(1024) for _ in range(8)]
    for t in range(NT):
        hb = hsc[t % 8]
        hbk = "hsc%d" % (t % 8)
        p.dma("sp", (lambda hb=hb, t=t: lambda e: e.dma_start(out=hb, in_=h2tm_v[:, t, :]))(), "ld_" + hbk,
              reads=["h2tmd_%d" % t], writes=[hbk])
        for k in range(2):
            p.dma("pool", (lambda hb=hb, t=t, k=k: lambda e: e.indirect_dma_start(
                out=xs_d, out_offset=bass.IndirectOffsetOnAxis(ap=desti[:, 2 * t + k:2 * t + k + 1], axis=0),
                in_=hb, in_offset=None))(), "sc_%d_%d" % (t % 8, k), reads=[hbk, "desti"] + XSZK, writes=["xs_sc_%d_%d" % (t, k)])
    XSK = ["xs_sc_%d_%d" % (t, k) for t in range(NT) for k in range(2)]

    p.barrier(barw)
    A.top = const_top
    wb = [[A.bf16(8, 512), A.bf16(8, 512), A.bf16(4, 1024)] for _ in range(2)]
    xs_sb = [A.bf16(4, 1024) for _ in range(1)]
    xsT = [A.bf16(8, 512) for _ in range(2)]
    gT = [A.bf16(4, 512) for _ in range(2)]
    s1 = [A.f32(512) for _ in range(1)]
    ysb = [A.f32(1024) for _ in range(4)]
    print("stage C arena top", A.top, "of", NW - NSTG * 4096)
    assert A.top <= NW - NSTG * 4096
    xs_v = xs_d.rearrange("(b s p) d -> b p s d", p=128, s=4)
    yb_v = ybuf_d.rearrange("(b s p) d -> b s p d", p=128, s=4)

    def cast_block_weights(b):
        sset = b % 2
        wbuf = wb[b % 2]
        st0 = stg2[3 * sset].rearrange("p (k n) -> p k n", k=8)
        st1 = stg2[3 * sset + 1].rearrange("p (k n) -> p k n", k=8)
        st2 = stg2[3 * sset + 2].rearrange("p (k n) -> p k n", k=4)
        p.op("dve", (lambda st0=st0, wbuf=wbuf: lambda e: e.tensor_copy(out=wbuf[0], in_=st0))(),
             reads=["stg2_%d_0" % sset], writes=["wb%d_0" % (b % 2)])
        p.op("act", (lambda st1=st1, wbuf=wbuf: lambda e: e.copy(out=wbuf[1], in_=st1))(),
             reads=["stg2_%d_1" % sset], writes=["wb%d_1" % (b % 2)])
        p.op("dve", (lambda st2=st2, wbuf=wbuf: lambda e: e.tensor_copy(out=wbuf[2], in_=st2))(),
             reads=["stg2_%d_2_%d" % (sset, hc) for hc in range(4)], writes=["wb%d_2" % (b % 2)])

    def load_xs(b):
        xb_ = xs_sb[0]
        p.dma("sp", (lambda xb_=xb_, b=b: lambda e: e.dma_start(out=xb_, in_=xs_v[b]))(), "ld_xs_sb0",
              reads=XSK, writes=["xs_sb0"])

    def transposes(b):
        xb_ = xs_sb[0]
        xbk = "xs_sb0"
        xT = xsT[b % 2]
        for st_ in range(4):
            tb, tbk = nb()
            tpv = tb.bitcast(BF16)
            xv = xb_[:, st_, :].rearrange("s (p k) -> s k p", k=8)
            for kc in range(8):
                p.op("pe", (lambda tpv=tpv, xv=xv, kc=kc: lambda e: e.transpose(out=tpv[:, kc * 128:(kc + 1) * 128], in_=xv[:, kc, :],
                                                                                identity=identb))(),
                     reads=[xbk, "identb"], writes=[tbk])
            src3 = tpv.rearrange("p (k s) -> p k s", k=8)
            dst3 = xT[:, :, st_ * 128:(st_ + 1) * 128]
            xTk = "xsT%d_%d" % (b % 2, st_)
            if st_ % 2 == 0:
                p.op("act", (lambda src3=src3, dst3=dst3: lambda e: e.copy(out=dst3, in_=src3))(), reads=[tbk], writes=[xTk])
            else:
                p.op("dve", (lambda src3=src3, dst3=dst3: lambda e: e.tensor_copy(out=dst3, in_=src3))(), reads=[tbk], writes=[xTk])

    load_xs(0)
    cast_block_weights(0)
    transposes(0)
    load_xs(1)
    for b in range(NBLK):
        wbuf = wb[b % 2]
        WK = ["wb%d_%d" % (b % 2, m) for m in range(3)]
        xT = xsT[b % 2]
        XTK = ["xsT%d_%d" % (b % 2, st_) for st_ in range(4)]
        gTb = gT[b % 2]
        for hc in range(4):
            b1, b1k = nb()
            b3, b3k = nb()
            for kc in range(8):
                p.op("pe", (lambda b1=b1, wbuf=wbuf, kc=kc, hc=hc, xT=xT: lambda e: e.matmul(
                    b1, lhsT=wbuf[0][:, kc, hc * 128:(hc + 1) * 128], rhs=xT[:, kc, :], start=(kc == 0), stop=(kc == 7)))(),
                    reads=XTK + [WK[0]], writes=[b1k])
            for kc in range(8):
                p.op("pe", (lambda b3=b3, wbuf=wbuf, kc=kc, hc=hc, xT=xT: lambda e: e.matmul(
                    b3, lhsT=wbuf[1][:, kc, hc * 128:(hc + 1) * 128], rhs=xT[:, kc, :], start=(kc == 0), stop=(kc == 7)))(),
                    reads=XTK + [WK[1]], writes=[b3k])
            s1b = s1[0]
            s1k = "s1_0"
            p.op("act", (lambda b1=b1, s1b=s1b: lambda e: e.activation(out=s1b, in_=b1, func=AF.Silu))(), reads=[b1k], writes=[s1k])
            p.op("dve", (lambda b3=b3, s1b=s1b, gTb=gTb, hc=hc: lambda e: e.tensor_tensor(out=gTb[:, hc, :], in0=b3, in1=s1b, op=ALU.mult))(),
                 reads=[b3k, s1k], writes=["gT%d_%d" % (b % 2, hc)])
        if b + 1 < NBLK:
            transposes(b + 1)
            if b + 2 < NBLK:
                load_xs(b + 2)
            cast_block_weights(b + 1)
        if b + 2 < NBLK:
            gather_block_weights(b + 2)
        GK = ["gT%d_%d" % (b % 2, hc) for hc in range(4)]
        for st_ in range(4):
            yi = st_
            yb_ = ysb[yi]
            for dh in range(2):
                by, byk = nb()
                for hc in range(4):
                    p.op("pe", (lambda by=by, gTb=gTb, hc=hc, st_=st_, dh=dh, wbuf=wbuf: lambda e: e.matmul(
                        by, lhsT=gTb[:, hc, st_ * 128:(st_ + 1) * 128], rhs=wbuf[2][:, hc, dh * 512:(dh + 1) * 512],
                        start=(hc == 0), stop=(hc == 3)))(), reads=GK + [WK[2]], writes=[byk])
                if dh == 0:
                    p.op("act", (lambda by=by, yb_=yb_: lambda e: e.copy(out=yb_[:, 0:512], in_=by))(), reads=[byk], writes=["ysb%d_0" % yi])
                else:
                    p.op("dve", (lambda by=by, yb_=yb_: lambda e: e.tensor_copy(out=yb_[:, 512:1024], in_=by))(), reads=[byk], writes=["ysb%d_1" % yi])
            p.dma("sp", (lambda yb_=yb_, b=b, st_=st_: lambda e: e.dma_start(out=yb_v[b, st_], in_=yb_))(), "st_ysb%d" % yi,
                  reads=["ysb%d_0" % yi, "ysb%d_1" % yi], writes=["ybuf_%d_%d" % (b, st_)])
    YBK = ["ybuf_%d_%d" % (b, st_) for b in range(NBLK) for st_ in range(4)]

    p.barrier(barw)
    A.top = const_top
    x1r = [A.f32(1024) for _ in range(4)]
    Y1 = [A.f32(1024) for _ in range(4)]
    Y2 = [A.f32(1024) for _ in range(4)]
    tcm = [A.f32(1024) for _ in range(4)]
    oo = [A.f32(1024) for _ in range(4)]
    junk2 = A.bf16(1024)
    sm2 = A.f32(16)
    ssD = A.f32(NT)
    p.op("pool", lambda e: e.memset(ssD, 0.0), writes=["ssD"])

    def d_a(t):
        i2 = t % 4
        p.dma("sp", (lambda t=t, i2=i2: lambda e: e.dma_start(out=x1r[i2], in_=x1_v[:, t, :]))(), ["ld_xa0", "ld_xa1", "ld_xres0", "ld_xres1"][i2],
              reads=["x1d_%d" % t], writes=["x1r%d" % i2])
        for k, Yb in enumerate((Y1, Y2)):
            p.dma("pool", (lambda t=t, k=k, Yb=Yb, i2=i2: lambda e: e.indirect_dma_start(
                out=Yb[i2], out_offset=None, in_=ybuf_d,
                in_offset=bass.IndirectOffsetOnAxis(ap=desti[:, 2 * t + k:2 * t + k + 1], axis=0)))(),
                "ld_stg2_%d_2_%d" % (k, i2), reads=YBK + ["desti"], writes=["Y%d_%d" % (k, i2)])
        p.op("act", (lambda t=t, i2=i2: lambda e: e.activation(out=tcm[i2], in_=Y1[i2], func=AF.Copy, scale=W12[:, t, 0:1]))(),
             reads=["Y0_%d" % i2, "W1_%d" % t], writes=["tcm%d" % i2])

    def d_b(t):
        i2 = t % 4
        tk = "tcm%d" % i2
        p.op("dve", (lambda t=t, i2=i2: lambda e: e.scalar_tensor_tensor(out=tcm[i2], in0=Y2[i2], scalar=W12[:, t, 1:2], in1=tcm[i2],
                                                                         op0=ALU.mult, op1=ALU.add))(),
             reads=["Y1_%d" % i2, "W2_%d" % t, tk], writes=[tk])
        p.op("dve", (lambda i2=i2: lambda e: e.tensor_tensor(out=tcm[i2], in0=tcm[i2], in1=g2_bc, op=ALU.mult))(), reads=[tk, "g2_bc0", "g2_bc1"], writes=[tk])
        p.op("dve", (lambda i2=i2: lambda e: e.tensor_tensor(out=tcm[i2], in0=tcm[i2], in1=x1r[i2], op=ALU.add))(),
             reads=[tk, "x1r%d" % i2], writes=[tk])

    def d_c(t):
        i2 = t % 4
        tk = "tcm%d" % i2
        ssf = ssD[:, t:t + 1]
        lnc = sm2[:, 2 * i2:2 * i2 + 1]
        rsc = sm2[:, 2 * i2 + 1:2 * i2 + 2]
        p.op("act", (lambda ssf=ssf, i2=i2: lambda e: e.activation(out=junk2, in_=tcm[i2], func=AF.Square, accum_out=ssf))(),
             reads=[tk, "ssD"], writes=["junk2", "ssf%d" % i2])
        p.op("act", (lambda ssf=ssf, lnc=lnc: lambda e: e.activation(out=lnc, in_=ssf, func=AF.Ln, bias=epsA[:, 0:1], scale=1.0 / D))(),
             reads=["ssf%d" % i2, "epsA"], writes=["sqf%d" % i2])
        p.op("act", (lambda lnc=lnc, rsc=rsc: lambda e: e.activation(out=rsc, in_=lnc, func=AF.Exp, scale=-0.5))(), reads=["sqf%d" % i2], writes=["rstdf%d" % i2])

    def d_d(t):
        i2 = t % 4
        rsc = sm2[:, 2 * i2 + 1:2 * i2 + 2]
        p.op("dve", (lambda i2=i2, rsc=rsc: lambda e: e.scalar_tensor_tensor(out=oo[i2], in0=tcm[i2], scalar=rsc, in1=fg_bc, op0=ALU.mult, op1=ALU.mult))(),
             reads=["tcm%d" % i2, "rstdf%d" % i2, "fg_bc"], writes=["oo%d" % i2])
        p.dma("sp", (lambda t=t, i2=i2: lambda e: e.dma_start(out=out_v[:, t, :], in_=oo[i2]))(), "st_ysb%d" % i2,
              reads=["oo%d" % i2], writes=["outd_%d" % t])

    d_a(0)
    for t in range(NT):
        if t + 1 < NT:
            d_a(t + 1)
        d_b(t)
        d_c(t)
        if t >= 1:
            d_d(t - 1)
    d_d(NT - 1)

    p.wait_all("sp", ["outd_%d" % t for t in range(NT)])
    p.emit()
    return nc


_NC_CACHE = {}


def _prep_inputs(inp, b):
    f = np.float32

    def colz(v, n):
        return np.ascontiguousarray(np.asarray(v, f).reshape(n, 128).T)

    m = {}
    m["x"] = np.ascontiguousarray(inp["x"][b])
    m["c_col"] = colz(inp["c"][b], 8)
    m["ada_w"] = np.ascontiguousarray(inp["ada_w"][0])
    m["ada_b"] = colz(inp["ada_b"][0], 48)
    m["adab_bc"] = np.ascontiguousarray(np.broadcast_to(np.asarray(inp["ada_b"][0], f)[None, :], (128, 6 * D)))
    m["n2g_bc"] = np.ascontiguousarray(np.broadcast_to(np.asarray(inp["norm2_g"][0], f)[None, :], (128, D)))
    m["n1g"] = colz(inp["norm1_g"][0], 8)
    m["n2g"] = colz(inp["norm2_g"][0], 8)
    m["fg_bc"] = np.ascontiguousarray(np.broadcast_to(np.asarray(inp["final_g"], f)[None, :], (128, D)))
    m["w_in"] = np.ascontiguousarray(inp["w_in"][0])
    m["w_out"] = np.ascontiguousarray(inp["w_out"][0])
    cwv = np.asarray(inp["conv_w"][0], f)
    m["conv_w"] = np.ascontiguousarray(cwv.T.reshape(4, 128, 4).transpose(1, 0, 2))
    m["conv_b"] = colz(inp["conv_b"][0], 4)
    m["gaw"] = np.ascontiguousarray(inp["gate_a_w"][0])
    m["giw"] = np.ascontiguousarray(inp["gate_i_w"][0])
    m["gab"] = colz(inp["gate_a_b"][0], 4)
    m["gib"] = colz(inp["gate_i_b"][0], 4)
    m["lam"] = colz(inp["lru_lambda"][0], 4)
    m["lng_bc"] = np.ascontiguousarray(np.broadcast_to(np.asarray(inp["sgu_ln_g"][0], f)[None, :], (128, 512)))
    m["lnb_bc"] = np.ascontiguousarray(np.broadcast_to(np.asarray(inp["sgu_ln_b"][0], f)[None, :], (128, 512)))
    m["sgu_wT"] = np.ascontiguousarray(np.asarray(inp["sgu_w"][0], f).transpose(2, 0, 1))
    bs = np.asarray(inp["sgu_b"][0], f)
    bsr = np.repeat(bs, 64, axis=0)
    bsr = np.tile(bsr, (1, 4))
    m["bsrep"] = np.ascontiguousarray(bsr.reshape(4, 128, 512).transpose(1, 0, 2))
    wr = np.concatenate([np.asarray(inp["router_group_w"][0], f), np.asarray(inp["router_expert_w"][0], f)], axis=1)
    m["wr"] = np.ascontiguousarray(wr.reshape(8, 128, 36).transpose(1, 0, 2))
    br = np.concatenate([np.asarray(inp["router_group_b"][0], f), np.asarray(inp["router_expert_b"][0], f)])
    m["br_bc"] = np.ascontiguousarray(np.broadcast_to(br[None, :], (128, 36)))
    m["zeros_blk"] = np.zeros((512, D), dtype=ml_dtypes.bfloat16)
    m["ew1"] = np.ascontiguousarray(inp["expert_w1"][0])
    m["ew3"] = np.ascontiguousarray(inp["expert_w3"][0])
    m["ew2"] = np.ascontiguousarray(inp["expert_w2"][0])
    return m


def kernel(**inputs):
    inp = {k: np.asarray(v) for k, v in inputs.items()}
    if "nc" not in _NC_CACHE:
        _NC_CACHE["nc"] = build_nc()
    nc = _NC_CACHE["nc"]
    in_maps = [_prep_inputs(inp, b) for b in range(8)]
    res = run_bass_kernel_spmd(nc, in_maps, core_ids=list(range(8)))
    _NC_CACHE["last"] = res
    outs = [np.asarray(res.results[b]["out"]).reshape(S, D) for b in range(8)]
    return np.stack(outs, axis=0).astype(np.float32)
```

```python
import contextlib
import numpy as np
import ml_dtypes
import concourse.bass as bass
import concourse.mybir as mybir
from concourse.bass_utils import run_bass_kernel_spmd

F32 = mybir.dt.float32
BF16 = mybir.dt.bfloat16
I32 = mybir.dt.int32
ALU = mybir.AluOpType
AF = mybir.ActivationFunctionType
AX = mybir.AxisListType

D = 1024
S = 4096
NT = S // 128
NE = 32
DH = 512
EPS = 1e-6
DEBUG_STOP = None
import os
KCUT = os.environ.get('KCUT', '')


class Prog:
    ENG = ("pe", "act", "dve", "pool", "sp")

    def __init__(self, nc):
        self.nc = nc
        self.es = contextlib.ExitStack()
        self.ops = {e: [] for e in self.ENG}
        self.cnt = {e: 0 for e in self.ENG}
        self.esem = {e: self.es.enter_context(nc.semaphore("s_" + e)) for e in self.ENG}
        self.seen = {e: {} for e in self.ENG}
        self.dsem = {}
        self.state = {}
        self.nsem = 0

    def sb(self, name, shape, dt):
        return self.es.enter_context(self.nc.sbuf_tensor(name, list(shape), dt))

    def ps(self, name, shape, dt):
        return self.es.enter_context(self.nc.psum_tensor(name, list(shape), dt))

    def _st(self, k):
        s = self.state.get(k)
        if s is None:
            s = self.state[k] = {"w": [], "r": []}
        return s

    def _deps(self, eng, reads, writes):
        need = []
        for k in reads:
            if k.startswith("bank"):
                s = self._st(k)
                need += [(ev, True) for ev in s["w"] + s["r"]]
            else:
                need += [(ev, False) for ev in self._st(k)["w"]]
        for k in writes:
            s = self._st(k)
            isp = k.startswith("bank")
            need += [(ev, isp) for ev in s["w"] + s["r"]]
        waits = {}
        for ((sem_id, sem, val, src_eng), isp) in need:
            if src_eng == eng and (isp or eng in ("pe", "sp")):
                continue
            if self.seen[eng].get(sem_id, 0) >= val:
                continue
            if waits.get(sem_id, (None, 0))[1] < val:
                waits[sem_id] = (sem, val)
        for sem_id, (sem, val) in waits.items():
            self.seen[eng][sem_id] = val
        return list(waits.values())

    def _commit(self, ev, reads, writes):
        for k in reads:
            if k.startswith("bank"):
                s = self._st(k)
                s["w"] = [ev]
                s["r"] = []
            else:
                self._st(k)["r"].append(ev)
        for k in writes:
            s = self._st(k)
            s["w"] = [ev]
            s["r"] = []

    def op(self, eng, fn, reads=(), writes=()):
        waits = self._deps(eng, reads, writes)
        self.cnt[eng] += 1
        val = self.cnt[eng]
        sem = self.esem[eng]
        self.ops[eng].append((waits, fn, sem, 1))
        ev = ("e_" + eng, sem, val, eng)
        self._commit(ev, reads, writes)
        return ev

    def dma(self, q, fn, semkey, reads=(), writes=()):
        waits = self._deps(q, reads, writes)
        ent = self.dsem.get(semkey)
        if ent is None:
            sem = self.es.enter_context(self.nc.semaphore("d%d" % self.nsem))
            self.nsem += 1
            ent = self.dsem[semkey] = [sem, 0]
        ent[1] += 16
        self.ops[q].append((waits, fn, ent[0], 16))
        ev = ("d_" + str(semkey), ent[0], ent[1], "dma")
        self._commit(ev, reads, writes)
        return ev

    def barrier(self, scratch):
        keys = list(self.state.keys())
        self.op("pool", lambda e: e.memset(scratch, 0.0), reads=keys, writes=keys + ["__bar"])
        for eng in ("pe", "act", "dve", "sp"):
            waits = self._deps(eng, ["__bar"], ())
            self.ops[eng].append((waits, None, None, 0))

    def wait_all(self, eng, keys):
        waits = self._deps(eng, keys, ())
        self.ops[eng].append((waits, None, None, 0))

    def emit(self):
        nc = self.nc
        with nc.Block() as block:
            def run(e):
                def body(h):
                    for (waits, fn, sem, inc) in self.ops[e]:
                        for (s, v) in waits:
                            h.wait_ge(s, v)
                        if fn is not None:
                            fn(h).then_inc(sem, inc)
                return body
            block.tensor(run("pe"))
            block.scalar(run("act"))
            block.vector(run("dve"))
            block.gpsimd(run("pool"))
            block.sync(run("sp"))
        self.es.close()


class Arena:
    def __init__(self, ar, nwords):
        self.ar = ar
        self.n = nwords
        self.top = 0
        self.gen = 0

    def _view(self, v, shape):
        if len(shape) == 2:
            v = v.rearrange("p (a b) -> p a b", a=shape[0])
        elif len(shape) == 3:
            v = v.rearrange("p (a b c) -> p a b c", a=shape[0], b=shape[1])
        return v

    def f32(self, *shape):
        n = int(np.prod(shape))
        a = self.top
        self.top += n
        assert self.top <= self.n, ("arena overflow", self.top, self.n)
        return self._view(self.ar[:, a:a + n], shape)

    def bf16(self, *shape):
        n = int(np.prod(shape))
        w = (n + 1) // 2
        a = self.top
        self.top += w
        assert self.top <= self.n, ("arena overflow", self.top, self.n)
        return self._view(self.ar[:, a:a + w].bitcast(BF16), shape)


def build_nc():
    nc = bass.Bass("TRN2", target_bir_lowering=False)

    def din(name, shape, dt=F32):
        return nc.dram_tensor(name, list(shape), dt, kind="ExternalInput").ap()

    x = din("x", [S, D])
    c_col = din("c_col", [128, 8])
    ada_w = din("ada_w", [D, 6 * D])
    ada_b = din("ada_b", [128, 48])
    adab_bc_d = din("adab_bc", [128, 6 * D])
    n2g_bc_d = din("n2g_bc", [128, D])
    n1g = din("n1g", [128, 8])
    n2g = din("n2g", [128, 8])
    fg_bc_d = din("fg_bc", [128, D])
    w_in = din("w_in", [D, 2048])
    w_out = din("w_out", [D, D])
    conv_w = din("conv_w", [128, 4, 4])
    conv_b = din("conv_b", [128, 4])
    gaw = din("gaw", [8, 64, 64])
    giw = din("giw", [8, 64, 64])
    gab = din("gab", [128, 4])
    gib = din("gib", [128, 4])
    lam = din("lam", [128, 4])
    lng_bc_d = din("lng_bc", [128, 512])
    lnb_bc_d = din("lnb_bc", [128, 512])
    sgu_wT = din("sgu_wT", [128, 8, 128])
    bsrep_d = din("bsrep", [128, 4, 512])
    wr_d = din("wr", [128, 8, 36])
    br_bc_d = din("br_bc", [128, 36])
    zeros_d = din("zeros_blk", [512, D], BF16)
    ew1 = din("ew1", [NE, D, DH])
    ew3 = din("ew3", [NE, D, DH])
    ew2 = din("ew2", [NE, DH, D])
    out = nc.dram_tensor("out", [S, D], F32, kind="ExternalOutput").ap()
    x1_d = nc.dram_tensor("x1_scr", [S, D], F32, kind="ExternalOutput" if DEBUG_STOP else "Internal").ap()
    h2tm_d = nc.dram_tensor("h2tm_scr", [S, D], BF16, kind="Internal").ap()
    NBLK = 47
    BLK = 512
    xs_d = nc.dram_tensor("xs_scr", [NBLK * BLK, D], BF16, kind="Internal").ap()
    ybuf_d = nc.dram_tensor("ybuf_scr", [NBLK * BLK, D], F32, kind="Internal").ap()

    p = Prog(nc)
    NW = 53200
    ar = p.sb("arena", [128, NW], F32)
    A = Arena(ar, NW)
    banks = [p.ps("bank%d" % i, [128, 512], F32) for i in range(8)]
    bank_i = [0]

    def nb():
        i = bank_i[0] % 8
        bank_i[0] += 1
        return banks[i][:], "bank%d" % i

    dcount = [0]

    def ld(dst, src, key, reads=(), q="sp"):
        dcount[0] += 1
        p.dma(q, lambda e: e.dma_start(out=dst, in_=src), "ld_" + key, reads=list(reads), writes=[key])

    ident = A.f32(128)
    ones = A.f32(128)
    identb = A.bf16(128)
    modT = A.f32(48)
    gam1 = A.f32(8)
    gam2 = A.f32(8)
    n1g_t = A.f32(8)
    n2g_t = A.f32(8)
    adab_t = A.f32(48)
    ccol = A.f32(8)
    cond = A.f32(8)
    g2_bc = A.f32(1024)
    fg_bc = A.f32(1024)
    W12 = A.f32(NT, 2)
    br_bc = A.f32(36)
    wr_t = A.f32(8, 36)
    barw = A.f32(1)
    NBLK_ = 47
    idxwi = A.ar[:, A.top:A.top + NBLK_].bitcast(I32); A.top += NBLK_
    desti = A.ar[:, A.top:A.top + 2 * NT].bitcast(I32); A.top += 2 * NT
    idx2i = A.ar[:, A.top:A.top + 4 * NBLK_].bitcast(I32); A.top += 4 * NBLK_
    epsA = A.f32(1)
    const_top = A.top
    OH1 = A.bf16(NT, 32)
    OH2 = A.bf16(NT, 32)
    oh_top = A.top
    gam2_bc = A.f32(1024)
    sh2_bc = A.f32(1024)
    w_inb = A.bf16(8, 2048)
    w_outb = A.bf16(8, 1024)
    bda = A.bf16(4, 128)
    bdi = A.bf16(4, 128)
    wmT = A.bf16(8, 128)
    bsrep = A.f32(4, 512)
    lng_bc = A.f32(512)
    lnb_bc = A.f32(512)
    cw = A.f32(4, 4)
    cb = A.f32(4)
    gab_t = A.f32(4)
    gib_t = A.f32(4)
    lam_t = A.f32(4)
    cneg = A.f32(4)
    cneg2 = A.f32(4)
    lt0 = A.f32(4)
    lt1 = A.f32(4)
    hstate = A.f32(4)
    ngab_t = A.f32(4)
    ngib_t = A.f32(4)
    work_base = A.top

    p.op("pool", lambda e: e.memset(ident, 1.0), writes=["ident"])
    p.op("pool", lambda e: e.affine_select(out=ident, in_=ident, pattern=[[-1, 128]], compare_op=ALU.is_equal,
                                           fill=0.0, base=0, channel_multiplier=1), reads=["ident"], writes=["ident"])
    p.op("pool", lambda e: e.tensor_copy(out=identb, in_=ident), reads=["ident"], writes=["identb"])
    p.op("pool", lambda e: e.memset(ones, 1.0), writes=["ones"])

    ld(ccol, c_col, "ccol")
    ld(adab_t, ada_b, "adab")
    ld(n1g_t, n1g, "n1g")
    ld(n2g_t, n2g, "n2g")
    ld(fg_bc, fg_bc_d, "fg_bc")
    ld(br_bc, br_bc_d, "br_bc")
    ld(wr_t, wr_d, "wr")


    def cut(label):
        if KCUT == label:
            p.barrier(barw)
            p.dma("sp", lambda e: e.dma_start(out=x1_d[0:128, 0:48], in_=modT), "dbgc", reads=["__bar"], writes=["dbgc"])
            p.wait_all("sp", ["dbgc"])
            p.emit()
            return True
        return False

    if cut('c0'):
        return nc
    ld(bsrep, bsrep_d, "bsrep")
    ld(lng_bc, lng_bc_d, "lng_bc")
    ld(lnb_bc, lnb_bc_d, "lnb_bc")
    ld(cw, conv_w, "cw")
    ld(cb, conv_b, "cb")
    ld(gab_t, gab, "gab")
    ld(gib_t, gib, "gib")
    ld(lam_t, lam, "lam")
    st1 = A.f32(2, 1024)
    bdst = st1[:, 0, :].rearrange("p (g c m) -> p g c m", g=2, c=4)
    p.op("pool", lambda e: e.memset(st1[:, 0, :], 0.0), reads=[], writes=["misc0", "misc1"])
    for gi, gw in enumerate((gaw, giw)):
        for cc in range(4):
            for hh in range(2):
                p.dma("sp", (lambda gi=gi, gw=gw, cc=cc, hh=hh: lambda e: e.dma_start(
                    out=bdst[hh * 64:(hh + 1) * 64, gi, cc, hh * 64:(hh + 1) * 64], in_=gw[2 * cc + hh]))(),
                    "ld_bd%d_%d_%d" % (gi, cc, hh), reads=["misc0"], writes=["bdst%d_%d_%d" % (gi, cc, hh)])
    BDK = ["bdst%d_%d_%d" % (gi, cc, hh) for gi in range(2) for cc in range(4) for hh in range(2)]
    p.op("pool", lambda e: e.tensor_copy(out=bda, in_=bdst[:, 0, :, :]), reads=BDK, writes=["bda"])
    p.op("pool", lambda e: e.tensor_copy(out=bdi, in_=bdst[:, 1, :, :]), reads=BDK, writes=["bdi"])
    if cut('c4'):
        return nc
    wsg = st1[:, 1, :].rearrange("p (g i) -> p g i", g=8)
    p.dma("sp", lambda e: e.dma_start(out=wsg, in_=sgu_wT), "ld_wsg", reads=["misc1"], writes=["wsg"])
    p.op("pool", lambda e: e.memset(wsg[64:128, :, 0:64], 0.0), reads=["wsg"], writes=["wsg"])
    p.op("pool", lambda e: e.tensor_copy(out=wmT, in_=wsg), reads=["wsg"], writes=["wmT"])
    if cut('c5'):
        return nc
    p.op("act", lambda e: e.activation(out=lt0, in_=lam_t, func=AF.Exp, scale=-1.0), reads=["lam"], writes=["lt0"])
    p.op("dve", lambda e: e.tensor_scalar(out=lt1, in0=lt0, scalar1=-0.25, scalar2=1.0 / 3.0, op0=ALU.mult, op1=ALU.add),
         reads=["lt0"], writes=["lt1"])
    p.op("dve", lambda e: e.tensor_tensor(out=lt1, in0=lt1, in1=lt0, op=ALU.mult), reads=["lt1", "lt0"], writes=["lt1"])
    p.op("dve", lambda e: e.tensor_scalar(out=lt1, in0=lt1, scalar1=-0.5, scalar2=None, op0=ALU.add), reads=["lt1"], writes=["lt1"])
    p.op("dve", lambda e: e.tensor_tensor(out=lt1, in0=lt1, in1=lt0, op=ALU.mult), reads=["lt1", "lt0"], writes=["lt1"])
    p.op("dve", lambda e: e.tensor_scalar(out=lt1, in0=lt1, scalar1=1.0, scalar2=None, op0=ALU.add), reads=["lt1"], writes=["lt1"])
    p.op("dve", lambda e: e.tensor_tensor(out=lt1, in0=lt1, in1=lt0, op=ALU.mult), reads=["lt1", "lt0"], writes=["lt1"])
    p.op("dve", lambda e: e.tensor_scalar(out=cneg, in0=lt1, scalar1=-8.0, scalar2=None, op0=ALU.mult), reads=["lt1"], writes=["cneg"])
    p.op("dve", lambda e: e.tensor_scalar(out=cneg2, in0=lt1, scalar1=-16.0, scalar2=None, op0=ALU.mult), reads=["lt1"], writes=["cneg2"])
    p.op("dve", lambda e: e.memset(hstate, 0.0), writes=["hstate%d" % cc for cc in range(4)])
    p.op("dve", lambda e: e.tensor_scalar(out=ngab_t, in0=gab_t, scalar1=-1.0, scalar2=None, op0=ALU.mult), reads=["gab"], writes=["ngab"])
    p.op("dve", lambda e: e.tensor_scalar(out=ngib_t, in0=gib_t, scalar1=-1.0, scalar2=None, op0=ALU.mult), reads=["gib"], writes=["ngib"])
    p.op("act", lambda e: e.activation(out=cond, in_=ccol, func=AF.Silu), reads=["ccol"], writes=["cond"])
    ada_stg = [A.f32(8, 1024) for _ in range(2)]
    diag = A.f32(128)
    g1_bc = A.f32(1024)
    accm = A.f32(1024)
    abb = A.f32(1024)
    rowt = A.f32(1024)
    n2gb = A.f32(1024)
    ada_v = ada_w.rearrange("(kc p) n -> p kc n", p=128)
    ld(n2gb, n2g_bc_d, "n2gb")
    row_dst = {2: (g1_bc, "g1_bc"), 3: (sh2_bc, "sh2_bc"), 5: (g2_bc, "g2_bc")}
    condM = A.f32(4, 128)
    for q in range(4):
        p.op("dve", (lambda q=q: lambda e: e.tensor_scalar(out=condM[:, q, :], in0=ones, scalar1=cond[:, 4 + q:5 + q], scalar2=None, op0=ALU.mult))(),
             reads=["ones", "cond"], writes=["condM%d" % q])
    CMK = ["condM%d" % q for q in range(4)]
    wst = [A.f32(4, 512) for _ in range(2)]
    w_in_v = w_in.rearrange("(kc p) n -> p kc n", p=128)

    def w_in_piece(i):
        pc, hf = i // 2, i % 2
        buf = wst[i % 2]
        bk_ = "wst%d" % (i % 2)
        p.dma("sp", (lambda buf=buf, pc=pc, hf=hf: lambda e: e.dma_start(out=buf, in_=w_in_v[:, hf * 4:(hf + 1) * 4, pc * 512:(pc + 1) * 512]))(),
              "ld_" + bk_, writes=[bk_])
        for q in range(4):
            kc = hf * 4 + q
            p.op("pool", (lambda buf=buf, q=q, kc=kc, pc=pc: lambda e: e.tensor_copy(out=w_inb[:, kc, pc * 512:(pc + 1) * 512], in_=buf[:, q, :]))(),
                 reads=[bk_], writes=["w_inb_%d_%d" % (pc, kc)])

    w_out_v = w_out.rearrange("(kc p) n -> p kc n", p=128)

    def w_out_piece(i):
        hf, nh = i // 2, i % 2
        buf = wst[i % 2]
        bk_ = "wst%d" % (i % 2)
        p.dma("sp", (lambda buf=buf, hf=hf, nh=nh: lambda e: e.dma_start(out=buf, in_=w_out_v[:, hf * 4:(hf + 1) * 4, nh * 512:(nh + 1) * 512]))(),
              "ld_" + bk_, writes=[bk_])
        for q in range(4):
            kc = hf * 4 + q
            p.op("pool", (lambda buf=buf, q=q, kc=kc, nh=nh: lambda e: e.tensor_tensor(
                out=w_outb[:, kc, nh * 512:(nh + 1) * 512], in0=buf[:, q, :], in1=g1_bc[:, nh * 512:(nh + 1) * 512], op=ALU.mult))(),
                reads=[bk_, "g1_bc%d" % nh], writes=["w_outb_%d_%d" % (kc, nh)])

    for s in range(6):
        st = ada_stg[s % 2]
        sk = "adastg%d" % (s % 2)
        for hf in range(2):
            ld(st[:, hf * 4:(hf + 1) * 4, :], ada_v[:, hf * 4:(hf + 1) * 4, s * 1024:(s + 1) * 1024], sk + "_%d" % hf)
        ld(abb, adab_bc_d[:, s * 1024:(s + 1) * 1024], "abb")
        if s < 4:
            w_in_piece(2 * s)
            w_in_piece(2 * s + 1)
        else:
            w_out_piece(2 * (s - 4))
            w_out_piece(2 * (s - 4) + 1)
        p.op("dve", (lambda st=st: lambda e: e.tensor_scalar(out=accm, in0=st[:, 0, :], scalar1=cond[:, 0:1], scalar2=None, op0=ALU.mult))(),
             reads=[sk + "_0", "cond"], writes=["accm"])
        for kc in range(1, 4):
            p.op("dve", (lambda st=st, kc=kc: lambda e: e.scalar_tensor_tensor(out=accm, in0=st[:, kc, :], scalar=cond[:, kc:kc + 1], in1=accm,
                                                                              op0=ALU.mult, op1=ALU.add))(),
                 reads=[sk + "_0", "cond", "accm"], writes=["accm"])
        dst, dkey = row_dst.get(s, (rowt, "rowt"))
        for half in range(2):
            bk, bkk = nb()
            for q in range(4):
                p.op("pe", (lambda bk=bk, half=half, q=q, st=st: lambda e: e.matmul(
                    bk, lhsT=condM[:, q, :], rhs=st[:, 4 + q, half * 512:(half + 1) * 512], start=(q == 0), stop=False))(),
                    reads=CMK + [sk + "_1"], writes=[bkk])
            p.op("pe", (lambda bk=bk, half=half: lambda e: e.matmul(bk, lhsT=ones, rhs=accm[:, half * 512:(half + 1) * 512], start=False, stop=True))(),
                 reads=["ones", "accm"], writes=[bkk])
            p.op("dve", (lambda bk=bk, half=half, dst=dst: lambda e: e.tensor_tensor(
                out=dst[:, half * 512:(half + 1) * 512], in0=bk, in1=abb[:, half * 512:(half + 1) * 512], op=ALU.add))(),
                reads=[bkk, "abb"], writes=[dkey + "%d" % half])
        if s in (0, 1, 3, 4):
            for kc in range(8):
                p.op("dve", (lambda dst=dst, kc=kc: lambda e: e.tensor_tensor(out=diag, in0=dst[:, kc * 128:(kc + 1) * 128], in1=ident, op=ALU.mult))(),
                     reads=[dkey + "0", dkey + "1", "ident"], writes=["diag"])
                p.op("dve", (lambda s=s, kc=kc: lambda e: e.tensor_reduce(out=modT[:, s * 8 + kc:s * 8 + kc + 1], in_=diag, axis=AX.X, op=ALU.add))(),
                     reads=["diag"], writes=["modT_%d_%d" % (s, kc)])
        if s == 4:
            p.op("dve", lambda e: e.scalar_tensor_tensor(out=gam2_bc, in0=rowt, scalar=1.0, in1=n2gb, op0=ALU.add, op1=ALU.mult),
                 reads=["rowt0", "rowt1", "n2gb"], writes=["gam2_bc0", "gam2_bc1"])
    MODK = ["modT_%d_%d" % (s, kc) for s in (0, 1, 3, 4) for kc in range(8)]
    p.op("dve", lambda e: e.tensor_copy(out=modT[:, 16:17], in_=modT[:, 0:1]), reads=MODK, writes=["modT"])
    p.op("dve", lambda e: e.scalar_tensor_tensor(out=gam1, in0=modT[:, 8:16], scalar=1.0, in1=n1g_t,
                                                 op0=ALU.add, op1=ALU.mult), reads=["modT", "n1g"], writes=["gam1"])
    p.op("dve", lambda e: e.scalar_tensor_tensor(out=gam2, in0=modT[:, 32:40], scalar=1.0, in1=n2g_t,
                                                 op0=ALU.add, op1=ALU.mult), reads=["modT", "n2g"], writes=["gam2"])
    if cut('c1'):
        return nc
    sh1 = modT[:, 0:8]
    sh2 = modT[:, 24:32]

    if cut('c2'):
        return nc
    stg = [ada_stg[0], ada_stg[1]]


    W_INB_KEYS = ["w_inb_%d_%d" % (pc, kc) for pc in range(4) for kc in range(8)]
    W_OUTB_KEYS = ["w_outb_%d_%d" % (kc, nh) for kc in range(8) for nh in range(2)]
    if cut('c3'):
        return nc

    if DEBUG_STOP == "setup":
        p.dma("sp", lambda e: e.dma_start(out=x1_d[0:128, 0:48], in_=modT), "dbg0", reads=["modT"], writes=["dbg0"])
        p.dma("sp", lambda e: e.dma_start(out=x1_d[128:256, :], in_=g2_bc), "dbg1", reads=["g2_bc0", "g2_bc1"], writes=["dbg1"])
        p.dma("sp", lambda e: e.dma_start(out=x1_d[256:384, 0:4], in_=cneg), "dbg2", reads=["cneg"], writes=["dbg2"])
        p.op("pool", lambda e: e.tensor_copy(out=g1_bc, in_=w_outb[:, 0, :]), reads=W_OUTB_KEYS + ["g1_bc0", "g1_bc1"], writes=["g1x"])
        p.dma("sp", lambda e: e.dma_start(out=x1_d[384:512, :], in_=g1_bc), "dbg3", reads=["g1x"], writes=["dbg3"])
        p.barrier(barw)
        p.wait_all("sp", ["dbg0", "dbg1", "dbg2", "dbg3"])
        p.emit()
        return nc
    p.barrier(barw)
    A.top = work_base
    xa = [A.f32(1024) for _ in range(2)]
    xres = [A.f32(1024) for _ in range(2)]
    hT = A.bf16(8, 512)
    xr = A.f32(4, 515)
    grg = A.f32(4, 512)
    ug = A.f32(4, 512)
    vnb = A.bf16(4, 512)
    ysT = A.bf16(8, 512)
    xc = [A.f32(512) for _ in range(2)]
    xcb = [A.bf16(512) for _ in range(2)]
    rg = [A.f32(512) for _ in range(2)]
    ig = [A.f32(512) for _ in range(2)]
    av = [A.f32(512) for _ in range(2)]
    a2 = [A.f32(512) for _ in range(2)]
    hs = A.f32(512)
    sgt = A.f32(512)
    xn = A.bf16(1024)
    junk = xn
    x1t = [A.f32(1024) for _ in range(2)]
    xn2 = [A.f32(1024) for _ in range(2)]
    h2f = [A.f32(8, 128) for _ in range(2)]
    h2tm = [A.bf16(1024) for _ in range(2)]
    sm = A.f32(128)
    sm2v = sm
    lgt = A.f32(36)
    elm = A.f32(32)
    mx8 = A.f32(8)
    ssall = A.f32(96)
    print("phase1 arena top", A.top, "of", NW)

    p.op("pool", lambda e: e.memset(xr[:, :, 0:3], 0.0), writes=["xr%d" % cc for cc in range(4)])

    def rms_rstd(src, skey, dst_col, tag, slot=0, acc=0):
        ss = ssall[:, acc:acc + 1]
        sq = sm[:, 11 + 2 * slot:12 + 2 * slot]
        ssk = "ssacc%d" % acc
        p.op("act", lambda e: e.activation(out=junk, in_=src, func=AF.Square, accum_out=ss),
             reads=list(skey) + ["ssall"], writes=["xn", ssk])
        p.op("act", lambda e: e.activation(out=sq, in_=ss, func=AF.Ln, bias=sm[:, 8:9], scale=1.0 / D),
             reads=[ssk, "epsc"], writes=["sq%d" % slot])
        p.op("act", lambda e: e.activation(out=dst_col, in_=sq, func=AF.Exp, scale=-0.5), reads=["sq%d" % slot], writes=[tag])

    p.op("pool", lambda e: e.memset(ssall, 0.0), writes=["ssall"])
    p.op("pool", lambda e: e.memset(sm[:, 8:9], EPS), writes=["epsc"])
    p.op("pool", lambda e: e.memset(sm[:, 9:10], 1.0), writes=["onec"])

    x_v = x.rearrange("(t p) d -> p t d", p=128)
    out_v = out.rearrange("(t p) d -> p t d", p=128)
    x1_v = x1_d.rearrange("(t p) d -> p t d", p=128)
    h2tm_v = h2tm_d.rearrange("(t p) d -> p t d", p=128)

    def x_load(tq_):
        ld(xa[tq_ % 2], x_v[:, tq_, :], "xa%d" % (tq_ % 2))

    def norm1_a(c, j):
        tq_ = 4 * c + j
        src = xa[tq_ % 2]
        xak = "xa%d" % (tq_ % 2)
        rms_rstd(src, [xak], sm[:, 2:3], "rstd1", slot=0, acc=tq_)
        p.op("dve", (lambda src=src: lambda e: e.tensor_scalar(out=xn, in0=src, scalar1=sm[:, 2:3], scalar2=None,
                                                                op0=ALU.mult))(),
             reads=[xak, "rstd1"], writes=["xn"])
        tb, tbk = nb()
        trp = tb.bitcast(BF16)
        for kc in range(8):
            p.op("pe", (lambda kc=kc, trp=trp: lambda e: e.transpose(out=trp[:, kc * 128:(kc + 1) * 128],
                                                            in_=xn[:, kc * 128:(kc + 1) * 128], identity=identb))(),
                 reads=["xn", "identb"], writes=[tbk])
        return (trp, tbk)

    def norm1_b(c, j, tt):
        trp, tbk = tt
        for kc in range(8):
            dst = hT[:, kc, j * 128:(j + 1) * 128]
            if j % 2 == 0:
                p.op("dve", (lambda kc=kc, dst=dst, trp=trp: lambda e: e.tensor_scalar(
                    out=dst, in0=trp[:, kc * 128:(kc + 1) * 128], scalar1=gam1[:, kc:kc + 1],
                    scalar2=sh1[:, kc:kc + 1], op0=ALU.mult, op1=ALU.add))(),
                    reads=[tbk, "gam1", "modT"], writes=["hT_%d_%d" % (j, kc)])
            else:
                p.op("act", (lambda kc=kc, dst=dst, trp=trp: lambda e: e.activation(
                    out=dst, in_=trp[:, kc * 128:(kc + 1) * 128], func=AF.Identity,
                    bias=sh1[:, kc:kc + 1], scale=gam1[:, kc:kc + 1]))(),
                    reads=[tbk, "gam1", "modT"], writes=["hT_%d_%d" % (j, kc)])

    def zero_fill(b):
        p.dma("act", (lambda b=b: lambda e: e.dma_start(out=xs_d[b * BLK:(b + 1) * BLK, :], in_=zeros_d))(), "xszero",
              writes=["xszero_%d" % b])
    XSZK = ["xszero_%d" % b for b in range(NBLK)]

    x_load(0)
    for j in range(4):
        if j + 1 < 4:
            x_load(j + 1)
        norm1_b(0, j, norm1_a(0, j))


    for c in range(8):
        HTK = ["hT_%d_%d" % (j, kc) for j in range(4) for kc in range(8)]
        if c == 0 and cut('p1'):
            return nc
        for b in range(6 * c, min(6 * c + 6, NBLK)):
            zero_fill(b)
        for fc in range(12):
            bk, bkk = nb()
            for kc in range(8):
                p.op("pe", (lambda bk=bk, fc=fc, kc=kc: lambda e: e.matmul(
                    bk, lhsT=w_inb[:, kc, fc * 128:(fc + 1) * 128], rhs=hT[:, kc, :], start=(kc == 0), stop=(kc == 7)))(),
                    reads=HTK + W_INB_KEYS, writes=[bkk])
            if fc < 4:
                p.op("act", (lambda bk=bk, fc=fc: lambda e: e.copy(out=xr[:, fc, 3:515], in_=bk))(),
                     reads=[bkk], writes=["xr%d" % fc])
            elif fc < 8:
                p.op("act", (lambda bk=bk, fc=fc: lambda e: e.activation(out=grg[:, fc - 4, :], in_=bk, func=AF.Gelu_apprx_tanh))(),
                     reads=[bkk], writes=["grg%d" % (fc - 4)])
            else:
                p.op("act", (lambda bk=bk, fc=fc: lambda e: e.activation(out=ug[:, fc - 8, :], in_=bk, func=AF.Gelu_apprx_tanh))(),
                     reads=[bkk], writes=["ug%d" % (fc - 8)])
        if c == 0 and cut('p2'):
            return nc
        vgs = [(xc[0], "xc0"), (xc[1], "xc1"), (hs, "hs"), (sgt, "sgtV")]
        for j in range(4):
            bk, bkk = nb()
            vgj, vgk = vgs[j]
            for kc in range(8):
                p.op("pe", (lambda bk=bk, j=j, kc=kc: lambda e: e.matmul(
                    bk, lhsT=hT[:, kc, j * 128:(j + 1) * 128], rhs=w_inb[:, kc, 1536:2048], start=(kc == 0), stop=(kc == 7)))(),
                    reads=HTK + W_INB_KEYS, writes=[bkk])
            wk = [vgk] if vgk != "sgtV" else ["sgtA", "sgtB", "sgtV"]
            p.op("act", (lambda bk=bk, vgj=vgj: lambda e: e.activation(out=vgj, in_=bk, func=AF.Gelu_apprx_tanh))(),
                 reads=[bkk], writes=wk)
        vsl = []
        for j in range(4):
            o = 64 + 16 * j
            vsl.append((sm2v[:, o:o + 6], sm2v[:, o + 6:o + 8], sm2v[:, o + 8:o + 9], sm2v[:, o + 9:o + 10]))
        for j in range(4):
            vgj, vgk = vgs[j]
            st6, mv, lnv, rsv = vsl[j]
            p.op("dve", (lambda vgj=vgj, st6=st6: lambda e: e.bn_stats(out=st6, in_=vgj))(), reads=[vgk], writes=["bnst%d" % j])
            p.op("dve", (lambda st6=st6, mv=mv: lambda e: e.bn_aggr(out=mv, in_=st6))(), reads=["bnst%d" % j], writes=["mv%d" % j])
        for j in range(4):
            st6, mv, lnv, rsv = vsl[j]
            p.op("act", (lambda mv=mv, lnv=lnv: lambda e: e.activation(out=lnv, in_=mv[:, 1:2], func=AF.Ln, bias=sm[:, 8:9], scale=1.0))(),
                 reads=["mv%d" % j, "epsc"], writes=["vsq%d" % j])
            p.op("act", (lambda lnv=lnv, rsv=rsv: lambda e: e.activation(out=rsv, in_=lnv, func=AF.Exp, scale=-0.5))(),
                 reads=["vsq%d" % j], writes=["vrstd%d" % j])
        for j in range(4):
            vgj, vgk = vgs[j]
            st6, mv, lnv, rsv = vsl[j]
            p.op("dve", (lambda vgj=vgj, mv=mv, rsv=rsv: lambda e: e.tensor_scalar(out=vgj, in0=vgj, scalar1=mv[:, 0:1], scalar2=rsv,
                                                                                 op0=ALU.subtract, op1=ALU.mult))(),
                 reads=[vgk, "mv%d" % j, "vrstd%d" % j], writes=[vgk])
            p.op("dve", (lambda vgj=vgj: lambda e: e.tensor_tensor(out=vgj, in0=vgj, in1=lng_bc, op=ALU.mult))(), reads=[vgk, "lng_bc"], writes=[vgk])
            p.op("pool", (lambda j=j, vgj=vgj: lambda e: e.tensor_tensor(out=vnb[:, j, :], in0=vgj, in1=lnb_bc, op=ALU.add))(),
                 reads=[vgk, "lnb_bc"], writes=["vnb%d" % j])
        if c == 0 and cut('p3'):
            return nc
        for cc in range(4):
            bkA, bkAk = nb()
            bkB, bkBk = nb()
            for j in range(4):
                p.op("pe", (lambda bkA=bkA, j=j, cc=cc: lambda e: e.matmul(
                    bkA[:, j * 128:(j + 1) * 128], lhsT=vnb[:, j, cc * 128:(cc + 1) * 128], rhs=wmT[:, 2 * cc, :],
                    start=True, stop=True))(), reads=["vnb%d" % j, "wmT"], writes=[bkAk])
                p.op("pe", (lambda bkB=bkB, j=j, cc=cc: lambda e: e.matmul(
                    bkB[:, j * 128:(j + 1) * 128], lhsT=vnb[:, j, cc * 128:(cc + 1) * 128], rhs=wmT[:, 2 * cc + 1, :],
                    start=True, stop=True))(), reads=["vnb%d" % j, "wmT"], writes=[bkBk])
            p.op("dve", (lambda bkA=bkA, cc=cc: lambda e: e.tensor_tensor(
                out=sgt[0:64, :], in0=bkA[0:64, :], in1=bsrep[0:64, cc, :], op=ALU.add))(),
                reads=[bkAk, "bsrep"], writes=["sgtA", "sgtV"])
            p.op("dve", (lambda bkB=bkB, cc=cc: lambda e: e.tensor_tensor(
                out=sgt[64:128, :], in0=bkB[64:128, :], in1=bsrep[64:128, cc, :], op=ALU.add))(),
                reads=[bkBk, "bsrep"], writes=["sgtB", "sgtV"])
            p.op("pool", (lambda cc=cc: lambda e: e.tensor_tensor(out=ysT[:, 4 + cc, :], in0=sgt, in1=ug[:, cc, :], op=ALU.mult))(),
                 reads=["sgtA", "sgtB", "ug%d" % cc], writes=["ysT%d" % (4 + cc)])
        if c == 0 and cut('p4'):
            return nc
        def lru_front(cc):
            q = cc % 2
            xrk = "xr%d" % cc
            xcq, xcbq = xc[q], xcb[q]
            p.op("dve", (lambda cc=cc, xcq=xcq: lambda e: e.tensor_scalar(
                out=xcq, in0=xr[:, cc, 0:512], scalar1=cw[:, cc, 0:1], scalar2=cb[:, cc:cc + 1], op0=ALU.mult, op1=ALU.add))(),
                reads=[xrk, "cw", "cb"], writes=["xc%d" % q])
            for k in range(1, 4):
                p.op("dve", (lambda cc=cc, k=k, xcq=xcq: lambda e: e.scalar_tensor_tensor(
                    out=xcq, in0=xr[:, cc, k:k + 512], scalar=cw[:, cc, k:k + 1], in1=xcq, op0=ALU.mult, op1=ALU.add))(),
                    reads=[xrk, "cw", "xc%d" % q], writes=["xc%d" % q])
            p.op("pool", (lambda cc=cc: lambda e: e.tensor_copy(out=xr[:, cc, 0:3], in_=xr[:, cc, 512:515]))(),
                 reads=[xrk], writes=[xrk])
            p.op("dve", (lambda xcq=xcq, xcbq=xcbq: lambda e: e.tensor_copy(out=xcbq, in_=xcq))(), reads=["xc%d" % q], writes=["xcb%d" % q])
            bka, bkak = nb()
            bki, bkik = nb()
            p.op("pe", (lambda bka=bka, cc=cc, xcbq=xcbq: lambda e: e.matmul(bka, lhsT=bda[:, cc, :], rhs=xcbq, start=True, stop=True))(),
                 reads=["bda", "xcb%d" % q], writes=[bkak])
            p.op("pe", (lambda bki=bki, cc=cc, xcbq=xcbq: lambda e: e.matmul(bki, lhsT=bdi[:, cc, :], rhs=xcbq, start=True, stop=True))(),
                 reads=["bdi", "xcb%d" % q], writes=[bkik])
            return (bka, bkak, bki, bkik)

        def lru_act(cc, bks):
            q = cc % 2
            bka, bkak, bki, bkik = bks
            rgq, igq, avq, a2q = rg[q], ig[q], av[q], a2[q]
            p.op("act", (lambda bka=bka, cc=cc, rgq=rgq: lambda e: e.activation(out=rgq, in_=bka, func=AF.Exp, bias=ngab_t[:, cc:cc + 1], scale=-1.0))(),
                 reads=[bkak, "ngab"], writes=["rg%d" % q])
            p.op("act", (lambda bki=bki, cc=cc, igq=igq: lambda e: e.activation(out=igq, in_=bki, func=AF.Exp, bias=ngib_t[:, cc:cc + 1], scale=-1.0))(),
                 reads=[bkik, "ngib"], writes=["ig%d" % q])
            for (buf, k) in ((rgq, "rg%d" % q), (igq, "ig%d" % q)):
                p.op("act", (lambda buf=buf: lambda e: e.activation(out=buf, in_=buf, func=AF.Ln, bias=sm[:, 9:10], scale=1.0))(),
                     reads=[k, "onec"], writes=[k])
                p.op("act", (lambda buf=buf: lambda e: e.activation(out=buf, in_=buf, func=AF.Exp, scale=-1.0))(), reads=[k], writes=[k])
            p.op("act", (lambda cc=cc, rgq=rgq, avq=avq: lambda e: e.activation(out=avq, in_=rgq, func=AF.Exp, scale=cneg[:, cc:cc + 1]))(),
                 reads=["rg%d" % q, "cneg"], writes=["av%d" % q])
            p.op("act", (lambda cc=cc, rgq=rgq, a2q=a2q: lambda e: e.activation(out=a2q, in_=rgq, func=AF.Exp, scale=cneg2[:, cc:cc + 1]))(),
                 reads=["rg%d" % q, "cneg2"], writes=["a2%d" % q])
            p.op("act", (lambda a2q=a2q: lambda e: e.activation(out=a2q, in_=a2q, func=AF.Ln, bias=sm[:, 9:10], scale=-1.0))(),
                 reads=["a2%d" % q, "onec"], writes=["a2%d" % q])
            p.op("act", (lambda a2q=a2q: lambda e: e.activation(out=a2q, in_=a2q, func=AF.Exp, scale=0.5))(), reads=["a2%d" % q], writes=["a2%d" % q])

        def lru_back(cc):
            q = cc % 2
            igq, xcq, a2q, avq = ig[q], xc[q], a2[q], av[q]
            p.op("pool", (lambda igq=igq, xcq=xcq: lambda e: e.tensor_tensor(out=igq, in0=igq, in1=xcq, op=ALU.mult))(),
                 reads=["ig%d" % q, "xc%d" % q], writes=["ig%d" % q])
            p.op("dve", (lambda igq=igq, a2q=a2q: lambda e: e.tensor_tensor(out=igq, in0=igq, in1=a2q, op=ALU.mult))(),
                 reads=["ig%d" % q, "a2%d" % q], writes=["ig%d" % q])
            p.op("dve", (lambda cc=cc, avq=avq, igq=igq: lambda e: e.tensor_tensor_scan(
                out=hs, data0=avq, data1=igq, initial=hstate[:, cc:cc + 1], op0=ALU.mult, op1=ALU.add))(),
                reads=["av%d" % q, "ig%d" % q, "hstate%d" % cc], writes=["hs"])
            p.op("dve", (lambda cc=cc: lambda e: e.tensor_copy(out=hstate[:, cc:cc + 1], in_=hs[:, 511:512]))(),
                 reads=["hs"], writes=["hstate%d" % cc])
            p.op("pool", (lambda cc=cc: lambda e: e.tensor_tensor(out=ysT[:, cc, :], in0=hs, in1=grg[:, cc, :], op=ALU.mult))(),
                 reads=["hs", "grg%d" % cc], writes=["ysT%d" % cc])

        bks_ = {0: lru_front(0)}
        if c + 1 < 8:
            x_load(4 * (c + 1))
        for cc in range(4):
            if cc + 1 < 4:
                bks_[cc + 1] = lru_front(cc + 1)
            if c + 1 < 8:
                if cc + 1 < 4:
                    x_load(4 * (c + 1) + cc + 1)
                tt_ = norm1_a(c + 1, cc)
            lru_act(cc, bks_[cc])
            if c + 1 < 8:
                norm1_b(c + 1, cc, tt_)
            lru_back(cc)
        YSK = ["ysT%d" % k for k in range(8)]
        if c == 0 and cut('p5'):
            return nc
        gmax = sm[:, 32:33]
        ngmax = sm[:, 33:34]
        pg = sm[:, 35:36]
        goh = sm[:, 36:40]
        pen = sm[:, 40:44]
        gex = sm[:, 44:48]
        dd = sm[:, 48:49]
        w1c = sm[:, 49:50]
        w2c = sm[:, 50:51]
        ELK = ["elm%d" % g for g in range(4)]

        def mix_m1(j):
            t = 4 * c + j
            q = t % 2
            x1b = x1t[q]
            x1k = "x1t%d" % q
            xrs = xres[q]
            xrsk = "xres%d" % q
            xn2q = xn2[q]
            xn2k = "xn2_%d" % q
            ld(xrs, x_v[:, t, :], xrsk)
            for nh in range(2):
                bk, bkk = nb()
                for kc in range(8):
                    p.op("pe", (lambda bk=bk, j=j, kc=kc, nh=nh: lambda e: e.matmul(
                        bk, lhsT=ysT[:, kc, j * 128:(j + 1) * 128], rhs=w_outb[:, kc, nh * 512:(nh + 1) * 512],
                        start=(kc == 0), stop=(kc == 7)))(), reads=YSK + W_OUTB_KEYS, writes=[bkk])
                p.op("dve", (lambda bk=bk, nh=nh, x1b=x1b, xrs=xrs: lambda e: e.tensor_tensor(
                    out=x1b[:, nh * 512:(nh + 1) * 512], in0=bk, in1=xrs[:, nh * 512:(nh + 1) * 512], op=ALU.add))(),
                    reads=[bkk, xrsk], writes=[x1k + "_%d" % nh])
            x1keys = [x1k + "_0", x1k + "_1"]
            p.dma("sp", (lambda x1b=x1b, t=t: lambda e: e.dma_start(out=x1_v[:, t, :], in_=x1b))(),
                  "st_x1_%d" % q, reads=x1keys, writes=["x1d_%d" % t])
            rcol = sm[:, 3 + q:4 + q]
            rtag = "rstd2_%d" % q
            rms_rstd(x1b, x1keys, rcol, rtag, slot=1 + q, acc=32 + t)
            p.op("dve", (lambda x1b=x1b, xn2q=xn2q, rcol=rcol: lambda e: e.tensor_scalar(out=xn2q, in0=x1b, scalar1=rcol, scalar2=None,
                                                                                      op0=ALU.mult))(),
                 reads=x1keys + [rtag], writes=[xn2k])

        def mix_m2(j):
            t = 4 * c + j
            q = t % 2
            xn2q = xn2[q]
            xn2k = "xn2_%d" % q
            h2fq = h2f[q]
            tfs = [nb(), nb()]
            for kc in range(8):
                tf, tfk = tfs[kc // 4]
                qq = kc % 4
                p.op("pe", (lambda kc=kc, tf=tf, qq=qq, xn2q=xn2q: lambda e: e.transpose(out=tf[:, qq * 128:(qq + 1) * 128],
                                                                                      in_=xn2q[:, kc * 128:(kc + 1) * 128], identity=ident))(),
                     reads=[xn2k, "ident"], writes=[tfk])
            for kc in range(8):
                tf, tfk = tfs[kc // 4]
                qq = kc % 4
                if kc // 4 == 0:
                    p.op("dve", (lambda kc=kc, tf=tf, qq=qq, h2fq=h2fq: lambda e: e.tensor_scalar(
                        out=h2fq[:, kc, :], in0=tf[:, qq * 128:(qq + 1) * 128], scalar1=gam2[:, kc:kc + 1],
                        scalar2=sh2[:, kc:kc + 1], op0=ALU.mult, op1=ALU.add))(),
                        reads=[tfk, "gam2", "modT"], writes=["h2f%d_%d" % (q, kc)])
                else:
                    p.op("act", (lambda kc=kc, tf=tf, qq=qq, h2fq=h2fq: lambda e: e.activation(
                        out=h2fq[:, kc, :], in_=tf[:, qq * 128:(qq + 1) * 128], func=AF.Identity,
                        bias=sh2[:, kc:kc + 1], scale=gam2[:, kc:kc + 1]))(),
                        reads=[tfk, "gam2", "modT"], writes=["h2f%d_%d" % (q, kc)])
            H2FK = ["h2f%d_%d" % (q, kc) for kc in range(8)]
            hb = h2tm[q]
            hbk = "h2tm%d" % q
            p.op("dve", (lambda xn2q=xn2q: lambda e: e.tensor_tensor(out=xn2q, in0=xn2q, in1=gam2_bc, op=ALU.mult))(),
                 reads=[xn2k, "gam2_bc0", "gam2_bc1"], writes=[xn2k])
            p.op("pool", (lambda hb=hb, xn2q=xn2q: lambda e: e.tensor_tensor(out=hb, in0=xn2q, in1=sh2_bc, op=ALU.add))(),
                 reads=[xn2k, "sh2_bc0", "sh2_bc1"], writes=[hbk])
            p.dma("sp", (lambda hb=hb, t=t: lambda e: e.dma_start(out=h2tm_v[:, t, :], in_=hb))(),
                  "st_h2tm%d" % q, reads=[hbk], writes=["h2tmd_%d" % t])
            bk, bkk = nb()
            for kc in range(8):
                p.op("pe", (lambda bk=bk, kc=kc, h2fq=h2fq: lambda e: e.matmul(bk[:, 0:36], lhsT=h2fq[:, kc, :], rhs=wr_t[:, kc, :],
                                                                              start=(kc == 0), stop=(kc == 7)))(),
                     reads=H2FK + ["wr"], writes=[bkk])
            return (bk, bkk)

        def router_a(j, rb):
            bk, bkk = rb
            p.op("dve", (lambda bk=bk: lambda e: e.tensor_tensor(out=lgt, in0=bk[:, 0:36], in1=br_bc, op=ALU.add))(),
                 reads=[bkk, "br_bc"], writes=["lgt"])
            p.op("dve", lambda e: e.tensor_reduce(out=gmax, in_=lgt[:, 0:4], axis=AX.X, op=ALU.max), reads=["lgt"], writes=["gmax"])
            p.op("dve", lambda e: e.tensor_scalar(out=ngmax, in0=gmax, scalar1=-1.0, scalar2=None, op0=ALU.mult),
                 reads=["gmax"], writes=["ngmax"])
            gsum = ssall[:, 64 + 4 * c + j:65 + 4 * c + j]
            p.op("act", (lambda gsum=gsum: lambda e: e.activation(out=gex, in_=lgt[:, 0:4], func=AF.Exp, bias=ngmax, scale=1.0, accum_out=gsum))(),
                 reads=["lgt", "ngmax", "ssall"], writes=["gex", "gsum"])
            p.op("dve", lambda e: e.tensor_scalar(out=goh, in0=lgt[:, 0:4], scalar1=gmax, scalar2=-1.0, op0=ALU.is_ge, op1=ALU.add),
                 reads=["lgt", "gmax"], writes=["goh"])
            p.op("dve", lambda e: e.tensor_scalar(out=pen, in0=goh, scalar1=1e30, scalar2=None, op0=ALU.mult),
                 reads=["goh"], writes=["pen"])
            for g in range(4):
                p.op("dve", (lambda g=g: lambda e: e.tensor_scalar(out=elm[:, g * 8:(g + 1) * 8], in0=lgt[:, 4 + g * 8:4 + (g + 1) * 8],
                                                                   scalar1=pen[:, g:g + 1], scalar2=None, op0=ALU.add))(),
                     reads=["lgt", "pen"], writes=["elm%d" % g])
            p.op("dve", lambda e: e.max(out=mx8, in_=elm), reads=ELK, writes=["mx8"])
            p.op("dve", lambda e: e.tensor_tensor(out=dd, in0=mx8[:, 0:1], in1=mx8[:, 1:2], op=ALU.subtract), reads=["mx8"], writes=["dd"])
            p.op("act", lambda e: e.activation(out=w1c, in_=dd, func=AF.Exp, scale=-1.0), reads=["dd"], writes=["w1c"])

        def router_b(j):
            t = 4 * c + j
            gsum = ssall[:, 64 + t:65 + t]
            p.op("dve", (lambda gsum=gsum: lambda e: e.reciprocal(out=pg, in_=gsum))(), reads=["gsum"], writes=["pg"])
            p.op("dve", lambda e: e.tensor_scalar(out=w1c, in0=w1c, scalar1=1.0, scalar2=None, op0=ALU.add), reads=["w1c"], writes=["w1c"])
            p.op("dve", lambda e: e.reciprocal(out=w1c, in_=w1c), reads=["w1c"], writes=["w1c"])
            p.op("dve", lambda e: e.tensor_tensor(out=w1c, in0=w1c, in1=pg, op=ALU.mult), reads=["w1c", "pg"], writes=["w1c"])
            p.op("dve", lambda e: e.tensor_tensor(out=w2c, in0=pg, in1=w1c, op=ALU.subtract), reads=["w1c", "pg"], writes=["w2c"])
            p.op("dve", (lambda t=t: lambda e: e.tensor_scalar(out=OH1[:, t, :], in0=elm, scalar1=mx8[:, 0:1], scalar2=None, op0=ALU.is_equal))(),
                 reads=ELK + ["mx8"], writes=["OH1_%d" % t])
            p.op("dve", (lambda t=t: lambda e: e.tensor_scalar(out=OH2[:, t, :], in0=elm, scalar1=mx8[:, 1:2], scalar2=None, op0=ALU.is_equal))(),
                 reads=ELK + ["mx8"], writes=["OH2_%d" % t])
            p.op("dve", (lambda t=t: lambda e: e.tensor_copy(out=W12[:, t, 0:1], in_=w1c))(), reads=["w1c"], writes=["W1_%d" % t])
            p.op("dve", (lambda t=t: lambda e: e.tensor_copy(out=W12[:, t, 1:2], in_=w2c))(), reads=["w2c"], writes=["W2_%d" % t])

        mix_m1(0)
        for j in range(4):
            if j + 1 < 4:
                mix_m1(j + 1)
            if j >= 1:
                router_b(j - 1)
            rb_ = mix_m2(j)
            router_a(j, rb_)
        router_b(3)
        if c == 0 and cut('p6'):
            return nc

    if DEBUG_STOP == "phase1":
        p.wait_all("sp", ["x1d_%d" % t for t in range(NT)] + ["h2tmd_%d" % t for t in range(NT)])
        p.emit()
        return nc

    p.barrier(barw)
    A.top = oh_top
    Ltri = A.bf16(128)
    onesb = A.bf16(128)
    OS = A.bf16(32)
    base = A.f32(32)
    Rall = A.f32(NT, 32)
    nblk = A.f32(32)
    padded = A.f32(32)
    pend = A.f32(32)
    pstart = A.f32(32)
    ones32 = A.f32(32)
    cmpt = A.f32(32)
    tmpd = A.f32(32)
    prod = A.f32(32)
    bef = A.f32(NBLK)
    idxwf = A.f32(NBLK)
    be512 = A.f32(NBLK)
    idx2f = A.f32(4 * NBLK)
    iop_i = A.ar[:, A.top:A.top + 1].bitcast(I32); A.top += 1
    iop_f = A.f32(1)
    destf = A.f32(2 * NT)
    disp_top = A.top

    p.op("pool", lambda e: e.memset(onesb, 1.0), writes=["onesb"])
    p.op("pool", lambda e: e.memset(Ltri, 1.0), writes=["Ltri"])
    p.op("pool", lambda e: e.affine_select(out=Ltri, in_=Ltri, pattern=[[1, 128]], compare_op=ALU.is_gt,
                                           fill=0.0, base=0, channel_multiplier=-1), reads=["Ltri"], writes=["Ltri"])
    p.op("pool", lambda e: e.iota(out=iop_i, pattern=[[0, 1]], base=0, channel_multiplier=1), writes=["iop_i"])
    p.op("pool", lambda e: e.tensor_copy(out=iop_f, in_=iop_i), reads=["iop_i"], writes=["iop_f"])
    p.op("dve", lambda e: e.memset(base, 0.0), writes=["base"])
    p.op("dve", lambda e: e.memset(ones32, 1.0), writes=["ones32"])
    p.op("dve", lambda e: e.memset(epsA, EPS), writes=["epsA"])
    for t in range(NT):
        p.op("dve", (lambda t=t: lambda e: e.tensor_tensor(out=OS, in0=OH1[:, t, :], in1=OH2[:, t, :], op=ALU.add))(),
             reads=["OH1_%d" % t, "OH2_%d" % t], writes=["OS"])
        bk, bkk = nb()
        p.op("pe", (lambda bk=bk: lambda e: e.matmul(bk[:, 0:32], lhsT=onesb, rhs=OS, start=True, stop=True))(),
             reads=["onesb", "OS"], writes=[bkk])
        p.op("pe", (lambda bk=bk: lambda e: e.matmul(bk[:, 32:64], lhsT=Ltri, rhs=OS, start=True, stop=True))(),
             reads=["Ltri", "OS"], writes=[bkk])
        p.op("dve", (lambda bk=bk, t=t: lambda e: e.tensor_tensor(out=Rall[:, t, :], in0=bk[:, 32:64], in1=base, op=ALU.add))(),
             reads=[bkk, "base"], writes=["R_%d" % t])
        p.op("dve", (lambda bk=bk: lambda e: e.tensor_tensor(out=base, in0=bk[:, 0:32], in1=base, op=ALU.add))(),
             reads=[bkk, "base"], writes=["base"])
    p.op("dve", lambda e: e.memset(nblk, 0.0), writes=["nblk"])
    for m in range(17):
        p.op("dve", (lambda m=m: lambda e: e.scalar_tensor_tensor(out=nblk, in0=base, scalar=float(BLK * m), in1=nblk,
                                                                 op0=ALU.is_gt, op1=ALU.add))(), reads=["base", "nblk"], writes=["nblk"])
    p.op("dve", lambda e: e.tensor_scalar(out=padded, in0=nblk, scalar1=float(BLK), scalar2=None, op0=ALU.mult),
         reads=["nblk"], writes=["padded"])
    p.op("dve", lambda e: e.tensor_tensor_scan(out=pend, data0=ones32, data1=padded, initial=0.0, op0=ALU.mult, op1=ALU.add),
         reads=["ones32", "padded"], writes=["pend"])
    p.op("dve", lambda e: e.tensor_tensor(out=pstart, in0=pend, in1=padded, op=ALU.subtract), reads=["pend", "padded"], writes=["pstart"])
    for b in range(NBLK):
        p.op("dve", (lambda b=b: lambda e: e.tensor_scalar(out=cmpt, in0=pend, scalar1=float(b * BLK), scalar2=0.0,
                                                          op0=ALU.is_le, op1=ALU.add, accum_out=bef[:, b:b + 1]))(),
             reads=["pend"], writes=["cmpt", "bef%d" % b])
    BEK = ["bef%d" % b for b in range(NBLK)]
    p.op("dve", lambda e: e.tensor_scalar(out=idxwf, in0=bef, scalar1=31.0, scalar2=128.0, op0=ALU.min, op1=ALU.mult),
         reads=BEK, writes=["idxwf"])
    p.op("dve", lambda e: e.tensor_scalar(out=idxwf, in0=idxwf, scalar1=iop_f[:, 0:1], scalar2=None, op0=ALU.add),
         reads=["idxwf", "iop_f"], writes=["idxwf"])
    p.op("dve", lambda e: e.tensor_copy(out=idxwi, in_=idxwf), reads=["idxwf"], writes=["idxwi"])
    p.op("dve", lambda e: e.tensor_scalar(out=be512, in0=bef, scalar1=31.0, scalar2=512.0, op0=ALU.min, op1=ALU.mult),
         reads=BEK, writes=["be512"])
    p.op("dve", lambda e: e.tensor_scalar(out=be512, in0=be512, scalar1=iop_f[:, 0:1], scalar2=None, op0=ALU.add),
         reads=["be512", "iop_f"], writes=["be512"])
    idx2f3 = idx2f.rearrange("p (b h) -> p b h", h=4)
    for hc in range(4):
        p.op("dve", (lambda hc=hc: lambda e: e.tensor_scalar(out=idx2f3[:, :, hc], in0=be512, scalar1=float(hc * 128), scalar2=None, op0=ALU.add))(),
             reads=["be512"], writes=["idx2f_%d" % hc])
    p.op("dve", lambda e: e.tensor_copy(out=idx2i, in_=idx2f), reads=["idx2f_%d" % hc for hc in range(4)], writes=["idx2i"])
    NSTG = 6
    stg2 = [A.ar[:, NW - (NSTG - i) * 4096:NW - (NSTG - i - 1) * 4096] for i in range(NSTG)]
    ew1_r = ew1.rearrange("e (p k) n -> (e p) (k n)", p=128)
    ew3_r = ew3.rearrange("e (p k) n -> (e p) (k n)", p=128)
    ew2_r = ew2.rearrange("e h n -> (e h) n")

    def gather_block_weights(b):
        sset = b % 2
        for m, src in enumerate((ew1_r, ew3_r)):
            st = stg2[3 * sset + m]
            sk = "stg2_%d_%d" % (sset, m)
            p.dma("pool", (lambda st=st, src=src, b=b: lambda e: e.indirect_dma_start(
                out=st, out_offset=None, in_=src, in_offset=bass.IndirectOffsetOnAxis(ap=idxwi[:, b:b + 1], axis=0)))(),
                "ld_" + sk, reads=["idxwi"], writes=[sk])
        st = stg2[3 * sset + 2]
        for hc in range(4):
            sk = "stg2_%d_2_%d" % (sset, hc)
            p.dma("pool", (lambda st=st, b=b, hc=hc: lambda e: e.indirect_dma_start(
                out=st[:, hc * 1024:(hc + 1) * 1024], out_offset=None, in_=ew2_r,
                in_offset=bass.IndirectOffsetOnAxis(ap=idx2i[:, 4 * b + hc:4 * b + hc + 1], axis=0)))(),
                "ld_" + sk, reads=["idx2i"], writes=[sk])

    gather_block_weights(0)
    gather_block_weights(1)
    for t in range(NT):
        p.op("dve", (lambda t=t: lambda e: e.tensor_tensor(out=tmpd, in0=Rall[:, t, :], in1=pstart, op=ALU.add))(),
             reads=["R_%d" % t, "pstart"], writes=["tmpd"])
        for k, OH in enumerate((OH1, OH2)):
            p.op("dve", (lambda t=t, OH=OH: lambda e: e.tensor_tensor(out=prod, in0=tmpd, in1=OH[:, t, :], op=ALU.mult))(),
                 reads=["tmpd", "OH%d_%d" % (k + 1, t)], writes=["prod"])
            p.op("dve", (lambda t=t, k=k: lambda e: e.tensor_reduce(out=destf[:, 2 * t + k:2 * t + k + 1], in_=prod, axis=AX.X, op=ALU.add))(),
                 reads=["prod"], writes=["destf_%d_%d" % (t, k)])
    DFK = ["destf_%d_%d" % (t, k) for t in range(NT) for k in range(2)]
    p.op("dve", lambda e: e.tensor_copy(out=desti, in_=destf), reads=DFK, writes=["desti"])

    hsc = [A.bf16(1024) for _ in range(8)]
    for t in range(NT):
        hb = hsc[t % 8]
        hbk = "hsc%d" % (t % 8)
        p.dma("sp", (lambda hb=hb, t=t: lambda e: e.dma_start(out=hb, in_=h2tm_v[:, t, :]))(), "ld_" + hbk,
              reads=["h2tmd_%d" % t], writes=[hbk])
        for k in range(2):
            p.dma("pool", (lambda hb=hb, t=t, k=k: lambda e: e.indirect_dma_start(
                out=xs_d, out_offset=bass.IndirectOffsetOnAxis(ap=desti[:, 2 * t + k:2 * t + k + 1], axis=0),
                in_=hb, in_offset=None))(), "sc_%d_%d" % (t % 8, k), reads=[hbk, "desti"] + XSZK, writes=["xs_sc_%d_%d" % (t, k)])
    XSK = ["xs_sc_%d_%d" % (t, k) for t in range(NT) for k in range(2)]

    p.barrier(barw)
    A.top = const_top
    wb = [[A.bf16(8, 512), A.bf16(8, 512), A.bf16(4, 1024)] for _ in range(2)]
    xs_sb = [A.bf16(4, 1024) for _ in range(1)]
    xsT = [A.bf16(8, 512) for _ in range(2)]
    gT = [A.bf16(4, 512) for _ in range(2)]
    s1 = [A.f32(512) for _ in range(1)]
    ysb = [A.f32(1024) for _ in range(4)]
    print("stage C arena top", A.top, "of", NW - NSTG * 4096)
    assert A.top <= NW - NSTG * 4096
    xs_v = xs_d.rearrange("(b s p) d -> b p s d", p=128, s=4)
    yb_v = ybuf_d.rearrange("(b s p) d -> b s p d", p=128, s=4)

    def cast_block_weights(b):
        sset = b % 2
        wbuf = wb[b % 2]
        st0 = stg2[3 * sset].rearrange("p (k n) -> p k n", k=8)
        st1 = stg2[3 * sset + 1].rearrange("p (k n) -> p k n", k=8)
        st2 = stg2[3 * sset + 2].rearrange("p (k n) -> p k n", k=4)
        p.op("dve", (lambda st0=st0, wbuf=wbuf: lambda e: e.tensor_copy(out=wbuf[0], in_=st0))(),
             reads=["stg2_%d_0" % sset], writes=["wb%d_0" % (b % 2)])
        p.op("act", (lambda st1=st1, wbuf=wbuf: lambda e: e.copy(out=wbuf[1], in_=st1))(),
             reads=["stg2_%d_1" % sset], writes=["wb%d_1" % (b % 2)])
        p.op("dve", (lambda st2=st2, wbuf=wbuf: lambda e: e.tensor_copy(out=wbuf[2], in_=st2))(),
             reads=["stg2_%d_2_%d" % (sset, hc) for hc in range(4)], writes=["wb%d_2" % (b % 2)])

    def load_xs(b):
        xb_ = xs_sb[0]
        p.dma("sp", (lambda xb_=xb_, b=b: lambda e: e.dma_start(out=xb_, in_=xs_v[b]))(), "ld_xs_sb0",
              reads=XSK, writes=["xs_sb0"])

    def transposes(b):
        xb_ = xs_sb[0]
        xbk = "xs_sb0"
        xT = xsT[b % 2]
        for st_ in range(4):
            tb, tbk = nb()
            tpv = tb.bitcast(BF16)
            xv = xb_[:, st_, :].rearrange("s (p k) -> s k p", k=8)
            for kc in range(8):
                p.op("pe", (lambda tpv=tpv, xv=xv, kc=kc: lambda e: e.transpose(out=tpv[:, kc * 128:(kc + 1) * 128], in_=xv[:, kc, :],
                                                                                identity=identb))(),
                     reads=[xbk, "identb"], writes=[tbk])
            src3 = tpv.rearrange("p (k s) -> p k s", k=8)
            dst3 = xT[:, :, st_ * 128:(st_ + 1) * 128]
            xTk = "xsT%d_%d" % (b % 2, st_)
            if st_ % 2 == 0:
                p.op("act", (lambda src3=src3, dst3=dst3: lambda e: e.copy(out=dst3, in_=src3))(), reads=[tbk], writes=[xTk])
            else:
                p.op("dve", (lambda src3=src3, dst3=dst3: lambda e: e.tensor_copy(out=dst3, in_=src3))(), reads=[tbk], writes=[xTk])

    load_xs(0)
    cast_block_weights(0)
    transposes(0)
    load_xs(1)
    for b in range(NBLK):
        wbuf = wb[b % 2]
        WK = ["wb%d_%d" % (b % 2, m) for m in range(3)]
        xT = xsT[b % 2]
        XTK = ["xsT%d_%d" % (b % 2, st_) for st_ in range(4)]
        gTb = gT[b % 2]
        for hc in range(4):
            b1, b1k = nb()
            b3, b3k = nb()
            for kc in range(8):
                p.op("pe", (lambda b1=b1, wbuf=wbuf, kc=kc, hc=hc, xT=xT: lambda e: e.matmul(
                    b1, lhsT=wbuf[0][:, kc, hc * 128:(hc + 1) * 128], rhs=xT[:, kc, :], start=(kc == 0), stop=(kc == 7)))(),
                    reads=XTK + [WK[0]], writes=[b1k])
            for kc in range(8):
                p.op("pe", (lambda b3=b3, wbuf=wbuf, kc=kc, hc=hc, xT=xT: lambda e: e.matmul(
                    b3, lhsT=wbuf[1][:, kc, hc * 128:(hc + 1) * 128], rhs=xT[:, kc, :], start=(kc == 0), stop=(kc == 7)))(),
                    reads=XTK + [WK[1]], writes=[b3k])
            s1b = s1[0]
            s1k = "s1_0"
            p.op("act", (lambda b1=b1, s1b=s1b: lambda e: e.activation(out=s1b, in_=b1, func=AF.Silu))(), reads=[b1k], writes=[s1k])
            p.op("dve", (lambda b3=b3, s1b=s1b, gTb=gTb, hc=hc: lambda e: e.tensor_tensor(out=gTb[:, hc, :], in0=b3, in1=s1b, op=ALU.mult))(),
                 reads=[b3k, s1k], writes=["gT%d_%d" % (b % 2, hc)])
        if b + 1 < NBLK:
            transposes(b + 1)
            if b + 2 < NBLK:
                load_xs(b + 2)
            cast_block_weights(b + 1)
        if b + 2 < NBLK:
            gather_block_weights(b + 2)
        GK = ["gT%d_%d" % (b % 2, hc) for hc in range(4)]
        for st_ in range(4):
            yi = st_
            yb_ = ysb[yi]
            for dh in range(2):
                by, byk = nb()
                for hc in range(4):
                    p.op("pe", (lambda by=by, gTb=gTb, hc=hc, st_=st_, dh=dh, wbuf=wbuf: lambda e: e.matmul(
                        by, lhsT=gTb[:, hc, st_ * 128:(st_ + 1) * 128], rhs=wbuf[2][:, hc, dh * 512:(dh + 1) * 512],
                        start=(hc == 0), stop=(hc == 3)))(), reads=GK + [WK[2]], writes=[byk])
                if dh == 0:
                    p.op("act", (lambda by=by, yb_=yb_: lambda e: e.copy(out=yb_[:, 0:512], in_=by))(), reads=[byk], writes=["ysb%d_0" % yi])
                else:
                    p.op("dve", (lambda by=by, yb_=yb_: lambda e: e.tensor_copy(out=yb_[:, 512:1024], in_=by))(), reads=[byk], writes=["ysb%d_1" % yi])
            p.dma("sp", (lambda yb_=yb_, b=b, st_=st_: lambda e: e.dma_start(out=yb_v[b, st_], in_=yb_))(), "st_ysb%d" % yi,
                  reads=["ysb%d_0" % yi, "ysb%d_1" % yi], writes=["ybuf_%d_%d" % (b, st_)])
    YBK = ["ybuf_%d_%d" % (b, st_) for b in range(NBLK) for st_ in range(4)]

    p.barrier(barw)
    A.top = const_top
    x1r = [A.f32(1024) for _ in range(4)]
    Y1 = [A.f32(1024) for _ in range(4)]
    Y2 = [A.f32(1024) for _ in range(4)]
    tcm = [A.f32(1024) for _ in range(4)]
    oo = [A.f32(1024) for _ in range(4)]
    junk2 = A.bf16(1024)
    sm2 = A.f32(16)
    ssD = A.f32(NT)
    p.op("pool", lambda e: e.memset(ssD, 0.0), writes=["ssD"])

    def d_a(t):
        i2 = t % 4
        p.dma("sp", (lambda t=t, i2=i2: lambda e: e.dma_start(out=x1r[i2], in_=x1_v[:, t, :]))(), ["ld_xa0", "ld_xa1", "ld_xres0", "ld_xres1"][i2],
              reads=["x1d_%d" % t], writes=["x1r%d" % i2])
        for k, Yb in enumerate((Y1, Y2)):
            p.dma("pool", (lambda t=t, k=k, Yb=Yb, i2=i2: lambda e: e.indirect_dma_start(
                out=Yb[i2], out_offset=None, in_=ybuf_d,
                in_offset=bass.IndirectOffsetOnAxis(ap=desti[:, 2 * t + k:2 * t + k + 1], axis=0)))(),
                "ld_stg2_%d_2_%d" % (k, i2), reads=YBK + ["desti"], writes=["Y%d_%d" % (k, i2)])
        p.op("act", (lambda t=t, i2=i2: lambda e: e.activation(out=tcm[i2], in_=Y1[i2], func=AF.Copy, scale=W12[:, t, 0:1]))(),
             reads=["Y0_%d" % i2, "W1_%d" % t], writes=["tcm%d" % i2])

    def d_b(t):
        i2 = t % 4
        tk = "tcm%d" % i2
        p.op("dve", (lambda t=t, i2=i2: lambda e: e.scalar_tensor_tensor(out=tcm[i2], in0=Y2[i2], scalar=W12[:, t, 1:2], in1=tcm[i2],
                                                                         op0=ALU.mult, op1=ALU.add))(),
             reads=["Y1_%d" % i2, "W2_%d" % t, tk], writes=[tk])
        p.op("dve", (lambda i2=i2: lambda e: e.tensor_tensor(out=tcm[i2], in0=tcm[i2], in1=g2_bc, op=ALU.mult))(), reads=[tk, "g2_bc0", "g2_bc1"], writes=[tk])
        p.op("dve", (lambda i2=i2: lambda e: e.tensor_tensor(out=tcm[i2], in0=tcm[i2], in1=x1r[i2], op=ALU.add))(),
             reads=[tk, "x1r%d" % i2], writes=[tk])

    def d_c(t):
        i2 = t % 4
        tk = "tcm%d" % i2
        ssf = ssD[:, t:t + 1]
        lnc = sm2[:, 2 * i2:2 * i2 + 1]
        rsc = sm2[:, 2 * i2 + 1:2 * i2 + 2]
        p.op("act", (lambda ssf=ssf, i2=i2: lambda e: e.activation(out=junk2, in_=tcm[i2], func=AF.Square, accum_out=ssf))(),
             reads=[tk, "ssD"], writes=["junk2", "ssf%d" % i2])
        p.op("act", (lambda ssf=ssf, lnc=lnc: lambda e: e.activation(out=lnc, in_=ssf, func=AF.Ln, bias=epsA[:, 0:1], scale=1.0 / D))(),
             reads=["ssf%d" % i2, "epsA"], writes=["sqf%d" % i2])
        p.op("act", (lambda lnc=lnc, rsc=rsc: lambda e: e.activation(out=rsc, in_=lnc, func=AF.Exp, scale=-0.5))(), reads=["sqf%d" % i2], writes=["rstdf%d" % i2])

    def d_d(t):
        i2 = t % 4
        rsc = sm2[:, 2 * i2 + 1:2 * i2 + 2]
        p.op("dve", (lambda i2=i2, rsc=rsc: lambda e: e.scalar_tensor_tensor(out=oo[i2], in0=tcm[i2], scalar=rsc, in1=fg_bc, op0=ALU.mult, op1=ALU.mult))(),
             reads=["tcm%d" % i2, "rstdf%d" % i2, "fg_bc"], writes=["oo%d" % i2])
        p.dma("sp", (lambda t=t, i2=i2: lambda e: e.dma_start(out=out_v[:, t, :], in_=oo[i2]))(), "st_ysb%d" % i2,
              reads=["oo%d" % i2], writes=["outd_%d" % t])

    d_a(0)
    for t in range(NT):
        if t + 1 < NT:
            d_a(t + 1)
        d_b(t)
        d_c(t)
        if t >= 1:
            d_d(t - 1)
    d_d(NT - 1)

    p.wait_all("sp", ["outd_%d" % t for t in range(NT)])
    p.emit()
    return nc


_NC_CACHE = {}


def _prep_inputs(inp, b):
    f = np.float32

    def colz(v, n):
        return np.ascontiguousarray(np.asarray(v, f).reshape(n, 128).T)

    m = {}
    m["x"] = np.ascontiguousarray(inp["x"][b])
    m["c_col"] = colz(inp["c"][b], 8)
    m["ada_w"] = np.ascontiguousarray(inp["ada_w"][0])
    m["ada_b"] = colz(inp["ada_b"][0], 48)
    m["adab_bc"] = np.ascontiguousarray(np.broadcast_to(np.asarray(inp["ada_b"][0], f)[None, :], (128, 6 * D)))
    m["n2g_bc"] = np.ascontiguousarray(np.broadcast_to(np.asarray(inp["norm2_g"][0], f)[None, :], (128, D)))
    m["n1g"] = colz(inp["norm1_g"][0], 8)
    m["n2g"] = colz(inp["norm2_g"][0], 8)
    m["fg_bc"] = np.ascontiguousarray(np.broadcast_to(np.asarray(inp["final_g"], f)[None, :], (128, D)))
    m["w_in"] = np.ascontiguousarray(inp["w_in"][0])
    m["w_out"] = np.ascontiguousarray(inp["w_out"][0])
    cwv = np.asarray(inp["conv_w"][0], f)
    m["conv_w"] = np.ascontiguousarray(cwv.T.reshape(4, 128, 4).transpose(1, 0, 2))
    m["conv_b"] = colz(inp["conv_b"][0], 4)
    m["gaw"] = np.ascontiguousarray(inp["gate_a_w"][0])
    m["giw"] = np.ascontiguousarray(inp["gate_i_w"][0])
    m["gab"] = colz(inp["gate_a_b"][0], 4)
    m["gib"] = colz(inp["gate_i_b"][0], 4)
    m["lam"] = colz(inp["lru_lambda"][0], 4)
    m["lng_bc"] = np.ascontiguousarray(np.broadcast_to(np.asarray(inp["sgu_ln_g"][0], f)[None, :], (128, 512)))
    m["lnb_bc"] = np.ascontiguousarray(np.broadcast_to(np.asarray(inp["sgu_ln_b"][0], f)[None, :], (128, 512)))
    m["sgu_wT"] = np.ascontiguousarray(np.asarray(inp["sgu_w"][0], f).transpose(2, 0, 1))
    bs = np.asarray(inp["sgu_b"][0], f)
    bsr = np.repeat(bs, 64, axis=0)
    bsr = np.tile(bsr, (1, 4))
    m["bsrep"] = np.ascontiguousarray(bsr.reshape(4, 128, 512).transpose(1, 0, 2))
    wr = np.concatenate([np.asarray(inp["router_group_w"][0], f), np.asarray(inp["router_expert_w"][0], f)], axis=1)
    m["wr"] = np.ascontiguousarray(wr.reshape(8, 128, 36).transpose(1, 0, 2))
    br = np.concatenate([np.asarray(inp["router_group_b"][0], f), np.asarray(inp["router_expert_b"][0], f)])
    m["br_bc"] = np.ascontiguousarray(np.broadcast_to(br[None, :], (128, 36)))
    m["zeros_blk"] = np.zeros((512, D), dtype=ml_dtypes.bfloat16)
    m["ew1"] = np.ascontiguousarray(inp["expert_w1"][0])
    m["ew3"] = np.ascontiguousarray(inp["expert_w3"][0])
    m["ew2"] = np.ascontiguousarray(inp["expert_w2"][0])
    return m


def kernel(**inputs):
    inp = {k: np.asarray(v) for k, v in inputs.items()}
    if "nc" not in _NC_CACHE:
        _NC_CACHE["nc"] = build_nc()
    nc = _NC_CACHE["nc"]
    in_maps = [_prep_inputs(inp, b) for b in range(8)]
    res = run_bass_kernel_spmd(nc, in_maps, core_ids=list(range(8)))
    _NC_CACHE["last"] = res
    outs = [np.asarray(res.results[b]["out"]).reshape(S, D) for b in range(8)]
    return np.stack(outs, axis=0).astype(np.float32)
```

```python
import contextlib
import numpy as np
import ml_dtypes
import concourse.bass as bass
import concourse.mybir as mybir
from concourse.bass_utils import run_bass_kernel_spmd

F32 = mybir.dt.float32
BF16 = mybir.dt.bfloat16
I32 = mybir.dt.int32
ALU = mybir.AluOpType
AF = mybir.ActivationFunctionType
AX = mybir.AxisListType

D = 1024
S = 4096
NT = S // 128
NE = 32
DH = 512
EPS = 1e-6
DEBUG_STOP = None
import os
KCUT = os.environ.get('KCUT', '')


class Prog:
    ENG = ("pe", "act", "dve", "pool", "sp")

    def __init__(self, nc):
        self.nc = nc
        self.es = contextlib.ExitStack()
        self.ops = {e: [] for e in self.ENG}
        self.cnt = {e: 0 for e in self.ENG}
        self.esem = {e: self.es.enter_context(nc.semaphore("s_" + e)) for e in self.ENG}
        self.seen = {e: {} for e in self.ENG}
        self.dsem = {}
        self.state = {}
        self.nsem = 0

    def sb(self, name, shape, dt):
        return self.es.enter_context(self.nc.sbuf_tensor(name, list(shape), dt))

    def ps(self, name, shape, dt):
        return self.es.enter_context(self.nc.psum_tensor(name, list(shape), dt))

    def _st(self, k):
        s = self.state.get(k)
        if s is None:
            s = self.state[k] = {"w": [], "r": []}
        return s

    def _deps(self, eng, reads, writes):
        need = []
        for k in reads:
            if k.startswith("bank"):
                s = self._st(k)
                need += [(ev, True) for ev in s["w"] + s["r"]]
            else:
                need += [(ev, False) for ev in self._st(k)["w"]]
        for k in writes:
            s = self._st(k)
            isp = k.startswith("bank")
            need += [(ev, isp) for ev in s["w"] + s["r"]]
        waits = {}
        for ((sem_id, sem, val, src_eng), isp) in need:
            if src_eng == eng and (isp or eng in ("pe", "sp")):
                continue
            if self.seen[eng].get(sem_id, 0) >= val:
                continue
            if waits.get(sem_id, (None, 0))[1] < val:
                waits[sem_id] = (sem, val)
        for sem_id, (sem, val) in waits.items():
            self.seen[eng][sem_id] = val
        return list(waits.values())

    def _commit(self, ev, reads, writes):
        for k in reads:
            if k.startswith("bank"):
                s = self._st(k)
                s["w"] = [ev]
                s["r"] = []
            else:
                self._st(k)["r"].append(ev)
        for k in writes:
            s = self._st(k)
            s["w"] = [ev]
            s["r"] = []

    def op(self, eng, fn, reads=(), writes=()):
        waits = self._deps(eng, reads, writes)
        self.cnt[eng] += 1
        val = self.cnt[eng]
        sem = self.esem[eng]
        self.ops[eng].append((waits, fn, sem, 1))
        ev = ("e_" + eng, sem, val, eng)
        self._commit(ev, reads, writes)
        return ev

    def dma(self, q, fn, semkey, reads=(), writes=()):
        waits = self._deps(q, reads, writes)
        ent = self.dsem.get(semkey)
        if ent is None:
            sem = self.es.enter_context(self.nc.semaphore("d%d" % self.nsem))
            self.nsem += 1
            ent = self.dsem[semkey] = [sem, 0]
        ent[1] += 16
        self.ops[q].append((waits, fn, ent[0], 16))
        ev = ("d_" + str(semkey), ent[0], ent[1], "dma")
        self._commit(ev, reads, writes)
        return ev

    def barrier(self, scratch):
        keys = list(self.state.keys())
        self.op("pool", lambda e: e.memset(scratch, 0.0), reads=keys, writes=keys + ["__bar"])
        for eng in ("pe", "act", "dve", "sp"):
            waits = self._deps(eng, ["__bar"], ())
            self.ops[eng].append((waits, None, None, 0))

    def wait_all(self, eng, keys):
        waits = self._deps(eng, keys, ())
        self.ops[eng].append((waits, None, None, 0))

    def emit(self):
        nc = self.nc
        with nc.Block() as block:
            def run(e):
                def body(h):
                    for (waits, fn, sem, inc) in self.ops[e]:
                        for (s, v) in waits:
                            h.wait_ge(s, v)
                        if fn is not None:
                            fn(h).then_inc(sem, inc)
                return body
            block.tensor(run("pe"))
            block.scalar(run("act"))
            block.vector(run("dve"))
            block.gpsimd(run("pool"))
            block.sync(run("sp"))
        self.es.close()


class Arena:
    def __init__(self, ar, nwords):
        self.ar = ar
        self.n = nwords
        self.top = 0
        self.gen = 0

    def _view(self, v, shape):
        if len(shape) == 2:
            v = v.rearrange("p (a b) -> p a b", a=shape[0])
        elif len(shape) == 3:
            v = v.rearrange("p (a b c) -> p a b c", a=shape[0], b=shape[1])
        return v

    def f32(self, *shape):
        n = int(np.prod(shape))
        a = self.top
        self.top += n
        assert self.top <= self.n, ("arena overflow", self.top, self.n)
        return self._view(self.ar[:, a:a + n], shape)

    def bf16(self, *shape):
        n = int(np.prod(shape))
        w = (n + 1) // 2
        a = self.top
        self.top += w
        assert self.top <= self.n, ("arena overflow", self.top, self.n)
        return self._view(self.ar[:, a:a + w].bitcast(BF16), shape)


def build_nc():
    nc = bass.Bass("TRN2", target_bir_lowering=False)

    def din(name, shape, dt=F32):
        return nc.dram_tensor(name, list(shape), dt, kind="ExternalInput").ap()

    x = din("x", [S, D])
    c_col = din("c_col", [128, 8])
    ada_w = din("ada_w", [D, 6 * D])
    ada_b = din("ada_b", [128, 48])
    adab_bc_d = din("adab_bc", [128, 6 * D])
    n2g_bc_d = din("n2g_bc", [128, D])
    n1g = din("n1g", [128, 8])
    n2g = din("n2g", [128, 8])
    fg_bc_d = din("fg_bc", [128, D])
    w_in = din("w_in", [D, 2048])
    w_out = din("w_out", [D, D])
    conv_w = din("conv_w", [128, 4, 4])
    conv_b = din("conv_b", [128, 4])
    gaw = din("gaw", [8, 64, 64])
    giw = din("giw", [8, 64, 64])
    gab = din("gab", [128, 4])
    gib = din("gib", [128, 4])
    lam = din("lam", [128, 4])
    lng_bc_d = din("lng_bc", [128, 512])
    lnb_bc_d = din("lnb_bc", [128, 512])
    sgu_wT = din("sgu_wT", [128, 8, 128])
    bsrep_d = din("bsrep", [128, 4, 512])
    wr_d = din("wr", [128, 8, 36])
    br_bc_d = din("br_bc", [128, 36])
    zeros_d = din("zeros_blk", [512, D], BF16)
    ew1 = din("ew1", [NE, D, DH])
    ew3 = din("ew3", [NE, D, DH])
    ew2 = din("ew2", [NE, DH, D])
    out = nc.dram_tensor("out", [S, D], F32, kind="ExternalOutput").ap()
    x1_d = nc.dram_tensor("x1_scr", [S, D], F32, kind="ExternalOutput" if DEBUG_STOP else "Internal").ap()
    h2tm_d = nc.dram_tensor("h2tm_scr", [S, D], BF16, kind="Internal").ap()
    NBLK = 47
    BLK = 512
    xs_d = nc.dram_tensor("xs_scr", [NBLK * BLK, D], BF16, kind="Internal").ap()
    ybuf_d = nc.dram_tensor("ybuf_scr", [NBLK * BLK, D], F32, kind="Internal").ap()

    p = Prog(nc)
    NW = 53200
    ar = p.sb("arena", [128, NW], F32)
    A = Arena(ar, NW)
    banks = [p.ps("bank%d" % i, [128, 512], F32) for i in range(8)]
    bank_i = [0]

    def nb():
        i = bank_i[0] % 8
        bank_i[0] += 1
        return banks[i][:], "bank%d" % i

    dcount = [0]

    def ld(dst, src, key, reads=(), q="sp"):
        dcount[0] += 1
        p.dma(q, lambda e: e.dma_start(out=dst, in_=src), "ld_" + key, reads=list(reads), writes=[key])

    ident = A.f32(128)
    ones = A.f32(128)
    identb = A.bf16(128)
    modT = A.f32(48)
    gam1 = A.f32(8)
    gam2 = A.f32(8)
    n1g_t = A.f32(8)
    n2g_t = A.f32(8)
    adab_t = A.f32(48)
    ccol = A.f32(8)
    cond = A.f32(8)
    g2_bc = A.f32(1024)
    fg_bc = A.f32(1024)
    W12 = A.f32(NT, 2)
    br_bc = A.f32(36)
    wr_t = A.f32(8, 36)
    barw = A.f32(1)
    NBLK_ = 47
    idxwi = A.ar[:, A.top:A.top + NBLK_].bitcast(I32); A.top += NBLK_
    desti = A.ar[:, A.top:A.top + 2 * NT].bitcast(I32); A.top += 2 * NT
    idx2i = A.ar[:, A.top:A.top + 4 * NBLK_].bitcast(I32); A.top += 4 * NBLK_
    epsA = A.f32(1)
    const_top = A.top
    OH1 = A.bf16(NT, 32)
    OH2 = A.bf16(NT, 32)
    oh_top = A.top
    gam2_bc = A.f32(1024)
    sh2_bc = A.f32(1024)
    w_inb = A.bf16(8, 2048)
    w_outb = A.bf16(8, 1024)
    bda = A.bf16(4, 128)
    bdi = A.bf16(4, 128)
    wmT = A.bf16(8, 128)
    bsrep = A.f32(4, 512)
    lng_bc = A.f32(512)
    lnb_bc = A.f32(512)
    cw = A.f32(4, 4)
    cb = A.f32(4)
    gab_t = A.f32(4)
    gib_t = A.f32(4)
    lam_t = A.f32(4)
    cneg = A.f32(4)
    cneg2 = A.f32(4)
    lt0 = A.f32(4)
    lt1 = A.f32(4)
    hstate = A.f32(4)
    ngab_t = A.f32(4)
    ngib_t = A.f32(4)
    work_base = A.top

    p.op("pool", lambda e: e.memset(ident, 1.0), writes=["ident"])
    p.op("pool", lambda e: e.affine_select(out=ident, in_=ident, pattern=[[-1, 128]], compare_op=ALU.is_equal,
                                           fill=0.0, base=0, channel_multiplier=1), reads=["ident"], writes=["ident"])
    p.op("pool", lambda e: e.tensor_copy(out=identb, in_=ident), reads=["ident"], writes=["identb"])
    p.op("pool", lambda e: e.memset(ones, 1.0), writes=["ones"])

    ld(ccol, c_col, "ccol")
    ld(adab_t, ada_b, "adab")
    ld(n1g_t, n1g, "n1g")
    ld(n2g_t, n2g, "n2g")
    ld(fg_bc, fg_bc_d, "fg_bc")
    ld(br_bc, br_bc_d, "br_bc")
    ld(wr_t, wr_d, "wr")


    def cut(label):
        if KCUT == label:
            p.barrier(barw)
            p.dma("sp", lambda e: e.dma_start(out=x1_d[0:128, 0:48], in_=modT), "dbgc", reads=["__bar"], writes=["dbgc"])
            p.wait_all("sp", ["dbgc"])
            p.emit()
            return True
        return False

    if cut('c0'):
        return nc
    ld(bsrep, bsrep_d, "bsrep")
    ld(lng_bc, lng_bc_d, "lng_bc")
    ld(lnb_bc, lnb_bc_d, "lnb_bc")
    ld(cw, conv_w, "cw")
    ld(cb, conv_b, "cb")
    ld(gab_t, gab, "gab")
    ld(gib_t, gib, "gib")
    ld(lam_t, lam, "lam")
    st1 = A.f32(2, 1024)
    bdst = st1[:, 0, :].rearrange("p (g c m) -> p g c m", g=2, c=4)
    p.op("pool", lambda e: e.memset(st1[:, 0, :], 0.0), reads=[], writes=["misc0", "misc1"])
    for gi, gw in enumerate((gaw, giw)):
        for cc in range(4):
            for hh in range(2):
                p.dma("sp", (lambda gi=gi, gw=gw, cc=cc, hh=hh: lambda e: e.dma_start(
                    out=bdst[hh * 64:(hh + 1) * 64, gi, cc, hh * 64:(hh + 1) * 64], in_=gw[2 * cc + hh]))(),
                    "ld_bd%d_%d_%d" % (gi, cc, hh), reads=["misc0"], writes=["bdst%d_%d_%d" % (gi, cc, hh)])
    BDK = ["bdst%d_%d_%d" % (gi, cc, hh) for gi in range(2) for cc in range(4) for hh in range(2)]
    p.op("pool", lambda e: e.tensor_copy(out=bda, in_=bdst[:, 0, :, :]), reads=BDK, writes=["bda"])
    p.op("pool", lambda e: e.tensor_copy(out=bdi, in_=bdst[:, 1, :, :]), reads=BDK, writes=["bdi"])
    if cut('c4'):
        return nc
    wsg = st1[:, 1, :].rearrange("p (g i) -> p g i", g=8)
    p.dma("sp", lambda e: e.dma_start(out=wsg, in_=sgu_wT), "ld_wsg", reads=["misc1"], writes=["wsg"])
    p.op("pool", lambda e: e.memset(wsg[64:128, :, 0:64], 0.0), reads=["wsg"], writes=["wsg"])
    p.op("pool", lambda e: e.tensor_copy(out=wmT, in_=wsg), reads=["wsg"], writes=["wmT"])
    if cut('c5'):
        return nc
    p.op("act", lambda e: e.activation(out=lt0, in_=lam_t, func=AF.Exp, scale=-1.0), reads=["lam"], writes=["lt0"])
    p.op("dve", lambda e: e.tensor_scalar(out=lt1, in0=lt0, scalar1=-0.25, scalar2=1.0 / 3.0, op0=ALU.mult, op1=ALU.add),
         reads=["lt0"], writes=["lt1"])
    p.op("dve", lambda e: e.tensor_tensor(out=lt1, in0=lt1, in1=lt0, op=ALU.mult), reads=["lt1", "lt0"], writes=["lt1"])
    p.op("dve", lambda e: e.tensor_scalar(out=lt1, in0=lt1, scalar1=-0.5, scalar2=None, op0=ALU.add), reads=["lt1"], writes=["lt1"])
    p.op("dve", lambda e: e.tensor_tensor(out=lt1, in0=lt1, in1=lt0, op=ALU.mult), reads=["lt1", "lt0"], writes=["lt1"])
    p.op("dve", lambda e: e.tensor_scalar(out=lt1, in0=lt1, scalar1=1.0, scalar2=None, op0=ALU.add), reads=["lt1"], writes=["lt1"])
    p.op("dve", lambda e: e.tensor_tensor(out=lt1, in0=lt1, in1=lt0, op=ALU.mult), reads=["lt1", "lt0"], writes=["lt1"])
    p.op("dve", lambda e: e.tensor_scalar(out=cneg, in0=lt1, scalar1=-8.0, scalar2=None, op0=ALU.mult), reads=["lt1"], writes=["cneg"])
    p.op("dve", lambda e: e.tensor_scalar(out=cneg2, in0=lt1, scalar1=-16.0, scalar2=None, op0=ALU.mult), reads=["lt1"], writes=["cneg2"])
    p.op("dve", lambda e: e.memset(hstate, 0.0), writes=["hstate%d" % cc for cc in range(4)])
    p.op("dve", lambda e: e.tensor_scalar(out=ngab_t, in0=gab_t, scalar1=-1.0, scalar2=None, op0=ALU.mult), reads=["gab"], writes=["ngab"])
    p.op("dve", lambda e: e.tensor_scalar(out=ngib_t, in0=gib_t, scalar1=-1.0, scalar2=None, op0=ALU.mult), reads=["gib"], writes=["ngib"])
    p.op("act", lambda e: e.activation(out=cond, in_=ccol, func=AF.Silu), reads=["ccol"], writes=["cond"])
    ada_stg = [A.f32(8, 1024) for _ in range(2)]
    diag = A.f32(128)
    g1_bc = A.f32(1024)
    accm = A.f32(1024)
    abb = A.f32(1024)
    rowt = A.f32(1024)
    n2gb = A.f32(1024)
    ada_v = ada_w.rearrange("(kc p) n -> p kc n", p=128)
    ld(n2gb, n2g_bc_d, "n2gb")
    row_dst = {2: (g1_bc, "g1_bc"), 3: (sh2_bc, "sh2_bc"), 5: (g2_bc, "g2_bc")}
    condM = A.f32(4, 128)
    for q in range(4):
        p.op("dve", (lambda q=q: lambda e: e.tensor_scalar(out=condM[:, q, :], in0=ones, scalar1=cond[:, 4 + q:5 + q], scalar2=None, op0=ALU.mult))(),
             reads=["ones", "cond"], writes=["condM%d" % q])
    CMK = ["condM%d" % q for q in range(4)]
    wst = [A.f32(4, 512) for _ in range(2)]
    w_in_v = w_in.rearrange("(kc p) n -> p kc n", p=128)

    def w_in_piece(i):
        pc, hf = i // 2, i % 2
        buf = wst[i % 2]
        bk_ = "wst%d" % (i % 2)
        p.dma("sp", (lambda buf=buf, pc=pc, hf=hf: lambda e: e.dma_start(out=buf, in_=w_in_v[:, hf * 4:(hf + 1) * 4, pc * 512:(pc + 1) * 512]))(),
              "ld_" + bk_, writes=[bk_])
        for q in range(4):
            kc = hf * 4 + q
            p.op("pool", (lambda buf=buf, q=q, kc=kc, pc=pc: lambda e: e.tensor_copy(out=w_inb[:, kc, pc * 512:(pc + 1) * 512], in_=buf[:, q, :]))(),
                 reads=[bk_], writes=["w_inb_%d_%d" % (pc, kc)])

    w_out_v = w_out.rearrange("(kc p) n -> p kc n", p=128)

    def w_out_piece(i):
        hf, nh = i // 2, i % 2
        buf = wst[i % 2]
        bk_ = "wst%d" % (i % 2)
        p.dma("sp", (lambda buf=buf, hf=hf, nh=nh: lambda e: e.dma_start(out=buf, in_=w_out_v[:, hf * 4:(hf + 1) * 4, nh * 512:(nh + 1) * 512]))(),
              "ld_" + bk_, writes=[bk_])
        for q in range(4):
            kc = hf * 4 + q
            p.op("pool", (lambda buf=buf, q=q, kc=kc, nh=nh: lambda e: e.tensor_tensor(
                out=w_outb[:, kc, nh * 512:(nh + 1) * 512], in0=buf[:, q, :], in1=g1_bc[:, nh * 512:(nh + 1) * 512], op=ALU.mult))(),
                reads=[bk_, "g1_bc%d" % nh], writes=["w_outb_%d_%d" % (kc, nh)])

    for s in range(6):
        st = ada_stg[s % 2]
        sk = "adastg%d" % (s % 2)
        for hf in range(2):
            ld(st[:, hf * 4:(hf + 1) * 4, :], ada_v[:, hf * 4:(hf + 1) * 4, s * 1024:(s + 1) * 1024], sk + "_%d" % hf)
        ld(abb, adab_bc_d[:, s * 1024:(s + 1) * 1024], "abb")
        if s < 4:
            w_in_piece(2 * s)
            w_in_piece(2 * s + 1)
        else:
            w_out_piece(2 * (s - 4))
            w_out_piece(2 * (s - 4) + 1)
        p.op("dve", (lambda st=st: lambda e: e.tensor_scalar(out=accm, in0=st[:, 0, :], scalar1=cond[:, 0:1], scalar2=None, op0=ALU.mult))(),
             reads=[sk + "_0", "cond"], writes=["accm"])
        for kc in range(1, 4):
            p.op("dve", (lambda st=st, kc=kc: lambda e: e.scalar_tensor_tensor(out=accm, in0=st[:, kc, :], scalar=cond[:, kc:kc + 1], in1=accm,
                                                                              op0=ALU.mult, op1=ALU.add))(),
                 reads=[sk + "_0", "cond", "accm"], writes=["accm"])
        dst, dkey = row_dst.get(s, (rowt, "rowt"))
        for half in range(2):
            bk, bkk = nb()
            for q in range(4):
                p.op("pe", (lambda bk=bk, half=half, q=q, st=st: lambda e: e.matmul(
                    bk, lhsT=condM[:, q, :], rhs=st[:, 4 + q, half * 512:(half + 1) * 512], start=(q == 0), stop=False))(),
                    reads=CMK + [sk + "_1"], writes=[bkk])
            p.op("pe", (lambda bk=bk, half=half: lambda e: e.matmul(bk, lhsT=ones, rhs=accm[:, half * 512:(half + 1) * 512], start=False, stop=True))(),
                 reads=["ones", "accm"], writes=[bkk])
            p.op("dve", (lambda bk=bk, half=half, dst=dst: lambda e: e.tensor_tensor(
                out=dst[:, half * 512:(half + 1) * 512], in0=bk, in1=abb[:, half * 512:(half + 1) * 512], op=ALU.add))(),
                reads=[bkk, "abb"], writes=[dkey + "%d" % half])
        if s in (0, 1, 3, 4):
            for kc in range(8):
                p.op("dve", (lambda dst=dst, kc=kc: lambda e: e.tensor_tensor(out=diag, in0=dst[:, kc * 128:(kc + 1) * 128], in1=ident, op=ALU.mult))(),
                     reads=[dkey + "0", dkey + "1", "ident"], writes=["diag"])
                p.op("dve", (lambda s=s, kc=kc: lambda e: e.tensor_reduce(out=modT[:, s * 8 + kc:s * 8 + kc + 1], in_=diag, axis=AX.X, op=ALU.add))(),
                     reads=["diag"], writes=["modT_%d_%d" % (s, kc)])
        if s == 4:
            p.op("dve", lambda e: e.scalar_tensor_tensor(out=gam2_bc, in0=rowt, scalar=1.0, in1=n2gb, op0=ALU.add, op1=ALU.mult),
                 reads=["rowt0", "rowt1", "n2gb"], writes=["gam2_bc0", "gam2_bc1"])
    MODK = ["modT_%d_%d" % (s, kc) for s in (0, 1, 3, 4) for kc in range(8)]
    p.op("dve", lambda e: e.tensor_copy(out=modT[:, 16:17], in_=modT[:, 0:1]), reads=MODK, writes=["modT"])
    p.op("dve", lambda e: e.scalar_tensor_tensor(out=gam1, in0=modT[:, 8:16], scalar=1.0, in1=n1g_t,
                                                 op0=ALU.add, op1=ALU.mult), reads=["modT", "n1g"], writes=["gam1"])
    p.op("dve", lambda e: e.scalar_tensor_tensor(out=gam2, in0=modT[:, 32:40], scalar=1.0, in1=n2g_t,
                                                 op0=ALU.add, op1=ALU.mult), reads=["modT", "n2g"], writes=["gam2"])
    if cut('c1'):
        return nc
    sh1 = modT[:, 0:8]
    sh2 = modT[:, 24:32]

    if cut('c2'):
        return nc
    stg = [ada_stg[0], ada_stg[1]]


    W_INB_KEYS = ["w_inb_%d_%d" % (pc, kc) for pc in range(4) for kc in range(8)]
    W_OUTB_KEYS = ["w_outb_%d_%d" % (kc, nh) for kc in range(8) for nh in range(2)]
    if cut('c3'):
        return nc

    if DEBUG_STOP == "setup":
        p.dma("sp", lambda e: e.dma_start(out=x1_d[0:128, 0:48], in_=modT), "dbg0", reads=["modT"], writes=["dbg0"])
        p.dma("sp", lambda e: e.dma_start(out=x1_d[128:256, :], in_=g2_bc), "dbg1", reads=["g2_bc0", "g2_bc1"], writes=["dbg1"])
        p.dma("sp", lambda e: e.dma_start(out=x1_d[256:384, 0:4], in_=cneg), "dbg2", reads=["cneg"], writes=["dbg2"])
        p.op("pool", lambda e: e.tensor_copy(out=g1_bc, in_=w_outb[:, 0, :]), reads=W_OUTB_KEYS + ["g1_bc0", "g1_bc1"], writes=["g1x"])
        p.dma("sp", lambda e: e.dma_start(out=x1_d[384:512, :], in_=g1_bc), "dbg3", reads=["g1x"], writes=["dbg3"])
        p.barrier(barw)
        p.wait_all("sp", ["dbg0", "dbg1", "dbg2", "dbg3"])
        p.emit()
        return nc
    p.barrier(barw)
    A.top = work_base
    xa = [A.f32(1024) for _ in range(2)]
    xres = [A.f32(1024) for _ in range(2)]
    hT = A.bf16(8, 512)
    xr = A.f32(4, 515)
    grg = A.f32(4, 512)
    ug = A.f32(4, 512)
    vnb = A.bf16(4, 512)
    ysT = A.bf16(8, 512)
    xc = [A.f32(512) for _ in range(2)]
    xcb = [A.bf16(512) for _ in range(2)]
    rg = [A.f32(512) for _ in range(2)]
    ig = [A.f32(512) for _ in range(2)]
    av = [A.f32(512) for _ in range(2)]
    a2 = [A.f32(512) for _ in range(2)]
    hs = A.f32(512)
    sgt = A.f32(512)
    xn = A.bf16(1024)
    junk = xn
    x1t = [A.f32(1024) for _ in range(2)]
    xn2 = [A.f32(1024) for _ in range(2)]
    h2f = [A.f32(8, 128) for _ in range(2)]
    h2tm = [A.bf16(1024) for _ in range(2)]
    sm = A.f32(128)
    sm2v = sm
    lgt = A.f32(36)
    elm = A.f32(32)
    mx8 = A.f32(8)
    ssall = A.f32(96)
    print("phase1 arena top", A.top, "of", NW)

    p.op("pool", lambda e: e.memset(xr[:, :, 0:3], 0.0), writes=["xr%d" % cc for cc in range(4)])

    def rms_rstd(src, skey, dst_col, tag, slot=0, acc=0):
        ss = ssall[:, acc:acc + 1]
        sq = sm[:, 11 + 2 * slot:12 + 2 * slot]
        ssk = "ssacc%d" % acc
        p.op("act", lambda e: e.activation(out=junk, in_=src, func=AF.Square, accum_out=ss),
             reads=list(skey) + ["ssall"], writes=["xn", ssk])
        p.op("act", lambda e: e.activation(out=sq, in_=ss, func=AF.Ln, bias=sm[:, 8:9], scale=1.0 / D),
             reads=[ssk, "epsc"], writes=["sq%d" % slot])
        p.op("act", lambda e: e.activation(out=dst_col, in_=sq, func=AF.Exp, scale=-0.5), reads=["sq%d" % slot], writes=[tag])

    p.op("pool", lambda e: e.memset(ssall, 0.0), writes=["ssall"])
    p.op("pool", lambda e: e.memset(sm[:, 8:9], EPS), writes=["epsc"])
    p.op("pool", lambda e: e.memset(sm[:, 9:10], 1.0), writes=["onec"])

    x_v = x.rearrange("(t p) d -> p t d", p=128)
    out_v = out.rearrange("(t p) d -> p t d", p=128)
    x1_v = x1_d.rearrange("(t p) d -> p t d", p=128)
    h2tm_v = h2tm_d.rearrange("(t p) d -> p t d", p=128)

    def x_load(tq_):
        ld(xa[tq_ % 2], x_v[:, tq_, :], "xa%d" % (tq_ % 2))

    def norm1_a(c, j):
        tq_ = 4 * c + j
        src = xa[tq_ % 2]
        xak = "xa%d" % (tq_ % 2)
        rms_rstd(src, [xak], sm[:, 2:3], "rstd1", slot=0, acc=tq_)
        p.op("dve", (lambda src=src: lambda e: e.tensor_scalar(out=xn, in0=src, scalar1=sm[:, 2:3], scalar2=None,
                                                                op0=ALU.mult))(),
             reads=[xak, "rstd1"], writes=["xn"])
        tb, tbk = nb()
        trp = tb.bitcast(BF16)
        for kc in range(8):
            p.op("pe", (lambda kc=kc, trp=trp: lambda e: e.transpose(out=trp[:, kc * 128:(kc + 1) * 128],
                                                            in_=xn[:, kc * 128:(kc + 1) * 128], identity=identb))(),
                 reads=["xn", "identb"], writes=[tbk])
        return (trp, tbk)

    def norm1_b(c, j, tt):
        trp, tbk = tt
        for kc in range(8):
            dst = hT[:, kc, j * 128:(j + 1) * 128]
            if j % 2 == 0:
                p.op("dve", (lambda kc=kc, dst=dst, trp=trp: lambda e: e.tensor_scalar(
                    out=dst, in0=trp[:, kc * 128:(kc + 1) * 128], scalar1=gam1[:, kc:kc + 1],
                    scalar2=sh1[:, kc:kc + 1], op0=ALU.mult, op1=ALU.add))(),
                    reads=[tbk, "gam1", "modT"], writes=["hT_%d_%d" % (j, kc)])
            else:
                p.op("act", (lambda kc=kc, dst=dst, trp=trp: lambda e: e.activation(
                    out=dst, in_=trp[:, kc * 128:(kc + 1) * 128], func=AF.Identity,
                    bias=sh1[:, kc:kc + 1], scale=gam1[:, kc:kc + 1]))(),
                    reads=[tbk, "gam1", "modT"], writes=["hT_%d_%d" % (j, kc)])

    def zero_fill(b):
        p.dma("act", (lambda b=b: lambda e: e.dma_start(out=xs_d[b * BLK:(b + 1) * BLK, :], in_=zeros_d))(), "xszero",
              writes=["xszero_%d" % b])
    XSZK = ["xszero_%d" % b for b in range(NBLK)]

    x_load(0)
    for j in range(4):
        if j + 1 < 4:
            x_load(j + 1)
        norm1_b(0, j, norm1_a(0, j))


    for c in range(8):
        HTK = ["hT_%d_%d" % (j, kc) for j in range(4) for kc in range(8)]
        if c == 0 and cut('p1'):
            return nc
        for b in range(6 * c, min(6 * c + 6, NBLK)):
            zero_fill(b)
        for fc in range(12):
            bk, bkk = nb()
            for kc in range(8):
                p.op("pe", (lambda bk=bk, fc=fc, kc=kc: lambda e: e.matmul(
                    bk, lhsT=w_inb[:, kc, fc * 128:(fc + 1) * 128], rhs=hT[:, kc, :], start=(kc == 0), stop=(kc == 7)))(),
                    reads=HTK + W_INB_KEYS, writes=[bkk])
            if fc < 4:
                p.op("act", (lambda bk=bk, fc=fc: lambda e: e.copy(out=xr[:, fc, 3:515], in_=bk))(),
                     reads=[bkk], writes=["xr%d" % fc])
            elif fc < 8:
                p.op("act", (lambda bk=bk, fc=fc: lambda e: e.activation(out=grg[:, fc - 4, :], in_=bk, func=AF.Gelu_apprx_tanh))(),
                     reads=[bkk], writes=["grg%d" % (fc - 4)])
            else:
                p.op("act", (lambda bk=bk, fc=fc: lambda e: e.activation(out=ug[:, fc - 8, :], in_=bk, func=AF.Gelu_apprx_tanh))(),
                     reads=[bkk], writes=["ug%d" % (fc - 8)])
        if c == 0 and cut('p2'):
            return nc
        vgs = [(xc[0], "xc0"), (xc[1], "xc1"), (hs, "hs"), (sgt, "sgtV")]
        for j in range(4):
            bk, bkk = nb()
            vgj, vgk = vgs[j]
            for kc in range(8):
                p.op("pe", (lambda bk=bk, j=j, kc=kc: lambda e: e.matmul(
                    bk, lhsT=hT[:, kc, j * 128:(j + 1) * 128], rhs=w_inb[:, kc, 1536:2048], start=(kc == 0), stop=(kc == 7)))(),
                    reads=HTK + W_INB_KEYS, writes=[bkk])
            wk = [vgk] if vgk != "sgtV" else ["sgtA", "sgtB", "sgtV"]
            p.op("act", (lambda bk=bk, vgj=vgj: lambda e: e.activation(out=vgj, in_=bk, func=AF.Gelu_apprx_tanh))(),
                 reads=[bkk], writes=wk)
        vsl = []
        for j in range(4):
            o = 64 + 16 * j
            vsl.append((sm2v[:, o:o + 6], sm2v[:, o + 6:o + 8], sm2v[:, o + 8:o + 9], sm2v[:, o + 9:o + 10]))
        for j in range(4):
            vgj, vgk = vgs[j]
            st6, mv, lnv, rsv = vsl[j]
            p.op("dve", (lambda vgj=vgj, st6=st6: lambda e: e.bn_stats(out=st6, in_=vgj))(), reads=[vgk], writes=["bnst%d" % j])
            p.op("dve", (lambda st6=st6, mv=mv: lambda e: e.bn_aggr(out=mv, in_=st6))(), reads=["bnst%d" % j], writes=["mv%d" % j])
        for j in range(4):
            st6, mv, lnv, rsv = vsl[j]
            p.op("act", (lambda mv=mv, lnv=lnv: lambda e: e.activation(out=lnv, in_=mv[:, 1:2], func=AF.Ln, bias=sm[:, 8:9], scale=1.0))(),
                 reads=["mv%d" % j, "epsc"], writes=["vsq%d" % j])
            p.op("act", (lambda lnv=lnv, rsv=rsv: lambda e: e.activation(out=rsv, in_=lnv, func=AF.Exp, scale=-0.5))(),
                 reads=["vsq%d" % j], writes=["vrstd%d" % j])
        for j in range(4):
            vgj, vgk = vgs[j]
            st6, mv, lnv, rsv = vsl[j]
            p.op("dve", (lambda vgj=vgj, mv=mv, rsv=rsv: lambda e: e.tensor_scalar(out=vgj, in0=vgj, scalar1=mv[:, 0:1], scalar2=rsv,
                                                                                 op0=ALU.subtract, op1=ALU.mult))(),
                 reads=[vgk, "mv%d" % j, "vrstd%d" % j], writes=[vgk])
            p.op("dve", (lambda vgj=vgj: lambda e: e.tensor_tensor(out=vgj, in0=vgj, in1=lng_bc, op=ALU.mult))(), reads=[vgk, "lng_bc"], writes=[vgk])
            p.op("pool", (lambda j=j, vgj=vgj: lambda e: e.tensor_tensor(out=vnb[:, j, :], in0=vgj, in1=lnb_bc, op=ALU.add))(),
                 reads=[vgk, "lnb_bc"], writes=["vnb%d" % j])
        if c == 0 and cut('p3'):
            return nc
        for cc in range(4):
            bkA, bkAk = nb()
            bkB, bkBk = nb()
            for j in range(4):
                p.op("pe", (lambda bkA=bkA, j=j, cc=cc: lambda e: e.matmul(
                    bkA[:, j * 128:(j + 1) * 128], lhsT=vnb[:, j, cc * 128:(cc + 1) * 128], rhs=wmT[:, 2 * cc, :],
                    start=True, stop=True))(), reads=["vnb%d" % j, "wmT"], writes=[bkAk])
                p.op("pe", (lambda bkB=bkB, j=j, cc=cc: lambda e: e.matmul(
                    bkB[:, j * 128:(j + 1) * 128], lhsT=vnb[:, j, cc * 128:(cc + 1) * 128], rhs=wmT[:, 2 * cc + 1, :],
                    start=True, stop=True))(), reads=["vnb%d" % j, "wmT"], writes=[bkBk])
            p.op("dve", (lambda bkA=bkA, cc=cc: lambda e: e.tensor_tensor(
                out=sgt[0:64, :], in0=bkA[0:64, :], in1=bsrep[0:64, cc, :], op=ALU.add))(),
                reads=[bkAk, "bsrep"], writes=["sgtA", "sgtV"])
            p.op("dve", (lambda bkB=bkB, cc=cc: lambda e: e.tensor_tensor(
                out=sgt[64:128, :], in0=bkB[64:128, :], in1=bsrep[64:128, cc, :], op=ALU.add))(),
                reads=[bkBk, "bsrep"], writes=["sgtB", "sgtV"])
            p.op("pool", (lambda cc=cc: lambda e: e.tensor_tensor(out=ysT[:, 4 + cc, :], in0=sgt, in1=ug[:, cc, :], op=ALU.mult))(),
                 reads=["sgtA", "sgtB", "ug%d" % cc], writes=["ysT%d" % (4 + cc)])
        if c == 0 and cut('p4'):
            return nc
        def lru_front(cc):
            q = cc % 2
            xrk = "xr%d" % cc
            xcq, xcbq = xc[q], xcb[q]
            p.op("dve", (lambda cc=cc, xcq=xcq: lambda e: e.tensor_scalar(
                out=xcq, in0=xr[:, cc, 0:512], scalar1=cw[:, cc, 0:1], scalar2=cb[:, cc:cc + 1], op0=ALU.mult, op1=ALU.add))(),
                reads=[xrk, "cw", "cb"], writes=["xc%d" % q])
            for k in range(1, 4):
                p.op("dve", (lambda cc=cc, k=k, xcq=xcq: lambda e: e.scalar_tensor_tensor(
                    out=xcq, in0=xr[:, cc, k:k + 512], scalar=cw[:, cc, k:k + 1], in1=xcq, op0=ALU.mult, op1=ALU.add))(),
                    reads=[xrk, "cw", "xc%d" % q], writes=["xc%d" % q])
            p.op("pool", (lambda cc=cc: lambda e: e.tensor_copy(out=xr[:, cc, 0:3], in_=xr[:, cc, 512:515]))(),
                 reads=[xrk], writes=[xrk])
            p.op("dve", (lambda xcq=xcq, xcbq=xcbq: lambda e: e.tensor_copy(out=xcbq, in_=xcq))(), reads=["xc%d" % q], writes=["xcb%d" % q])
            bka, bkak = nb()
            bki, bkik = nb()
            p.op("pe", (lambda bka=bka, cc=cc, xcbq=xcbq: lambda e: e.matmul(bka, lhsT=bda[:, cc, :], rhs=xcbq, start=True, stop=True))(),
                 reads=["bda", "xcb%d" % q], writes=[bkak])
            p.op("pe", (lambda bki=bki, cc=cc, xcbq=xcbq: lambda e: e.matmul(bki, lhsT=bdi[:, cc, :], rhs=xcbq, start=True, stop=True))(),
                 reads=["bdi", "xcb%d" % q], writes=[bkik])
            return (bka, bkak, bki, bkik)

        def lru_act(cc, bks):
            q = cc % 2
            bka, bkak, bki, bkik = bks
            rgq, igq, avq, a2q = rg[q], ig[q], av[q], a2[q]
            p.op("act", (lambda bka=bka, cc=cc, rgq=rgq: lambda e: e.activation(out=rgq, in_=bka, func=AF.Exp, bias=ngab_t[:, cc:cc + 1], scale=-1.0))(),
                 reads=[bkak, "ngab"], writes=["rg%d" % q])
            p.op("act", (lambda bki=bki, cc=cc, igq=igq: lambda e: e.activation(out=igq, in_=bki, func=AF.Exp, bias=ngib_t[:, cc:cc + 1], scale=-1.0))(),
                 reads=[bkik, "ngib"], writes=["ig%d" % q])
            for (buf, k) in ((rgq, "rg%d" % q), (igq, "ig%d" % q)):
                p.op("act", (lambda buf=buf: lambda e: e.activation(out=buf, in_=buf, func=AF.Ln, bias=sm[:, 9:10], scale=1.0))(),
                     reads=[k, "onec"], writes=[k])
                p.op("act", (lambda buf=buf: lambda e: e.activation(out=buf, in_=buf, func=AF.Exp, scale=-1.0))(), reads=[k], writes=[k])
            p.op("act", (lambda cc=cc, rgq=rgq, avq=avq: lambda e: e.activation(out=avq, in_=rgq, func=AF.Exp, scale=cneg[:, cc:cc + 1]))(),
                 reads=["rg%d" % q, "cneg"], writes=["av%d" % q])
            p.op("act", (lambda cc=cc, rgq=rgq, a2q=a2q: lambda e: e.activation(out=a2q, in_=rgq, func=AF.Exp, scale=cneg2[:, cc:cc + 1]))(),
                 reads=["rg%d" % q, "cneg2"], writes=["a2%d" % q])
            p.op("act", (lambda a2q=a2q: lambda e: e.activation(out=a2q, in_=a2q, func=AF.Ln, bias=sm[:, 9:10], scale=-1.0))(),
                 reads=["a2%d" % q, "onec"], writes=["a2%d" % q])
            p.op("act", (lambda a2q=a2q: lambda e: e.activation(out=a2q, in_=a2q, func=AF.Exp, scale=0.5))(), reads=["a2%d" % q], writes=["a2%d" % q])

        def lru_back(cc):
            q = cc % 2
            igq, xcq, a2q, avq = ig[q], xc[q], a2[q], av[q]
            p.op("pool", (lambda igq=igq, xcq=xcq: lambda e: e.tensor_tensor(out=igq, in0=igq, in1=xcq, op=ALU.mult))(),
                 reads=["ig%d" % q, "xc%d" % q], writes=["ig%d" % q])
            p.op("dve", (lambda igq=igq, a2q=a2q: lambda e: e.tensor_tensor(out=igq, in0=igq, in1=a2q, op=ALU.mult))(),
                 reads=["ig%d" % q, "a2%d" % q], writes=["ig%d" % q])
            p.op("dve", (lambda cc=cc, avq=avq, igq=igq: lambda e: e.tensor_tensor_scan(
                out=hs, data0=avq, data1=igq, initial=hstate[:, cc:cc + 1], op0=ALU.mult, op1=ALU.add))(),
                reads=["av%d" % q, "ig%d" % q, "hstate%d" % cc], writes=["hs"])
            p.op("dve", (lambda cc=cc: lambda e: e.tensor_copy(out=hstate[:, cc:cc + 1], in_=hs[:, 511:512]))(),
                 reads=["hs"], writes=["hstate%d" % cc])
            p.op("pool", (lambda cc=cc: lambda e: e.tensor_tensor(out=ysT[:, cc, :], in0=hs, in1=grg[:, cc, :], op=ALU.mult))(),
                 reads=["hs", "grg%d" % cc], writes=["ysT%d" % cc])

        bks_ = {0: lru_front(0)}
        if c + 1 < 8:
            x_load(4 * (c + 1))
        for cc in range(4):
            if cc + 1 < 4:
                bks_[cc + 1] = lru_front(cc + 1)
            if c + 1 < 8:
                if cc + 1 < 4:
                    x_load(4 * (c + 1) + cc + 1)
                tt_ = norm1_a(c + 1, cc)
            lru_act(cc, bks_[cc])
            if c + 1 < 8:
                norm1_b(c + 1, cc, tt_)
            lru_back(cc)
        YSK = ["ysT%d" % k for k in range(8)]
        if c == 0 and cut('p5'):
            return nc
        gmax = sm[:, 32:33]
        ngmax = sm[:, 33:34]
        pg = sm[:, 35:36]
        goh = sm[:, 36:40]
        pen = sm[:, 40:44]
        gex = sm[:, 44:48]
        dd = sm[:, 48:49]
        w1c = sm[:, 49:50]
        w2c = sm[:, 50:51]
        ELK = ["elm%d" % g for g in range(4)]

        def mix_m1(j):
            t = 4 * c + j
            q = t % 2
            x1b = x1t[q]
            x1k = "x1t%d" % q
            xrs = xres[q]
            xrsk = "xres%d" % q
            xn2q = xn2[q]
            xn2k = "xn2_%d" % q
            ld(xrs, x_v[:, t, :], xrsk)
            for nh in range(2):
                bk, bkk = nb()
                for kc in range(8):
                    p.op("pe", (lambda bk=bk, j=j, kc=kc, nh=nh: lambda e: e.matmul(
                        bk, lhsT=ysT[:, kc, j * 128:(j + 1) * 128], rhs=w_outb[:, kc, nh * 512:(nh + 1) * 512],
                        start=(kc == 0), stop=(kc == 7)))(), reads=YSK + W_OUTB_KEYS, writes=[bkk])
                p.op("dve", (lambda bk=bk, nh=nh, x1b=x1b, xrs=xrs: lambda e: e.tensor_tensor(
                    out=x1b[:, nh * 512:(nh + 1) * 512], in0=bk, in1=xrs[:, nh * 512:(nh + 1) * 512], op=ALU.add))(),
                    reads=[bkk, xrsk], writes=[x1k + "_%d" % nh])
            x1keys = [x1k + "_0", x1k + "_1"]
            p.dma("sp", (lambda x1b=x1b, t=t: lambda e: e.dma_start(out=x1_v[:, t, :], in_=x1b))(),
                  "st_x1_%d" % q, reads=x1keys, writes=["x1d_%d" % t])
            rcol = sm[:, 3 + q:4 + q]
            rtag = "rstd2_%d" % q
            rms_rstd(x1b, x1keys, rcol, rtag, slot=1 + q, acc=32 + t)
            p.op("dve", (lambda x1b=x1b, xn2q=xn2q, rcol=rcol: lambda e: e.tensor_scalar(out=xn2q, in0=x1b, scalar1=rcol, scalar2=None,
                                                                                      op0=ALU.mult))(),
                 reads=x1keys + [rtag], writes=[xn2k])

        def mix_m2(j):
            t = 4 * c + j
            q = t % 2
            xn2q = xn2[q]
            xn2k = "xn2_%d" % q
            h2fq = h2f[q]
            tfs = [nb(), nb()]
            for kc in range(8):
                tf, tfk = tfs[kc // 4]
                qq = kc % 4
                p.op("pe", (lambda kc=kc, tf=tf, qq=qq, xn2q=xn2q: lambda e: e.transpose(out=tf[:, qq * 128:(qq + 1) * 128],
                                                                                      in_=xn2q[:, kc * 128:(kc + 1) * 128], identity=ident))(),
                     reads=[xn2k, "ident"], writes=[tfk])
            for kc in range(8):
                tf, tfk = tfs[kc // 4]
                qq = kc % 4
                if kc // 4 == 0:
                    p.op("dve", (lambda kc=kc, tf=tf, qq=qq, h2fq=h2fq: lambda e: e.tensor_scalar(
                        out=h2fq[:, kc, :], in0=tf[:, qq * 128:(qq + 1) * 128], scalar1=gam2[:, kc:kc + 1],
                        scalar2=sh2[:, kc:kc + 1], op0=ALU.mult, op1=ALU.add))(),
                        reads=[tfk, "gam2", "modT"], writes=["h2f%d_%d" % (q, kc)])
                else:
                    p.op("act", (lambda kc=kc, tf=tf, qq=qq, h2fq=h2fq: lambda e: e.activation(
                        out=h2fq[:, kc, :], in_=tf[:, qq * 128:(qq + 1) * 128], func=AF.Identity,
                        bias=sh2[:, kc:kc + 1], scale=gam2[:, kc:kc + 1]))(),
                        reads=[tfk, "gam2", "modT"], writes=["h2f%d_%d" % (q, kc)])
            H2FK = ["h2f%d_%d" % (q, kc) for kc in range(8)]
            hb = h2tm[q]
            hbk = "h2tm%d" % q
            p.op("dve", (lambda xn2q=xn2q: lambda e: e.tensor_tensor(out=xn2q, in0=xn2q, in1=gam2_bc, op=ALU.mult))(),
                 reads=[xn2k, "gam2_bc0", "gam2_bc1"], writes=[xn2k])
            p.op("pool", (lambda hb=hb, xn2q=xn2q: lambda e: e.tensor_tensor(out=hb, in0=xn2q, in1=sh2_bc, op=ALU.add))(),
                 reads=[xn2k, "sh2_bc0", "sh2_bc1"], writes=[hbk])
            p.dma("sp", (lambda hb=hb, t=t: lambda e: e.dma_start(out=h2tm_v[:, t, :], in_=hb))(),
                  "st_h2tm%d" % q, reads=[hbk], writes=["h2tmd_%d" % t])
            bk, bkk = nb()
            for kc in range(8):
                p.op("pe", (lambda bk=bk, kc=kc, h2fq=h2fq: lambda e: e.matmul(bk[:, 0:36], lhsT=h2fq[:, kc, :], rhs=wr_t[:, kc, :],
                                                                              start=(kc == 0), stop=(kc == 7)))(),
                     reads=H2FK + ["wr"], writes=[bkk])
            return (bk, bkk)

        def router_a(j, rb):
            bk, bkk = rb
            p.op("dve", (lambda bk=bk: lambda e: e.tensor_tensor(out=lgt, in0=bk[:, 0:36], in1=br_bc, op=ALU.add))(),
                 reads=[bkk, "br_bc"], writes=["lgt"])
            p.op("dve", lambda e: e.tensor_reduce(out=gmax, in_=lgt[:, 0:4], axis=AX.X, op=ALU.max), reads=["lgt"], writes=["gmax"])
            p.op("dve", lambda e: e.tensor_scalar(out=ngmax, in0=gmax, scalar1=-1.0, scalar2=None, op0=ALU.mult),
                 reads=["gmax"], writes=["ngmax"])
            gsum = ssall[:, 64 + 4 * c + j:65 + 4 * c + j]
            p.op("act", (lambda gsum=gsum: lambda e: e.activation(out=gex, in_=lgt[:, 0:4], func=AF.Exp, bias=ngmax, scale=1.0, accum_out=gsum))(),
                 reads=["lgt", "ngmax", "ssall"], writes=["gex", "gsum"])
            p.op("dve", lambda e: e.tensor_scalar(out=goh, in0=lgt[:, 0:4], scalar1=gmax, scalar2=-1.0, op0=ALU.is_ge, op1=ALU.add),
                 reads=["lgt", "gmax"], writes=["goh"])
            p.op("dve", lambda e: e.tensor_scalar(out=pen, in0=goh, scalar1=1e30, scalar2=None, op0=ALU.mult),
                 reads=["goh"], writes=["pen"])
            for g in range(4):
                p.op("dve", (lambda g=g: lambda e: e.tensor_scalar(out=elm[:, g * 8:(g + 1) * 8], in0=lgt[:, 4 + g * 8:4 + (g + 1) * 8],
                                                                   scalar1=pen[:, g:g + 1], scalar2=None, op0=ALU.add))(),
                     reads=["lgt", "pen"], writes=["elm%d" % g])
            p.op("dve", lambda e: e.max(out=mx8, in_=elm), reads=ELK, writes=["mx8"])
            p.op("dve", lambda e: e.tensor_tensor(out=dd, in0=mx8[:, 0:1], in1=mx8[:, 1:2], op=ALU.subtract), reads=["mx8"], writes=["dd"])
            p.op("act", lambda e: e.activation(out=w1c, in_=dd, func=AF.Exp, scale=-1.0), reads=["dd"], writes=["w1c"])

        def router_b(j):
            t = 4 * c + j
            gsum = ssall[:, 64 + t:65 + t]
            p.op("dve", (lambda gsum=gsum: lambda e: e.reciprocal(out=pg, in_=gsum))(), reads=["gsum"], writes=["pg"])
            p.op("dve", lambda e: e.tensor_scalar(out=w1c, in0=w1c, scalar1=1.0, scalar2=None, op0=ALU.add), reads=["w1c"], writes=["w1c"])
            p.op("dve", lambda e: e.reciprocal(out=w1c, in_=w1c), reads=["w1c"], writes=["w1c"])
            p.op("dve", lambda e: e.tensor_tensor(out=w1c, in0=w1c, in1=pg, op=ALU.mult), reads=["w1c", "pg"], writes=["w1c"])
            p.op("dve", lambda e: e.tensor_tensor(out=w2c, in0=pg, in1=w1c, op=ALU.subtract), reads=["w1c", "pg"], writes=["w2c"])
            p.op("dve", (lambda t=t: lambda e: e.tensor_scalar(out=OH1[:, t, :], in0=elm, scalar1=mx8[:, 0:1], scalar2=None, op0=ALU.is_equal))(),
                 reads=ELK + ["mx8"], writes=["OH1_%d" % t])
            p.op("dve", (lambda t=t: lambda e: e.tensor_scalar(out=OH2[:, t, :], in0=elm, scalar1=mx8[:, 1:2], scalar2=None, op0=ALU.is_equal))(),
                 reads=ELK + ["mx8"], writes=["OH2_%d" % t])
            p.op("dve", (lambda t=t: lambda e: e.tensor_copy(out=W12[:, t, 0:1], in_=w1c))(), reads=["w1c"], writes=["W1_%d" % t])
            p.op("dve", (lambda t=t: lambda e: e.tensor_copy(out=W12[:, t, 1:2], in_=w2c))(), reads=["w2c"], writes=["W2_%d" % t])

        mix_m1(0)
        for j in range(4):
            if j + 1 < 4:
                mix_m1(j + 1)
            if j >= 1:
                router_b(j - 1)
            rb_ = mix_m2(j)
            router_a(j, rb_)
        router_b(3)
        if c == 0 and cut('p6'):
            return nc

    if DEBUG_STOP == "phase1":
        p.wait_all("sp", ["x1d_%d" % t for t in range(NT)] + ["h2tmd_%d" % t for t in range(NT)])
        p.emit()
        return nc

    p.barrier(barw)
    A.top = oh_top
    Ltri = A.bf16(128)
    onesb = A.bf16(128)
    OS = A.bf16(32)
    base = A.f32(32)
    Rall = A.f32(NT, 32)
    nblk = A.f32(32)
    padded = A.f32(32)
    pend = A.f32(32)
    pstart = A.f32(32)
    ones32 = A.f32(32)
    cmpt = A.f32(32)
    tmpd = A.f32(32)
    prod = A.f32(32)
    bef = A.f32(NBLK)
    idxwf = A.f32(NBLK)
    be512 = A.f32(NBLK)
    idx2f = A.f32(4 * NBLK)
    iop_i = A.ar[:, A.top:A.top + 1].bitcast(I32); A.top += 1
    iop_f = A.f32(1)
    destf = A.f32(2 * NT)
    disp_top = A.top

    p.op("pool", lambda e: e.memset(onesb, 1.0), writes=["onesb"])
    p.op("pool", lambda e: e.memset(Ltri, 1.0), writes=["Ltri"])
    p.op("pool", lambda e: e.affine_select(out=Ltri, in_=Ltri, pattern=[[1, 128]], compare_op=ALU.is_gt,
                                           fill=0.0, base=0, channel_multiplier=-1), reads=["Ltri"], writes=["Ltri"])
    p.op("pool", lambda e: e.iota(out=iop_i, pattern=[[0, 1]], base=0, channel_multiplier=1), writes=["iop_i"])
    p.op("pool", lambda e: e.tensor_copy(out=iop_f, in_=iop_i), reads=["iop_i"], writes=["iop_f"])
    p.op("dve", lambda e: e.memset(base, 0.0), writes=["base"])
    p.op("dve", lambda e: e.memset(ones32, 1.0), writes=["ones32"])
    p.op("dve", lambda e: e.memset(epsA, EPS), writes=["epsA"])
    for t in range(NT):
        p.op("dve", (lambda t=t: lambda e: e.tensor_tensor(out=OS, in0=OH1[:, t, :], in1=OH2[:, t, :], op=ALU.add))(),
             reads=["OH1_%d" % t, "OH2_%d" % t], writes=["OS"])
        bk, bkk = nb()
        p.op("pe", (lambda bk=bk: lambda e: e.matmul(bk[:, 0:32], lhsT=onesb, rhs=OS, start=True, stop=True))(),
             reads=["onesb", "OS"], writes=[bkk])
        p.op("pe", (lambda bk=bk: lambda e: e.matmul(bk[:, 32:64], lhsT=Ltri, rhs=OS, start=True, stop=True))(),
             reads=["Ltri", "OS"], writes=[bkk])
        p.op("dve", (lambda bk=bk, t=t: lambda e: e.tensor_tensor(out=Rall[:, t, :], in0=bk[:, 32:64], in1=base, op=ALU.add))(),
             reads=[bkk, "base"], writes=["R_%d" % t])
        p.op("dve", (lambda bk=bk: lambda e: e.tensor_tensor(out=base, in0=bk[:, 0:32], in1=base, op=ALU.add))(),
             reads=[bkk, "base"], writes=["base"])
    p.op("dve", lambda e: e.memset(nblk, 0.0), writes=["nblk"])
    for m in range(17):
        p.op("dve", (lambda m=m: lambda e: e.scalar_tensor_tensor(out=nblk, in0=base, scalar=float(BLK * m), in1=nblk,
                                                                 op0=ALU.is_gt, op1=ALU.add))(), reads=["base", "nblk"], writes=["nblk"])
    p.op("dve", lambda e: e.tensor_scalar(out=padded, in0=nblk, scalar1=float(BLK), scalar2=None, op0=ALU.mult),
         reads=["nblk"], writes=["padded"])
    p.op("dve", lambda e: e.tensor_tensor_scan(out=pend, data0=ones32, data1=padded, initial=0.0, op0=ALU.mult, op1=ALU.add),
         reads=["ones32", "padded"], writes=["pend"])
    p.op("dve", lambda e: e.tensor_tensor(out=pstart, in0=pend, in1=padded, op=ALU.subtract), reads=["pend", "padded"], writes=["pstart"])
    for b in range(NBLK):
        p.op("dve", (lambda b=b: lambda e: e.tensor_scalar(out=cmpt, in0=pend, scalar1=float(b * BLK), scalar2=0.0,
                                                          op0=ALU.is_le, op1=ALU.add, accum_out=bef[:, b:b + 1]))(),
             reads=["pend"], writes=["cmpt", "bef%d" % b])
    BEK = ["bef%d" % b for b in range(NBLK)]
    p.op("dve", lambda e: e.tensor_scalar(out=idxwf, in0=bef, scalar1=31.0, scalar2=128.0, op0=ALU.min, op1=ALU.mult),
         reads=BEK, writes=["idxwf"])
    p.op("dve", lambda e: e.tensor_scalar(out=idxwf, in0=idxwf, scalar1=iop_f[:, 0:1], scalar2=None, op0=ALU.add),
         reads=["idxwf", "iop_f"], writes=["idxwf"])
    p.op("dve", lambda e: e.tensor_copy(out=idxwi, in_=idxwf), reads=["idxwf"], writes=["idxwi"])
    p.op("dve", lambda e: e.tensor_scalar(out=be512, in0=bef, scalar1=31.0, scalar2=512.0, op0=ALU.min, op1=ALU.mult),
         reads=BEK, writes=["be512"])
    p.op("dve", lambda e: e.tensor_scalar(out=be512, in0=be512, scalar1=iop_f[:, 0:1], scalar2=None, op0=ALU.add),
         reads=["be512", "iop_f"], writes=["be512"])
    idx2f3 = idx2f.rearrange("p (b h) -> p b h", h=4)
    for hc in range(4):
        p.op("dve", (lambda hc=hc: lambda e: e.tensor_scalar(out=idx2f3[:, :, hc], in0=be512, scalar1=float(hc * 128), scalar2=None, op0=ALU.add))(),
             reads=["be512"], writes=["idx2f_%d" % hc])
    p.op("dve", lambda e: e.tensor_copy(out=idx2i, in_=idx2f), reads=["idx2f_%d" % hc for hc in range(4)], writes=["idx2i"])
    NSTG = 6
    stg2 = [A.ar[:, NW - (NSTG - i) * 4096:NW - (NSTG - i - 1) * 4096] for i in range(NSTG)]
    ew1_r = ew1.rearrange("e (p k) n -> (e p) (k n)", p=128)
    ew3_r = ew3.rearrange("e (p k) n -> (e p) (k n)", p=128)
    ew2_r = ew2.rearrange("e h n -> (e h) n")

    def gather_block_weights(b):
        sset = b % 2
        for m, src in enumerate((ew1_r, ew3_r)):
            st = stg2[3 * sset + m]
            sk = "stg2_%d_%d" % (sset, m)
            p.dma("pool", (lambda st=st, src=src, b=b: lambda e: e.indirect_dma_start(
                out=st, out_offset=None, in_=src, in_offset=bass.IndirectOffsetOnAxis(ap=idxwi[:, b:b + 1], axis=0)))(),
                "ld_" + sk, reads=["idxwi"], writes=[sk])
        st = stg2[3 * sset + 2]
        for hc in range(4):
            sk = "stg2_%d_2_%d" % (sset, hc)
            p.dma("pool", (lambda st=st, b=b, hc=hc: lambda e: e.indirect_dma_start(
                out=st[:, hc * 1024:(hc + 1) * 1024], out_offset=None, in_=ew2_r,
                in_offset=bass.IndirectOffsetOnAxis(ap=idx2i[:, 4 * b + hc:4 * b + hc + 1], axis=0)))(),
                "ld_" + sk, reads=["idx2i"], writes=[sk])

    gather_block_weights(0)
    gather_block_weights(1)
    for t in range(NT):
        p.op("dve", (lambda t=t: lambda e: e.tensor_tensor(out=tmpd, in0=Rall[:, t, :], in1=pstart, op=ALU.add))(),
             reads=["R_%d" % t, "pstart"], writes=["tmpd"])
        for k, OH in enumerate((OH1, OH2)):
            p.op("dve", (lambda t=t, OH=OH: lambda e: e.tensor_tensor(out=prod, in0=tmpd, in1=OH[:, t, :], op=ALU.mult))(),
                 reads=["tmpd", "OH%d_%d" % (k + 1, t)], writes=["prod"])
            p.op("dve", (lambda t=t, k=k: lambda e: e.tensor_reduce(out=destf[:, 2 * t + k:2 * t + k + 1], in_=prod, axis=AX.X, op=ALU.add))(),
                 reads=["prod"], writes=["destf_%d_%d" % (t, k)])
        p.op("dve", (lambda t=t: lambda e: e.tensor_copy(out=desti[:, 2 * t:2 * t + 2], in_=destf[:, 2 * t:2 * t + 2]))(),
             reads=["destf_%d_0" % t, "destf_%d_1" % t], writes=["desti_%d" % t])
    DSTK = ["desti_%d" % t for t in range(NT)]

    hsc = [A.bf16(1024) for _ in range(8)]
    for t in range(NT):
        hb = hsc[t % 8]
        hbk = "hsc%d" % (t % 8)
        p.dma("sp", (lambda hb=hb, t=t: lambda e: e.dma_start(out=hb, in_=h2tm_v[:, t, :]))(), "ld_" + hbk,
              reads=["h2tmd_%d" % t], writes=[hbk])
        for k in range(2):
            p.dma("pool", (lambda hb=hb, t=t, k=k: lambda e: e.indirect_dma_start(
                out=xs_d, out_offset=bass.IndirectOffsetOnAxis(ap=desti[:, 2 * t + k:2 * t + k + 1], axis=0),
                in_=hb, in_offset=None))(), "sc_%d_%d" % (t % 8, k), reads=[hbk, "desti_%d" % t] + XSZK, writes=["xs_sc_%d_%d" % (t, k)])
    XSK = ["xs_sc_%d_%d" % (t, k) for t in range(NT) for k in range(2)]

    p.barrier(barw)
    A.top = const_top
    wb = [[A.bf16(8, 512), A.bf16(8, 512), A.bf16(4, 1024)] for _ in range(2)]
    xs_sb = [A.bf16(4, 1024) for _ in range(1)]
    xsT = [A.bf16(8, 512) for _ in range(2)]
    gT = [A.bf16(4, 512) for _ in range(2)]
    s1 = [A.f32(512) for _ in range(1)]
    ysb = [A.f32(1024) for _ in range(4)]
    print("stage C arena top", A.top, "of", NW - NSTG * 4096)
    assert A.top <= NW - NSTG * 4096
    xs_v = xs_d.rearrange("(b s p) d -> b p s d", p=128, s=4)
    yb_v = ybuf_d.rearrange("(b s p) d -> b s p d", p=128, s=4)

    def cast_block_weights(b):
        sset = b % 2
        wbuf = wb[b % 2]
        st0 = stg2[3 * sset].rearrange("p (k n) -> p k n", k=8)
        st1 = stg2[3 * sset + 1].rearrange("p (k n) -> p k n", k=8)
        st2 = stg2[3 * sset + 2].rearrange("p (k n) -> p k n", k=4)
        p.op("dve", (lambda st0=st0, wbuf=wbuf: lambda e: e.tensor_copy(out=wbuf[0], in_=st0))(),
             reads=["stg2_%d_0" % sset], writes=["wb%d_0" % (b % 2)])
        p.op("act", (lambda st1=st1, wbuf=wbuf: lambda e: e.copy(out=wbuf[1], in_=st1))(),
             reads=["stg2_%d_1" % sset], writes=["wb%d_1" % (b % 2)])
        p.op("dve", (lambda st2=st2, wbuf=wbuf: lambda e: e.tensor_copy(out=wbuf[2], in_=st2))(),
             reads=["stg2_%d_2_%d" % (sset, hc) for hc in range(4)], writes=["wb%d_2" % (b % 2)])

    def load_xs(b):
        xb_ = xs_sb[0]
        p.dma("sp", (lambda xb_=xb_, b=b: lambda e: e.dma_start(out=xb_, in_=xs_v[b]))(), "ld_xs_sb0",
              reads=XSK, writes=["xs_sb0"])

    def transposes(b):
        xb_ = xs_sb[0]
        xbk = "xs_sb0"
        xT = xsT[b % 2]
        for st_ in range(4):
            tb, tbk = nb()
            tpv = tb.bitcast(BF16)
            xv = xb_[:, st_, :].rearrange("s (p k) -> s k p", k=8)
            for kc in range(8):
                p.op("pe", (lambda tpv=tpv, xv=xv, kc=kc: lambda e: e.transpose(out=tpv[:, kc * 128:(kc + 1) * 128], in_=xv[:, kc, :],
                                                                                identity=identb))(),
                     reads=[xbk, "identb"], writes=[tbk])
            src3 = tpv.rearrange("p (k s) -> p k s", k=8)
            dst3 = xT[:, :, st_ * 128:(st_ + 1) * 128]
            xTk = "xsT%d_%d" % (b % 2, st_)
            if st_ % 2 == 0:
                p.op("act", (lambda src3=src3, dst3=dst3: lambda e: e.copy(out=dst3, in_=src3))(), reads=[tbk], writes=[xTk])
            else:
                p.op("dve", (lambda src3=src3, dst3=dst3: lambda e: e.tensor_copy(out=dst3, in_=src3))(), reads=[tbk], writes=[xTk])

    load_xs(0)
    cast_block_weights(0)
    transposes(0)
    load_xs(1)
    for b in range(NBLK):
        wbuf = wb[b % 2]
        WK = ["wb%d_%d" % (b % 2, m) for m in range(3)]
        xT = xsT[b % 2]
        XTK = ["xsT%d_%d" % (b % 2, st_) for st_ in range(4)]
        gTb = gT[b % 2]
        for hc in range(4):
            b1, b1k = nb()
            b3, b3k = nb()
            for kc in range(8):
                p.op("pe", (lambda b1=b1, wbuf=wbuf, kc=kc, hc=hc, xT=xT: lambda e: e.matmul(
                    b1, lhsT=wbuf[0][:, kc, hc * 128:(hc + 1) * 128], rhs=xT[:, kc, :], start=(kc == 0), stop=(kc == 7)))(),
                    reads=XTK + [WK[0]], writes=[b1k])
            for kc in range(8):
                p.op("pe", (lambda b3=b3, wbuf=wbuf, kc=kc, hc=hc, xT=xT: lambda e: e.matmul(
                    b3, lhsT=wbuf[1][:, kc, hc * 128:(hc + 1) * 128], rhs=xT[:, kc, :], start=(kc == 0), stop=(kc == 7)))(),
                    reads=XTK + [WK[1]], writes=[b3k])
            s1b = s1[0]
            s1k = "s1_0"
            p.op("act", (lambda b1=b1, s1b=s1b: lambda e: e.activation(out=s1b, in_=b1, func=AF.Silu))(), reads=[b1k], writes=[s1k])
            p.op("dve", (lambda b3=b3, s1b=s1b, gTb=gTb, hc=hc: lambda e: e.tensor_tensor(out=gTb[:, hc, :], in0=b3, in1=s1b, op=ALU.mult))(),
                 reads=[b3k, s1k], writes=["gT%d_%d" % (b % 2, hc)])
        if b + 1 < NBLK:
            transposes(b + 1)
            if b + 2 < NBLK:
                load_xs(b + 2)
            cast_block_weights(b + 1)
        if b + 2 < NBLK:
            gather_block_weights(b + 2)
        GK = ["gT%d_%d" % (b % 2, hc) for hc in range(4)]
        for st_ in range(4):
            yi = st_
            yb_ = ysb[yi]
            for dh in range(2):
                by, byk = nb()
                for hc in range(4):
                    p.op("pe", (lambda by=by, gTb=gTb, hc=hc, st_=st_, dh=dh, wbuf=wbuf: lambda e: e.matmul(
                        by, lhsT=gTb[:, hc, st_ * 128:(st_ + 1) * 128], rhs=wbuf[2][:, hc, dh * 512:(dh + 1) * 512],
                        start=(hc == 0), stop=(hc == 3)))(), reads=GK + [WK[2]], writes=[byk])
                if dh == 0:
                    p.op("act", (lambda by=by, yb_=yb_: lambda e: e.copy(out=yb_[:, 0:512], in_=by))(), reads=[byk], writes=["ysb%d_0" % yi])
                else:
                    p.op("dve", (lambda by=by, yb_=yb_: lambda e: e.tensor_copy(out=yb_[:, 512:1024], in_=by))(), reads=[byk], writes=["ysb%d_1" % yi])
            p.dma("sp", (lambda yb_=yb_, b=b, st_=st_: lambda e: e.dma_start(out=yb_v[b, st_], in_=yb_))(), "st_ysb%d" % yi,
                  reads=["ysb%d_0" % yi, "ysb%d_1" % yi], writes=["ybuf_%d_%d" % (b, st_)])
    YBK = ["ybuf_%d_%d" % (b, st_) for b in range(NBLK) for st_ in range(4)]

    p.barrier(barw)
    A.top = const_top
    x1r = [A.f32(1024) for _ in range(4)]
    Y1 = [A.f32(1024) for _ in range(4)]
    Y2 = [A.f32(1024) for _ in range(4)]
    tcm = [A.f32(1024) for _ in range(4)]
    oo = [A.f32(1024) for _ in range(4)]
    junk2 = A.bf16(1024)
    sm2 = A.f32(16)
    ssD = A.f32(NT)
    p.op("pool", lambda e: e.memset(ssD, 0.0), writes=["ssD"])

    def d_a(t):
        i2 = t % 4
        p.dma("sp", (lambda t=t, i2=i2: lambda e: e.dma_start(out=x1r[i2], in_=x1_v[:, t, :]))(), ["ld_xa0", "ld_xa1", "ld_xres0", "ld_xres1"][i2],
              reads=["x1d_%d" % t], writes=["x1r%d" % i2])
        for k, Yb in enumerate((Y1, Y2)):
            p.dma("pool", (lambda t=t, k=k, Yb=Yb, i2=i2: lambda e: e.indirect_dma_start(
                out=Yb[i2], out_offset=None, in_=ybuf_d,
                in_offset=bass.IndirectOffsetOnAxis(ap=desti[:, 2 * t + k:2 * t + k + 1], axis=0)))(),
                "ld_stg2_%d_2_%d" % (k, i2), reads=YBK + ["desti_%d" % t], writes=["Y%d_%d" % (k, i2)])
        p.op("act", (lambda t=t, i2=i2: lambda e: e.activation(out=tcm[i2], in_=Y1[i2], func=AF.Copy, scale=W12[:, t, 0:1]))(),
             reads=["Y0_%d" % i2, "W1_%d" % t], writes=["tcm%d" % i2])

    def d_b(t):
        i2 = t % 4
        tk = "tcm%d" % i2
        p.op("dve", (lambda t=t, i2=i2: lambda e: e.scalar_tensor_tensor(out=tcm[i2], in0=Y2[i2], scalar=W12[:, t, 1:2], in1=tcm[i2],
                                                                         op0=ALU.mult, op1=ALU.add))(),
             reads=["Y1_%d" % i2, "W2_%d" % t, tk], writes=[tk])
        p.op("dve", (lambda i2=i2: lambda e: e.tensor_tensor(out=tcm[i2], in0=tcm[i2], in1=g2_bc, op=ALU.mult))(), reads=[tk, "g2_bc0", "g2_bc1"], writes=[tk])
        p.op("dve", (lambda i2=i2: lambda e: e.tensor_tensor(out=tcm[i2], in0=tcm[i2], in1=x1r[i2], op=ALU.add))(),
             reads=[tk, "x1r%d" % i2], writes=[tk])

    def d_c(t):
        i2 = t % 4
        tk = "tcm%d" % i2
        ssf = ssD[:, t:t + 1]
        lnc = sm2[:, 2 * i2:2 * i2 + 1]
        rsc = sm2[:, 2 * i2 + 1:2 * i2 + 2]
        p.op("act", (lambda ssf=ssf, i2=i2: lambda e: e.activation(out=junk2, in_=tcm[i2], func=AF.Square, accum_out=ssf))(),
             reads=[tk, "ssD"], writes=["junk2", "ssf%d" % i2])
        p.op("act", (lambda ssf=ssf, lnc=lnc: lambda e: e.activation(out=lnc, in_=ssf, func=AF.Ln, bias=epsA[:, 0:1], scale=1.0 / D))(),
             reads=["ssf%d" % i2, "epsA"], writes=["sqf%d" % i2])
        p.op("act", (lambda lnc=lnc, rsc=rsc: lambda e: e.activation(out=rsc, in_=lnc, func=AF.Exp, scale=-0.5))(), reads=["sqf%d" % i2], writes=["rstdf%d" % i2])

    def d_d(t):
        i2 = t % 4
        rsc = sm2[:, 2 * i2 + 1:2 * i2 + 2]
        p.op("dve", (lambda i2=i2, rsc=rsc: lambda e: e.scalar_tensor_tensor(out=oo[i2], in0=tcm[i2], scalar=rsc, in1=fg_bc, op0=ALU.mult, op1=ALU.mult))(),
             reads=["tcm%d" % i2, "rstdf%d" % i2, "fg_bc"], writes=["oo%d" % i2])
        p.dma("sp", (lambda t=t, i2=i2: lambda e: e.dma_start(out=out_v[:, t, :], in_=oo[i2]))(), "st_ysb%d" % i2,
              reads=["oo%d" % i2], writes=["outd_%d" % t])

    d_a(0)
    for t in range(NT):
        if t + 1 < NT:
            d_a(t + 1)
        d_b(t)
        d_c(t)
        if t >= 1:
            d_d(t - 1)
    d_d(NT - 1)

    p.wait_all("sp", ["outd_%d" % t for t in range(NT)])
    p.emit()
    return nc


_NC_CACHE = {}


def _prep_inputs(inp, b):
    f = np.float32

    def colz(v, n):
        return np.ascontiguousarray(np.asarray(v, f).reshape(n, 128).T)

    m = {}
    m["x"] = np.ascontiguousarray(inp["x"][b])
    m["c_col"] = colz(inp["c"][b], 8)
    m["ada_w"] = np.ascontiguousarray(inp["ada_w"][0])
    m["ada_b"] = colz(inp["ada_b"][0], 48)
    m["adab_bc"] = np.ascontiguousarray(np.broadcast_to(np.asarray(inp["ada_b"][0], f)[None, :], (128, 6 * D)))
    m["n2g_bc"] = np.ascontiguousarray(np.broadcast_to(np.asarray(inp["norm2_g"][0], f)[None, :], (128, D)))
    m["n1g"] = colz(inp["norm1_g"][0], 8)
    m["n2g"] = colz(inp["norm2_g"][0], 8)
    m["fg_bc"] = np.ascontiguousarray(np.broadcast_to(np.asarray(inp["final_g"], f)[None, :], (128, D)))
    m["w_in"] = np.ascontiguousarray(inp["w_in"][0])
    m["w_out"] = np.ascontiguousarray(inp["w_out"][0])
    cwv = np.asarray(inp["conv_w"][0], f)
    m["conv_w"] = np.ascontiguousarray(cwv.T.reshape(4, 128, 4).transpose(1, 0, 2))
    m["conv_b"] = colz(inp["conv_b"][0], 4)
    m["gaw"] = np.ascontiguousarray(inp["gate_a_w"][0])
    m["giw"] = np.ascontiguousarray(inp["gate_i_w"][0])
    m["gab"] = colz(inp["gate_a_b"][0], 4)
    m["gib"] = colz(inp["gate_i_b"][0], 4)
    m["lam"] = colz(inp["lru_lambda"][0], 4)
    m["lng_bc"] = np.ascontiguousarray(np.broadcast_to(np.asarray(inp["sgu_ln_g"][0], f)[None, :], (128, 512)))
    m["lnb_bc"] = np.ascontiguousarray(np.broadcast_to(np.asarray(inp["sgu_ln_b"][0], f)[None, :], (128, 512)))
    m["sgu_wT"] = np.ascontiguousarray(np.asarray(inp["sgu_w"][0], f).transpose(2, 0, 1))
    bs = np.asarray(inp["sgu_b"][0], f)
    bsr = np.repeat(bs, 64, axis=0)
    bsr = np.tile(bsr, (1, 4))
    m["bsrep"] = np.ascontiguousarray(bsr.reshape(4, 128, 512).transpose(1, 0, 2))
    wr = np.concatenate([np.asarray(inp["router_group_w"][0], f), np.asarray(inp["router_expert_w"][0], f)], axis=1)
    m["wr"] = np.ascontiguousarray(wr.reshape(8, 128, 36).transpose(1, 0, 2))
    br = np.concatenate([np.asarray(inp["router_group_b"][0], f), np.asarray(inp["router_expert_b"][0], f)])
    m["br_bc"] = np.ascontiguousarray(np.broadcast_to(br[None, :], (128, 36)))
    m["zeros_blk"] = np.zeros((512, D), dtype=ml_dtypes.bfloat16)
    m["ew1"] = np.ascontiguousarray(inp["expert_w1"][0])
    m["ew3"] = np.ascontiguousarray(inp["expert_w3"][0])
    m["ew2"] = np.ascontiguousarray(inp["expert_w2"][0])
    return m


def kernel(**inputs):
    inp = {k: np.asarray(v) for k, v in inputs.items()}
    if "nc" not in _NC_CACHE:
        _NC_CACHE["nc"] = build_nc()
    nc = _NC_CACHE["nc"]
    in_maps = [_prep_inputs(inp, b) for b in range(8)]
    res = run_bass_kernel_spmd(nc, in_maps, core_ids=list(range(8)))
    _NC_CACHE["last"] = res
    outs = [np.asarray(res.results[b]["out"]).reshape(S, D) for b in range(8)]
    return np.stack(outs, axis=0).astype(np.float32)
```
